# Optimizing a Trainium2 kernel written in Bass

```python
import jax, jax.numpy as jnp
from jax import lax
import numpy as np

D_MODEL = 2048
BATCH = 2
SEQ = 8192
DEPTH = 1

N_MEM = 256
GLA_HEADS = 4
GLA_KEY = D_MODEL // 2
GLA_VAL = D_MODEL
GLA_DK = GLA_KEY // GLA_HEADS
GLA_DV = GLA_VAL // GLA_HEADS
GLA_RANK = 16
GLA_TAU = 16.0
HGRN_DK = 128
HGRN_HEADS = D_MODEL // HGRN_DK
HGRN_KEY = HGRN_HEADS * HGRN_DK
HGRN_VAL = D_MODEL
HGRN_DV = HGRN_VAL // HGRN_HEADS
CHUNK = 64
X_HEADS = 4
X_DH = D_MODEL // X_HEADS
N_EXPERTS = 32
TOP_K = 4
D_EXPERT = D_MODEL
SWIGLU_LIMIT = 7.0
SWIGLU_ALPHA = 1.702
MOE_BLOCK = 256
DEEPNORM_ALPHA = (2.0 * DEPTH) ** 0.25
DEEPNORM_BETA = (8.0 * DEPTH) ** -0.25
NORM_EPS = 1e-5

IN_SPLITS = (GLA_KEY, GLA_KEY, GLA_VAL, GLA_VAL, GLA_RANK,
             HGRN_KEY, HGRN_KEY, HGRN_VAL, HGRN_VAL,
             D_MODEL, D_MODEL)
D_IN = sum(IN_SPLITS)
SPLIT_OFFSETS = tuple(int(o) for o in np.cumsum(IN_SPLITS)[:-1])

kernel_name = "hybrid_gla_hgrn2_deepnorm_memxattn_moe"


def layer_norm(x, g, b):
    xf = x.astype(jnp.float32)
    mu = jnp.mean(xf, axis=-1, keepdims=True)
    var = jnp.mean(jnp.square(xf - mu), axis=-1, keepdims=True)
    return ((xf - mu) * lax.rsqrt(var + NORM_EPS) * g + b).astype(x.dtype)


def head_rmsnorm(o, g):
    return o * lax.rsqrt(jnp.mean(jnp.square(o), axis=-1, keepdims=True) + NORM_EPS) * g.astype(jnp.float32)


def to_heads(t, n_heads):
    b, s, _ = t.shape
    return t.reshape(b, s, n_heads, -1).transpose(0, 2, 1, 3)


def from_heads(t):
    b, h, s, d = t.shape
    return t.transpose(0, 2, 1, 3).reshape(b, s, h * d)


def chunked_gated_linear_attention(q, k, v, log_g):
    b, h, s, dk = q.shape
    dv = v.shape[-1]
    n_chunks = s // CHUNK

    def to_chunks(t):
        return t.astype(jnp.float32).reshape(b, h, n_chunks, CHUNK, t.shape[-1]).transpose(2, 0, 1, 3, 4)

    causal = jnp.tril(jnp.ones((CHUNK, CHUNK), dtype=bool))[:, :, None]

    def step(state, inp):
        qc, kc, vc, gc = inp
        cum = jnp.cumsum(gc, axis=2)
        o_inter = jnp.einsum("bhcd,bhde->bhce", qc * jnp.exp(cum), state)
        diff = cum[:, :, :, None, :] - cum[:, :, None, :, :]
        decay = jnp.exp(jnp.where(causal, diff, -jnp.inf))
        scores = jnp.einsum("bhid,bhjd,bhijd->bhij", qc, kc, decay)
        o_intra = jnp.einsum("bhij,bhje->bhie", scores, vc)
        last = cum[:, :, -1:, :]
        new_state = (jnp.exp(last)[:, :, 0, :, None] * state
                     + jnp.einsum("bhcd,bhce->bhde", kc * jnp.exp(last - cum), vc))
        return new_state, o_inter + o_intra

    state0 = jnp.zeros((b, h, dk, dv), jnp.float32)
    _, o = lax.scan(step, state0, (to_chunks(q), to_chunks(k), to_chunks(v), to_chunks(log_g)))
    return o.transpose(1, 2, 0, 3, 4).reshape(b, h, s, dv)


def token_mixer(h, w_in, b_in, w_gla_a2, b_gla_a, gla_norm_g, hgrn_norm_g, hgrn_lb, w_o):
    dt = h.dtype
    proj = h @ w_in + b_in
    gq, gk, gv, gr, ga1, hq, hf, hi, hg, m_a, m_b = jnp.split(proj, SPLIT_OFFSETS, axis=-1)

    log_alpha = jax.nn.log_sigmoid((ga1 @ w_gla_a2 + b_gla_a).astype(jnp.float32)) / GLA_TAU
    o_gla = chunked_gated_linear_attention(
        to_heads(gq.astype(jnp.float32) * GLA_DK ** -0.5, GLA_HEADS), to_heads(gk, GLA_HEADS),
        to_heads(gv, GLA_HEADS), to_heads(log_alpha, GLA_HEADS))
    o_gla = from_heads(head_rmsnorm(o_gla, gla_norm_g)) * jax.nn.silu(gr.astype(jnp.float32))

    log_f = jnp.logaddexp(jnp.log(hgrn_lb), jnp.log1p(-hgrn_lb) + jax.nn.log_sigmoid(hf.astype(jnp.float32)))
    k_h = -jnp.expm1(log_f)
    o_h = chunked_gated_linear_attention(
        to_heads(jax.nn.silu(hq.astype(jnp.float32)), HGRN_HEADS), to_heads(k_h, HGRN_HEADS),
        to_heads(hi, HGRN_HEADS), to_heads(log_f, HGRN_HEADS))
    o_h = from_heads(head_rmsnorm(o_h, hgrn_norm_g)) * jax.nn.sigmoid(hg.astype(jnp.float32))

    merged = (jax.nn.sigmoid(m_a.astype(jnp.float32)) * o_gla
              + jax.nn.sigmoid(m_b.astype(jnp.float32)) * o_h)
    return merged.astype(dt) @ w_o


def memory_cross_attention(h, mem, w_xq, w_mem_kv, w_xo):
    b, s, d = h.shape
    q = (h @ w_xq).reshape(b, s, X_HEADS, X_DH)
    k, v = jnp.split(mem @ w_mem_kv, 2, axis=-1)
    k = k.reshape(b, N_MEM, X_HEADS, X_DH)
    v = v.reshape(b, N_MEM, X_HEADS, X_DH)
    scores = jnp.einsum("bshd,bmhd->bhsm", q, k).astype(jnp.float32) * X_DH ** -0.5
    p = jax.nn.softmax(scores, axis=-1)
    o = jnp.einsum("bhsm,bmhd->bshd", p.astype(v.dtype), v).reshape(b, s, d)
    return o @ w_xo


def clamped_swiglu(gu):
    glu = jnp.minimum(gu[..., ::2], SWIGLU_LIMIT)
    lin = jnp.clip(gu[..., 1::2], -SWIGLU_LIMIT, SWIGLU_LIMIT)
    return glu * jax.nn.sigmoid(SWIGLU_ALPHA * glu) * (lin + 1.0)


def moe(h, w_router, b_router, w_gate_up, b_gate_up, w_down, b_down):
    b, s, d = h.shape
    n_tok = b * s
    n_assign = n_tok * TOP_K
    xt = h.reshape(n_tok, d)
    logits = (xt @ w_router + b_router).astype(jnp.float32)
    top_logit, top_e = lax.top_k(logits, TOP_K)
    gate = jax.nn.softmax(top_logit, axis=-1)

    e_flat = top_e.reshape(n_assign)
    tok_flat = jnp.repeat(jnp.arange(n_tok, dtype=jnp.int32), TOP_K)
    order = jnp.argsort(e_flat)
    e_sorted = e_flat[order]
    tok_sorted = tok_flat[order]
    gate_sorted = gate.reshape(n_assign)[order]
    counts = jnp.bincount(e_flat, length=N_EXPERTS)
    start = jnp.cumsum(counts) - counts
    padded = (counts + MOE_BLOCK - 1) // MOE_BLOCK * MOE_BLOCK
    pend = jnp.cumsum(padded)
    pstart = pend - padded
    dest = pstart[e_sorted] + jnp.arange(n_assign, dtype=jnp.int32) - start[e_sorted]
    n_blocks = -(-n_assign // MOE_BLOCK) + N_EXPERTS
    n_rows = n_blocks * MOE_BLOCK
    row_tok = jnp.full((n_rows,), n_tok, jnp.int32).at[dest].set(tok_sorted)
    row_gate = jnp.zeros((n_rows,), jnp.float32).at[dest].set(gate_sorted)
    block_e = jnp.minimum(jnp.searchsorted(pend, jnp.arange(n_blocks) * MOE_BLOCK, side="right"),
                          N_EXPERTS - 1)
    x_pad = jnp.concatenate([xt, jnp.zeros((1, d), xt.dtype)], axis=0)

    def block_step(acc, inp):
        rows, rgate, e = inp
        xb = x_pad[rows]
        gu = xb @ w_gate_up[e] + b_gate_up[e]
        yb = clamped_swiglu(gu) @ w_down[e] + b_down[e]
        return acc.at[rows].add(yb.astype(jnp.float32) * rgate[:, None]), None

    acc0 = jnp.zeros((n_tok + 1, d), jnp.float32)
    acc, _ = lax.scan(block_step, acc0, (row_tok.reshape(n_blocks, MOE_BLOCK),
                                         row_gate.reshape(n_blocks, MOE_BLOCK), block_e))
    return acc[:n_tok].astype(h.dtype).reshape(b, s, d)


def setup_inputs(seed: int = 0) -> dict:
    key = jax.random.key(seed)
    ks = jax.random.split(key, 25)
    f32 = jnp.float32

    def nrm(k, shape, scale):
        return jax.random.normal(k, shape, f32) * scale

    L = DEPTH
    return {
        "x": nrm(ks[0], (BATCH, SEQ, D_MODEL), 1.0),
        "mem": nrm(ks[1], (BATCH, N_MEM, D_MODEL), 1.0),
        "w_in": nrm(ks[2], (L, D_MODEL, D_IN), D_MODEL ** -0.5),
        "b_in": nrm(ks[3], (L, D_IN), 0.01),
        "w_gla_a2": nrm(ks[4], (L, GLA_RANK, GLA_KEY), GLA_RANK ** -0.5),
        "b_gla_a": nrm(ks[5], (L, GLA_KEY), 0.01),
        "gla_norm_g": 1.0 + nrm(ks[6], (L, GLA_DV), 0.01),
        "hgrn_norm_g": 1.0 + nrm(ks[7], (L, HGRN_DV), 0.01),
        "hgrn_lb_logits": nrm(ks[8], (L + 1, HGRN_KEY), 0.1),
        "w_mix_o": nrm(ks[9], (L, GLA_VAL, D_MODEL), GLA_VAL ** -0.5 * DEEPNORM_BETA),
        "w_xq": nrm(ks[10], (L, D_MODEL, D_MODEL), D_MODEL ** -0.5),
        "w_mem_kv": nrm(ks[11], (L, D_MODEL, 2 * D_MODEL), D_MODEL ** -0.5),
        "w_xo": nrm(ks[12], (L, D_MODEL, D_MODEL), D_MODEL ** -0.5 * DEEPNORM_BETA),
        "w_router": nrm(ks[13], (L, D_MODEL, N_EXPERTS), D_MODEL ** -0.5),
        "b_router": nrm(ks[14], (L, N_EXPERTS), 0.01),
        "w_gate_up": nrm(ks[15], (L, N_EXPERTS, D_MODEL, 2 * D_EXPERT), D_MODEL ** -0.5),
        "b_gate_up": nrm(ks[16], (L, N_EXPERTS, 2 * D_EXPERT), 0.01),
        "w_down": nrm(ks[17], (L, N_EXPERTS, D_EXPERT, D_MODEL), D_EXPERT ** -0.5 * DEEPNORM_BETA),
        "b_down": nrm(ks[18], (L, N_EXPERTS, D_MODEL), 0.01),
        "ln1_g": 1.0 + nrm(ks[19], (L, D_MODEL), 0.01),
        "ln1_b": nrm(ks[20], (L, D_MODEL), 0.01),
        "ln2_g": 1.0 + nrm(ks[21], (L, D_MODEL), 0.01),
        "ln2_b": nrm(ks[22], (L, D_MODEL), 0.01),
        "ln3_g": 1.0 + nrm(ks[23], (L, D_MODEL), 0.01),
        "ln3_b": nrm(ks[24], (L, D_MODEL), 0.01),
    }


def reference(x, mem, w_in, b_in, w_gla_a2, b_gla_a, gla_norm_g, hgrn_norm_g, hgrn_lb_logits,
              w_mix_o, w_xq, w_mem_kv, w_xo, w_router, b_router, w_gate_up, b_gate_up,
              w_down, b_down, ln1_g, ln1_b, ln2_g, ln2_b, ln3_g, ln3_b):
    lb_table = jnp.cumsum(jax.nn.softmax(hgrn_lb_logits.astype(jnp.float32), axis=0), axis=0)
    h = x
    for l in range(DEPTH):
        mix = token_mixer(h, w_in[l], b_in[l], w_gla_a2[l], b_gla_a[l], gla_norm_g[l],
                          hgrn_norm_g[l], lb_table[l], w_mix_o[l])
        h = layer_norm(DEEPNORM_ALPHA * h + mix, ln1_g[l], ln1_b[l])
        xat = memory_cross_attention(h, mem, w_xq[l], w_mem_kv[l], w_xo[l])
        h = layer_norm(DEEPNORM_ALPHA * h + xat, ln2_g[l], ln2_b[l])
        ffn = moe(h, w_router[l], b_router[l], w_gate_up[l], b_gate_up[l], w_down[l], b_down[l])
        h = layer_norm(DEEPNORM_ALPHA * h + ffn, ln3_g[l], ln3_b[l])
    return h
```

```python
import contextlib
import numpy as np
import concourse.bass as bass
import concourse.mybir as mybir
from concourse.bass_utils import run_bass_kernel_spmd

F32 = mybir.dt.float32
BF16 = mybir.dt.bfloat16
I32 = mybir.dt.int32
U32 = mybir.dt.uint32
AF = mybir.ActivationFunctionType
ALU = mybir.AluOpType
AX = mybir.AxisListType

D = 2048
SEQ = 8192
NT_SEQ = SEQ // 128
EPS = 1e-5
ALPHA = 2.0 ** 0.25
SAME_ENGINE_SYNC = True


class V:
    __slots__ = ("ap", "key")

    def __init__(self, ap, key):
        self.ap = ap
        self.key = key


class Tile:
    def __init__(self, handle, key):
        self.h = handle
        self.key = key

    def __getitem__(self, idx):
        return V(self.h[idx], self.key)

    def k(self, sfx):
        return _Keyed(self.h, self.key + ":" + str(sfx))


class _Keyed:
    def __init__(self, h, key):
        self.h = h
        self.key = key

    def __getitem__(self, idx):
        return V(self.h[idx], self.key)


class Sched:
    QS = ("pe", "act", "dve", "pool", "sp")

    def __init__(self, nc, stack):
        self.nc = nc
        self.stack = stack
        self.items = {q: [] for q in self.QS}
        self.esem = {}
        for q in ("pe", "act", "dve", "pool"):
            self.esem[q] = stack.enter_context(nc.semaphore("es_" + q))
        self.cnt = {q: 0 for q in self.QS}
        self.waited = {q: {} for q in self.QS}
        self.res = {}
        self.dsem = {}
        self.semh = {"es_" + q: h for q, h in self.esem.items()}
        self.n_ops = 0

    def _deps(self, q, reads, writes):
        evs = []
        for r in reads:
            st = self.res.get(r)
            if st and st[0]:
                evs.append(st[0])
        for w in writes:
            st = self.res.get(w)
            if st:
                if st[0]:
                    evs.append(st[0])
                evs.extend(st[1])
        waits = {}
        for (sem, val, srcq) in evs:
            if srcq == q and (q == "pe" or not SAME_ENGINE_SYNC):
                continue
            if self.waited[q].get(sem, 0) >= val:
                continue
            if waits.get(sem, 0) < val:
                waits[sem] = val
        for sem, val in waits.items():
            self.waited[q][sem] = val
        return list(waits.items())

    def _commit(self, ev, reads, writes):
        for r in reads:
            if r in writes:
                continue
            self.res.setdefault(r, [None, []])[1].append(ev)
        for w in writes:
            self.res[w] = [ev, []]

    @staticmethod
    def _keys(views):
        return [v.key for v in views if v is not None and v.key is not None]

    def op(self, q, fn, reads=(), writes=()):
        rk, wk = self._keys(reads), self._keys(writes)
        waits = self._deps(q, rk, wk)
        self.cnt[q] += 1
        ev = ("es_" + q, self.cnt[q], q)
        self.items[q].append((waits, fn, ("es_" + q, 1)))
        self._commit(ev, rk, wk)
        self.n_ops += 1

    def dma(self, q, fn, sem, reads=(), writes=()):
        if sem not in self.dsem:
            h = self.stack.enter_context(self.nc.semaphore("ds_" + sem))
            self.dsem[sem] = [h, 0]
            self.semh["ds_" + sem] = h
        rk, wk = self._keys(reads), self._keys(writes)
        waits = self._deps(q, rk, wk)
        self.dsem[sem][1] += 16
        ev = ("ds_" + sem, self.dsem[sem][1], "dma")
        self.items[q].append((waits, fn, ("ds_" + sem, 16)))
        self._commit(ev, rk, wk)
        self.n_ops += 1

    def emit(self, final=False):
        nc = self.nc
        semh = self.semh
        items = self.items
        bar = [("es_" + q, self.cnt[q]) for q in ("pe", "act", "dve", "pool") if self.cnt[q] > 0]
        bar += [("ds_" + k, v[1]) for k, v in self.dsem.items() if v[1] > 0]

        def replay(q):
            def run(eng):
                for waits, fn, inc in items[q]:
                    for sem, val in waits:
                        eng.wait_ge(semh[sem], val)
                    ins = fn(eng)
                    ins.then_inc(semh[inc[0]], inc[1])
                for sem, val in bar:
                    if self.waited[q].get(sem, 0) < val:
                        eng.wait_ge(semh[sem], val)
                        self.waited[q][sem] = val
            return run

        with nc.Block() as block:
            block.sync(replay("sp"))
            block.tensor(replay("pe"))
            block.scalar(replay("act"))
            block.vector(replay("dve"))
            block.gpsimd(replay("pool"))
        self.items = {q: [] for q in self.QS}


class K:
    def __init__(self, nc, S, stack):
        self.nc, self.S, self.stack = nc, S, stack
        self._n = 0
        self.rr = 0
        self._tiles = {}

    def sb(self, shape, dt, name):
        if name in self._tiles:
            return self._tiles[name]
        h = self.stack.enter_context(self.nc.sbuf_tensor(name, list(shape), dt))
        t = Tile(h, name)
        self._tiles[name] = t
        return t

    def ps(self, name, dt=F32, cols=512):
        h = self.stack.enter_context(self.nc.psum_tensor(name, [128, cols], dt))
        return Tile(h, name)

    def mm(self, out, lhsT, rhs, start, stop):
        self.S.op("pe", lambda e: e.matmul(out.ap, lhsT.ap, rhs.ap, start=start, stop=stop),
                  reads=[lhsT, rhs] + ([] if start else [out]), writes=[out])

    def tr(self, out, in_, ident):
        self.S.op("pe", lambda e: e.transpose(out.ap, in_.ap, ident.ap), reads=[in_, ident], writes=[out])

    def act(self, out, in_, func, bias=None, scale=None, accum=None, q="act"):
        kw = {}
        rd = [in_]
        if bias is not None:
            if isinstance(bias, V):
                kw["bias"] = bias.ap
                rd.append(bias)
            else:
                kw["bias"] = float(bias)
        if scale is not None:
            if isinstance(scale, V):
                kw["scale"] = scale.ap
                rd.append(scale)
            else:
                kw["scale"] = float(scale)
        wr = [out]
        if accum is not None:
            kw["accum_out"] = accum.ap
            wr.append(accum)
        self.S.op("act", lambda e: e.activation(out.ap, in_.ap, func, **kw), reads=rd, writes=wr)

    def copy(self, q, out, in_):
        if q == "act":
            self.S.op("act", lambda e: e.copy(out.ap, in_.ap), reads=[in_], writes=[out])
        else:
            self.S.op(q, lambda e: e.tensor_copy(out.ap, in_.ap), reads=[in_], writes=[out])

    def tt(self, q, out, a, b, op):
        self.S.op(q, lambda e: e.tensor_tensor(out.ap, a.ap, b.ap, op), reads=[a, b], writes=[out])

    def ts(self, q, out, a, s1, op0, s2=None, op1=None, accum=None):
        rd = [a]
        s1v = s1.ap if isinstance(s1, V) else float(s1)
        if isinstance(s1, V):
            rd.append(s1)
        s2v = None
        if s2 is not None:
            s2v = s2.ap if isinstance(s2, V) else float(s2)
            if isinstance(s2, V):
                rd.append(s2)
        wr = [out]
        kw = {}
        if accum is not None:
            kw["accum_out"] = accum.ap
            wr.append(accum)
        o1 = op1 if op1 is not None else ALU.bypass
        self.S.op(q, lambda e: e.tensor_scalar(out.ap, a.ap, s1v, s2v, op0, o1, **kw), reads=rd, writes=wr)

    def stt(self, out, a, s, b, op0, op1):
        rd = [a, b]
        sv = s.ap if isinstance(s, V) else float(s)
        if isinstance(s, V):
            rd.append(s)
        self.S.op("dve", lambda e: e.scalar_tensor_tensor(out.ap, a.ap, sv, b.ap, op0, op1), reads=rd, writes=[out])

    def memset(self, q, out, val):
        self.S.op(q, lambda e: e.memset(out.ap, val), writes=[out])

    def dma(self, out, in_, sem, q="sp", **kw):
        self.S.dma(q, lambda e: e.dma_start(out.ap, in_.ap, **kw), sem, reads=[in_], writes=[out])

    def evac_q(self):
        self.rr += 1
        return "act" if self.rr % 2 else "dve"


def _consts(kb):
    nc, S = kb.nc, kb.S
    ones_f = kb.sb([128, 128], F32, "c_onesf")
    ident = kb.sb([128, 128], F32, "c_ident")
    cmask = kb.sb([128, 128], F32, "c_cmask")
    ones_b = kb.sb([128, 128], BF16, "c_onesb")
    kb.memset("pool", ones_f[:, :], 1.0)
    kb.memset("pool", ones_b[:, :], 1.0)
    S.op("pool", lambda e: e.affine_select(ident.h[:, :], ones_f.h[:, :], [[-1, 128]], ALU.is_equal, 0.0,
                                           base=0, channel_multiplier=1),
         reads=[ones_f[:, :]], writes=[ident[:, :]])
    S.op("pool", lambda e: e.affine_select(cmask.h[:, :], ones_f.h[:, :], [[1, 128]], ALU.is_ge, 0.0,
                                           base=0, channel_multiplier=-1),
         reads=[ones_f[:, :]], writes=[cmask[:, :]])
    return dict(ones_f=ones_f, ident=ident, cmask=cmask, ones_b=ones_b)


def _load_weights_bf16(kb, w_dram, ncols, w_sb, stage, tagsem):
    for k in range(16):
        st = stage[k % 2]
        kb.dma(st[:, :ncols], V(w_dram[k * 128:(k + 1) * 128, :], None), f"{tagsem}{k % 2}")
        q = ("act", "dve", "pool")[k % 3]
        kb.copy(q, w_sb.k(k)[:, k, :ncols], st[:, :ncols])


def _tr_tile(kb, src, dst, Pa, Pb, ident, ncols=128):
    for g in range(4):
        Tb = Pa if g % 2 == 0 else Pb
        for j in range(4):
            kk = 4 * g + j
            kb.tr(Tb[:, j * 128:(j + 1) * 128], src[:, kk * 128:(kk + 1) * 128], ident[:, :])
        kb.copy(kb.evac_q(), dst[:, g * 512:(g + 1) * 512], Tb[:, :])


def _ln_tile(kb, y, g_rep, b_rep, out, stats, mv, r):
    S = kb.S
    for c in range(4):
        S.op("dve", lambda e, c=c: e.bn_stats(stats.h[:, c * 6:(c + 1) * 6], y.h[:, c * 512:(c + 1) * 512]),
             reads=[y[:, :]], writes=[stats[:, :]])
    S.op("dve", lambda e: e.bn_aggr(mv.h[:, 0:2], stats.h[:, 0:24]), reads=[stats[:, :]], writes=[mv[:, :]])
    kb.ts("dve", r[:, :], mv[:, 1:2], EPS, ALU.add)
    kb.act(r[:, :], r[:, :], AF.Sqrt)
    S.op("dve", lambda e: e.reciprocal(r.h[:, :], r.h[:, :]), reads=[r[:, :]], writes=[r[:, :]])
    kb.ts("dve", out[:, :], y[:, :], mv[:, 0:1], ALU.subtract, r[:, 0:1], ALU.mult)
    kb.tt("pool", out[:, :], out[:, :], g_rep[:, :], ALU.mult)
    kb.tt("pool", out[:, :], out[:, :], b_rep[:, :], ALU.add)


def _load_w(kb, w_ap, ncols, w_sb, stage, tagsem, col0=0):
    for k in range(16):
        st = stage[k % 2]
        kb.dma(st[:, :ncols], V(w_ap[k * 128:(k + 1) * 128, col0:col0 + ncols], None), f"{tagsem}{k % 2}")
        q = ("act", "dve", "pool")[k % 3]
        kb.copy(q, w_sb.k(k)[:, k, :ncols], st[:, :ncols])


NTT = 64
NTOK = NTT * 128
CAP = 1280
NJ = CAP // 128
RGS = ((0, 512), (512, 512), (1024, 256))
YROWS = 1 + 4 * NTOK + 128


def phases_bcd(nc, S, kb, C, P, io, dbg):
    ident, ones_b, ones_f = C["ident"], C["ones_b"], C["ones_f"]
    T0, T1, F0, F1, M0, M1, M2, PO = P
    mg, x_tok, out_ap = io["merged"], io["x_tok"], io["out"]
    h1s, h2s, ybufs = io["h1s"], io["h2s"], io["ybuf"]
    n_exp = io.get("n_exp", 32)

    kb.stack = S.stack
    idx_tok = kb.sb([128, 32 * NJ], I32, "R_idxtok")
    dest = kb.sb([128, 32 * NJ], I32, "R_dest")
    gl = kb.sb([128, 32 * NJ], F32, "R_gl")

    pB = contextlib.ExitStack()
    kb.stack = pB
    wsb = kb.sb([128, 16, 2048], BF16, "B_w")
    stage = [kb.sb([128, 2048], F32, f"B_st{i}") for i in range(2)]
    g_rep = kb.sb([128, 2048], F32, "B_g")
    b_rep = kb.sb([128, 2048], F32, "B_b")
    mt = [kb.sb([128, 2048], F32, f"B_mt{i}") for i in range(2)]
    xk = [kb.sb([128, 2048], F32, f"B_xk{i}") for i in range(2)]
    mT = [kb.sb([128, 2048], BF16, f"B_mT{i}") for i in range(2)]
    yt = [kb.sb([128, 2048], F32, f"B_y{i}") for i in range(2)]
    stats = kb.sb([128, 24], F32, "B_stats")
    mv = kb.sb([128, 2], F32, "B_mv")
    rr = kb.sb([128, 1], F32, "B_r")
    kb.dma(g_rep[:, :], V(io["ln1g"], None), "c0")
    kb.dma(b_rep[:, :], V(io["ln1b"], None), "c1")
    _load_w(kb, io["w_o"], 2048, wsb, stage, "ws")
    kb.dma(mt[0][:, :], V(mg[0:128, :], "merged"), "mt0")
    kb.dma(xk[0][:, :], V(x_tok[0:128, :], None), "xk0")
    for i in range(NTT):
        sl = i % 2
        if i + 1 < NTT:
            kb.dma(mt[1 - sl][:, :], V(mg[(i + 1) * 128:(i + 2) * 128, :], "merged"), f"mt{1 - sl}")
            kb.dma(xk[1 - sl][:, :], V(x_tok[(i + 1) * 128:(i + 2) * 128, :], None), f"xk{1 - sl}")
        _tr_tile(kb, mt[sl], mT[sl], T0, T1, ident)
        for n, Pb in enumerate((F0, F1, M0, M1)):
            for kk in range(16):
                kb.mm(Pb[:, :], mT[sl][:, kk * 128:(kk + 1) * 128], wsb.k(kk)[:, kk, n * 512:(n + 1) * 512],
                      start=(kk == 0), stop=(kk == 15))
            kb.stt(yt[sl][:, n * 512:(n + 1) * 512], xk[sl][:, n * 512:(n + 1) * 512], ALPHA, Pb[:, :],
                   ALU.mult, ALU.add)
        _ln_tile(kb, yt[sl], g_rep, b_rep, yt[sl], stats, mv, rr)
        kb.dma(V(h1s[i * 128:(i + 1) * 128, :], "h1s"), yt[sl][:, :], f"ho{sl}")
    S.emit()
    pB.close()

    pR = contextlib.ExitStack()
    kb.stack = pR
    gate_all = kb.sb([128, NTT, 32], F32, "R_gate")

    pB = contextlib.ExitStack()
    kb.stack = pB
    wq = kb.sb([128, 16, 2048], BF16, "C_wq")
    wo = kb.sb([128, 16, 2048], BF16, "C_wo")
    g_rep = kb.sb([128, 2048], F32, "C_g")
    b_rep = kb.sb([128, 2048], F32, "C_b")
    big = kb.sb([128, 4096], BF16, "C_big")
    memT = Tile(big.h[:, :].rearrange("p (k m) -> p k m", k=16), "C_memT")
    KT = kb.sb([128, 16, 256], BF16, "C_KT")
    Vs = kb.sb([128, 2, 2048], BF16, "C_V")
    ht0 = kb.sb([128, 2048], F32, "C_ht0")
    ht = [ht0, ht0]
    stage = ht
    hT = kb.sb([128, 2048], BF16, "C_hT")
    qT = Tile(big.h[:, 0:2048], "C_qT")
    pT = Tile(big.h[:, 2048:3072], "C_pT")
    oT = hT
    yt = kb.sb([128, 2048], F32, "C_y")
    memt = yt
    pf = Tile(yt.h[:, 0:1024], "C_y")
    wr = kb.sb([128, 16, 32], F32, "C_wr")
    br = kb.sb([128, 32], F32, "C_br")
    stats = kb.sb([128, 24], F32, "C_stats")
    mv = kb.sb([128, 2], F32, "C_mv")
    rr = kb.sb([128, 1], F32, "C_r")
    mx = kb.sb([128, 4], F32, "C_mx")
    sm = kb.sb([128, 4], F32, "C_sm")
    m8 = kb.sb([128, 8], F32, "C_m8")
    ex = kb.sb([128, 32], F32, "C_ex")
    s1 = kb.sb([128, 2], F32, "C_s1")
    mk = kb.sb([128, 32], F32, "C_mk")
    kb.dma(g_rep[:, :], V(io["ln2g"], None), "c0")
    kb.dma(b_rep[:, :], V(io["ln2b"], None), "c1")
    kb.dma(wr[:, :, :], V(io["w_router"].rearrange("(k p) e -> p k e", p=128), None), "c2")
    kb.dma(br[:, :], V(io["br_rep"], None), "c3")
    for mtile in range(2):
        kb.dma(memt[:, :], V(io["mem_b"][mtile * 128:(mtile + 1) * 128, :], None), "mm")
        for g in range(4):
            Tb = T0 if g % 2 == 0 else T1
            for j in range(4):
                kk = 4 * g + j
                kb.tr(Tb[:, j * 128:(j + 1) * 128], memt[:, kk * 128:(kk + 1) * 128], ident[:, :])
            S.op("dve", lambda e, Tb=Tb, g=g, mtile=mtile: e.tensor_copy(
                memT.h[:, 4 * g:4 * g + 4, mtile * 128:(mtile + 1) * 128],
                Tb.h[:, :].rearrange("p (a b) -> p a b", a=4)), reads=[Tb[:, :]], writes=[memT[:, :, :]])
    _load_w(kb, io["w_kv"], 2048, wq, stage, "ws", col0=0)
    banks = (F0, F1, M0, M1)
    for c in range(16):
        Pb = banks[(c // 2) % 4]
        cs = slice((c % 2) * 256, (c % 2) * 256 + 256)
        for kk in range(16):
            kb.mm(Pb[:, cs], wq.k(kk)[:, kk, c * 128:(c + 1) * 128], memT[:, kk, :], start=(kk == 0), stop=(kk == 15))
        if c % 2 == 1:
            S.op("act", lambda e, Pb=Pb, c=c: e.copy(KT.h[:, c - 1:c + 1, :], Pb.h[:, :].rearrange("p (a b) -> p a b", a=2)),
                 reads=[Pb[:, :]], writes=[KT[:, :, :]])
    _load_w(kb, io["w_kv"], 2048, wq, stage, "ws", col0=2048)
    for mtile in range(2):
        for n in range(4):
            Pb = banks[n]
            for kk in range(16):
                kb.mm(Pb[:, :], memT[:, kk, mtile * 128:(mtile + 1) * 128], wq.k(kk)[:, kk, n * 512:(n + 1) * 512],
                      start=(kk == 0), stop=(kk == 15))
            kb.copy(kb.evac_q(), Vs[:, mtile, n * 512:(n + 1) * 512], Pb[:, :])
    _load_w(kb, io["w_xq"], 2048, wq, stage, "ws")
    _load_w(kb, io["w_xo"], 2048, wo, stage, "ws")
    SC = 512.0 ** -0.5
    for i in range(NTT):
        sl = i % 2
        kb.dma(ht[sl][:, :], V(h1s[i * 128:(i + 1) * 128, :], "h1s"), "ht")
        _tr_tile(kb, ht[sl], hT, T0, T1, ident)
        for c in range(16):
            Pb = banks[c // 4]
            cs = slice((c % 4) * 128, (c % 4) * 128 + 128)
            for kk in range(16):
                kb.mm(Pb[:, cs], wq.k(kk)[:, kk, c * 128:(c + 1) * 128], hT[:, kk * 128:(kk + 1) * 128],
                      start=(kk == 0), stop=(kk == 15))
            if c % 4 == 3:
                g = c // 4
                kb.act(qT[:, g * 512:(g + 1) * 512], Pb[:, :], AF.Identity, scale=SC)
        for hx in range(4):
            Pb = M2 if hx < 2 else PO
            cs = slice((hx % 2) * 256, (hx % 2) * 256 + 256)
            for cc in range(4):
                c = hx * 4 + cc
                kb.mm(Pb[:, cs], qT[:, c * 128:(c + 1) * 128], KT[:, c, :], start=(cc == 0), stop=(cc == 3))
        for hx in range(4):
            Pb = M2 if hx < 2 else PO
            cs = slice((hx % 2) * 256, (hx % 2) * 256 + 256)
            S.op("dve", lambda e, Pb=Pb, cs=cs, hx=hx: e.reduce_max(mx.h[:, hx:hx + 1], Pb.h[:, cs], AX.X),
                 reads=[Pb[:, :]], writes=[mx[:, :]])
        kb.ts("dve", mx[:, :], mx[:, :], -1.0, ALU.mult)
        for hx in range(4):
            Pb = M2 if hx < 2 else PO
            cs = slice((hx % 2) * 256, (hx % 2) * 256 + 256)
            kb.act(pf[:, hx * 256:(hx + 1) * 256], Pb[:, cs], AF.Exp, bias=mx[:, hx:hx + 1], accum=sm[:, hx:hx + 1])
        S.op("dve", lambda e: e.reciprocal(sm.h[:, :], sm.h[:, :]), reads=[sm[:, :]], writes=[sm[:, :]])
        for hx in range(4):
            kb.ts("dve", pf[:, hx * 256:(hx + 1) * 256], pf[:, hx * 256:(hx + 1) * 256], sm[:, hx:hx + 1], ALU.mult)
        for g in range(2):
            Tb = T0 if g == 0 else T1
            for j in range(4):
                kk = 4 * g + j
                kb.tr(Tb[:, j * 128:(j + 1) * 128], pf[:, kk * 128:(kk + 1) * 128], ident[:, :])
            kb.copy(kb.evac_q(), pT[:, g * 512:(g + 1) * 512], Tb[:, :])
        for c in range(16):
            Pb = banks[c // 4]
            cs = slice((c % 4) * 128, (c % 4) * 128 + 128)
            hx = c // 4
            for mc in range(2):
                kb.mm(Pb[:, cs], Vs[:, mc, c * 128:(c + 1) * 128], pT[:, (hx * 2 + mc) * 128:(hx * 2 + mc + 1) * 128],
                      start=(mc == 0), stop=(mc == 1))
            if c % 4 == 3:
                g = c // 4
                kb.copy(kb.evac_q(), oT[:, g * 512:(g + 1) * 512], Pb[:, :])
        for n in range(4):
            Pb = banks[n]
            for kk in range(16):
                kb.mm(Pb[:, :], oT[:, kk * 128:(kk + 1) * 128], wo.k(kk)[:, kk, n * 512:(n + 1) * 512],
                      start=(kk == 0), stop=(kk == 15))
            kb.stt(yt[:, n * 512:(n + 1) * 512], ht[sl][:, n * 512:(n + 1) * 512], ALPHA, Pb[:, :], ALU.mult, ALU.add)
        _ln_tile(kb, yt, g_rep, b_rep, yt, stats, mv, rr)
        kb.dma(V(h2s[i * 128:(i + 1) * 128, :], "h2s"), yt[:, :], "ho")
        h2T = ht[sl]
        for g in range(4):
            Tb = T0 if g % 2 == 0 else T1
            for j in range(4):
                kk = 4 * g + j
                kb.tr(Tb[:, j * 128:(j + 1) * 128], yt[:, kk * 128:(kk + 1) * 128], ident[:, :])
            kb.copy(kb.evac_q(), h2T[:, g * 512:(g + 1) * 512], Tb[:, :])
        for kk in range(16):
            kb.mm(M2[:, 0:32], h2T[:, kk * 128:(kk + 1) * 128], wr[:, kk, :], start=(kk == 0), stop=(kk == 15))
        lg = ex
        kb.tt("dve", lg[:, :], M2[:, 0:32], br[:, :], ALU.add)
        S.op("dve", lambda e: e.max(m8.h[:, :], lg.h[:, :]), reads=[lg[:, :]], writes=[m8[:, :]])
        kb.ts("dve", mk[:, :], lg[:, :], m8[:, 3:4], ALU.is_ge)
        kb.ts("dve", s1[:, 0:1], m8[:, 0:1], -1.0, ALU.mult)
        kb.act(lg[:, :], lg[:, :], AF.Exp, bias=s1[:, 0:1])
        kb.tt("dve", lg[:, :], lg[:, :], mk[:, :], ALU.mult)
        S.op("dve", lambda e: e.reduce_sum(s1.h[:, 1:2], lg.h[:, :], AX.X), reads=[lg[:, :]], writes=[s1[:, :]])
        S.op("dve", lambda e: e.reciprocal(s1.h[:, 1:2], s1.h[:, 1:2]), reads=[s1[:, :]], writes=[s1[:, :]])
        kb.ts("dve", gate_all[:, i, :], lg[:, :], s1[:, 1:2], ALU.mult)
    S.emit()
    pB.close()

    pB = contextlib.ExitStack()
    kb.stack = pB
    mask_bf = kb.sb([128, NTT, 32], BF16, "D_maskbf")
    SU = kb.sb([128, 128], BF16, "D_SU")
    pos = kb.sb([128, NTT, 32], F32, "D_pos")
    slot = kb.sb([128, NTT, 32], F32, "D_slot")
    toki = kb.sb([128, NTT], I32, "D_toki")
    tokf = kb.sb([128, NTT], F32, "D_tokf")
    tokp1 = kb.sb([128, NTT], F32, "D_tokp1")
    pay = kb.sb([128, NTT, 32, 3], F32, "D_pay")
    io_i = kb.sb([128, CAP], I32, "D_ioi")
    io_f = kb.sb([128, CAP], F32, "D_iof")
    tr_i = kb.sb([128, 1], I32, "D_tri")
    tr_f = kb.sb([128, 1], F32, "D_trf")
    zer = kb.sb([128, 128], F32, "D_zer")
    Sel = [kb.sb([128, CAP], F32, f"D_sel{i}") for i in range(3)]
    lst = kb.sb([128, 32, NJ * 3], F32, "D_lst")
    tmpd = kb.sb([128, 32 * NJ], F32, "D_tmpd")
    mask_all = kb.sb([128, NTT, 32], F32, "D_mask")
    kb.memset("pool", zer[:, :], 0.0)
    kb.ts("dve", mask_all[:, :, :], gate_all[:, :, :], 0.0, ALU.is_gt)
    kb.copy("dve", mask_bf[:, :, :], mask_all[:, :, :])
    S.op("pool", lambda e: e.affine_select(SU.h[:, :], ones_f.h[:, :], [[1, 128]], ALU.is_ge, 0.0,
                                           base=-1, channel_multiplier=-1), reads=[ones_f[:, :]], writes=[SU[:, :]])
    S.op("pool", lambda e: e.iota(toki.h[:, :], [[128, NTT]], base=0, channel_multiplier=1), writes=[toki[:, :]])
    S.op("pool", lambda e: e.iota(io_i.h[:, :], [[1, CAP]], base=0, channel_multiplier=0), writes=[io_i[:, :]])
    S.op("pool", lambda e: e.iota(tr_i.h[:, :], [[0, 1]], base=1 + 4 * NTOK, channel_multiplier=1), writes=[tr_i[:, :]])
    kb.copy("dve", tokf[:, :], toki[:, :])
    kb.copy("dve", io_f[:, :], io_i[:, :])
    kb.copy("dve", tr_f[:, :], tr_i[:, :])
    kb.ts("dve", tokp1[:, :], tokf[:, :], 1.0, ALU.add)
    pbanks = (F0, F1, M0, M1)
    for i in range(NTT):
        Pb = pbanks[i // 16]
        cs = slice((i % 16) * 32, (i % 16) * 32 + 32)
        for ip in range(i):
            kb.mm(Pb[:, cs], ones_b[:, :], mask_bf[:, ip, :], start=(ip == 0), stop=False)
        kb.mm(Pb[:, cs], SU[:, :], mask_bf[:, i, :], start=(i == 0), stop=True)
    for q_ in range(NTT // 16):
        S.op("dve", lambda e, q_=q_: e.tensor_copy(pos.h[:, q_ * 16:(q_ + 1) * 16, :],
                                                  pbanks[q_].h[:, :].rearrange("p (a b) -> p a b", a=16)),
             reads=[pbanks[q_][:, :]], writes=[pos[:, :, :]])
    for i in range(NTT):
        S.op("dve", lambda e, i=i: e.tensor_tensor_scan(slot.h[:, i, :], ones_f.h[:, 0:32], mask_all.h[:, i, :], 0.0,
                                                         ALU.mult, ALU.add),
             reads=[ones_f[:, :], mask_all[:, :, :]], writes=[slot[:, :, :]])
    kb.tt("dve", slot[:, :, :], slot[:, :, :], mask_all[:, :, :], ALU.subtract)
    for i in range(NTT):
        kb.ts("dve", pay[:, i, :, 0], mask_all[:, i, :], 0.0, ALU.mult, tokf[:, i:i + 1], ALU.add)
        kb.ts("dve", pay[:, i, :, 2], slot[:, i, :], float(NTOK), ALU.mult, tokp1[:, i:i + 1], ALU.add)
    kb.copy("dve", pay[:, :, :, 1], gate_all[:, :, :])
    n = 0
    for e_ in range(32):
        Pb = (M2, PO)[e_ % 2]
        kb.mm(Pb[:, 0:NJ * 3], zer[:, :], zer[:, 0:NJ * 3], start=True, stop=False)
        for i in range(NTT):
            sel = Sel[n % 3]
            n += 1
            kb.ts("dve", sel[:, :], io_f[:, :], pos[:, i, e_:e_ + 1], ALU.is_equal, mask_all[:, i, e_:e_ + 1], ALU.mult)
            for j in range(NJ):
                kb.mm(Pb[:, 3 * j:3 * j + 3], sel[:, j * 128:(j + 1) * 128], pay[:, i, e_, :],
                      start=False, stop=(i == NTT - 1 and j == NJ - 1))
        kb.copy("act", lst[:, e_, :], Pb[:, 0:NJ * 3])
    l4 = lst.h[:, :, :].rearrange("p e (j c) -> p e j c", c=3)
    iv = idx_tok.h[:, :].rearrange("p (e j) -> p e j", e=32)
    gv = gl.h[:, :].rearrange("p (e j) -> p e j", e=32)
    tv = tmpd.h[:, :].rearrange("p (e j) -> p e j", e=32)
    S.op("dve", lambda e: e.tensor_copy(iv, l4[:, :, :, 0]), reads=[lst[:, :, :]], writes=[idx_tok[:, :]])
    S.op("dve", lambda e: e.tensor_copy(gv, l4[:, :, :, 1]), reads=[lst[:, :, :]], writes=[gl[:, :]])
    S.op("dve", lambda e: e.tensor_scalar(tv, l4[:, :, :, 2], 0.0, tr_f.h[:, 0:1], ALU.is_equal, ALU.mult),
         reads=[lst[:, :, :], tr_f[:, :]], writes=[tmpd[:, :]])
    S.op("dve", lambda e: e.tensor_tensor(tv, tv, l4[:, :, :, 2], ALU.add),
         reads=[lst[:, :, :], tmpd[:, :]], writes=[tmpd[:, :]])
    kb.copy("dve", dest[:, :], tmpd[:, :])
    S.emit()
    pB.close()
    pR.close()

    pB = contextlib.ExitStack()
    kb.stack = pB
    stg = [kb.sb([128, 16, 512], F32, f"E_stg{i}") for i in range(2)]
    wb = [kb.sb([128, 16, 512], BF16, f"E_wb{i}") for i in range(2)]
    XgT = kb.sb([128, 16, CAP], BF16, "E_XgT")
    xg0 = kb.sb([128, 2048], F32, "E_xg0")
    xg = [xg0, xg0]
    actT = kb.sb([128, 16, CAP], BF16, "E_actT")
    Yp = [kb.sb([128, 512], F32, f"E_Y{j}") for j in range(2)]
    bgu = kb.sb([128, 1024], F32, "E_bgu")
    bdr = kb.sb([128, 512], F32, "E_bdr")
    bdb = [kb.sb([128, 512], BF16, f"E_bdb{i}") for i in range(2)]
    glu = kb.sb([128, 512], F32, "E_glu")
    lin = kb.sb([128, 512], F32, "E_lin")
    sg = kb.sb([128, 512], F32, "E_sg")
    kb.dma(bgu[:, :], V(io["bgu_col"], None), "c0")
    kb.memset("pool", bdr[:, :], 0.0)
    for i in range(2):
        kb.memset("pool", bdb[i][:, :], 0.0)
    w_gu, w_dn, b_dn = io["w_gu"], io["w_dn"], io["b_dn"]
    nblk = 0
    ny = 0
    gbanks = ((F0, F1), (M0, M1), (M2, PO))
    dbanks = (F0, F1, M0, M1, M2, PO)
    for e_ in range(n_exp):
        for j in range(NJ):
            col = e_ * NJ + j
            xs = 0
            S.dma("pool", lambda e, xs=xs, col=col: e.indirect_dma_start(
                xg[xs].h[:, :], None, h2s[:, :], bass.IndirectOffsetOnAxis(idx_tok.h[:, col:col + 1], 0)),
                f"xg{xs}", reads=[idx_tok[:, :], V(None, "h2s")], writes=[xg[xs][:, :]])
            for g in range(4):
                Tb = T0 if g % 2 == 0 else T1
                for jj in range(4):
                    kk = 4 * g + jj
                    kb.tr(Tb[:, jj * 128:(jj + 1) * 128], xg[xs][:, kk * 128:(kk + 1) * 128], ident[:, :])
                q = kb.evac_q()
                fn = (lambda e, Tb=Tb, g=g, j=j: e.copy(
                    XgT.h[:, 4 * g:4 * g + 4, j * 128:(j + 1) * 128], Tb.h[:, :].rearrange("p (a b) -> p a b", a=4))) \
                    if q == "act" else (lambda e, Tb=Tb, g=g, j=j: e.tensor_copy(
                        XgT.h[:, 4 * g:4 * g + 4, j * 128:(j + 1) * 128], Tb.h[:, :].rearrange("p (a b) -> p a b", a=4)))
                S.op(q, fn, reads=[Tb[:, :]], writes=[XgT[:, :, :]])
        for cb in range(8):
            bs = nblk % 2
            nblk += 1
            kb.dma(stg[bs][:, :, :], V(w_gu[e_ * D:(e_ + 1) * D, cb * 512:(cb + 1) * 512].rearrange("(k p) c -> p k c", p=128), None),
                   f"stg{bs}")
            sv = stg[bs].h[:, :, :].rearrange("p k (c two) -> p k c two", two=2)
            S.op("act", lambda e, bs=bs, sv=sv: e.copy(wb[bs].h[:, :, 0:256], sv[:, :, :, 0]),
                 reads=[stg[bs][:, :, :]], writes=[wb[bs][:, :, :]])
            S.op("pool", lambda e, bs=bs, sv=sv: e.tensor_copy(wb[bs].h[:, :, 256:512], sv[:, :, :, 1]),
                 reads=[stg[bs][:, :, :]], writes=[wb[bs][:, :, :]])
            for m in range(2):
                mc = cb * 2 + m
                bg = bgu[:, e_ * 32 + mc:e_ * 32 + mc + 1]
                bl = bgu[:, e_ * 32 + 16 + mc:e_ * 32 + 16 + mc + 1]
                for gi, (r0, rn) in enumerate(RGS):
                    Pg, Pl = gbanks[gi]
                    for kk in range(16):
                        kb.mm(Pg[:, 0:rn], wb[bs][:, kk, m * 128:(m + 1) * 128], XgT[:, kk, r0:r0 + rn],
                              start=(kk == 0), stop=(kk == 15))
                    for kk in range(16):
                        kb.mm(Pl[:, 0:rn], wb[bs][:, kk, 256 + m * 128:256 + (m + 1) * 128], XgT[:, kk, r0:r0 + rn],
                              start=(kk == 0), stop=(kk == 15))
                    kb.ts("dve", glu[:, 0:rn], Pg[:, 0:rn], bg, ALU.add, 7.0, ALU.min)
                    kb.ts("dve", lin[:, 0:rn], Pl[:, 0:rn], bl, ALU.add, 7.0, ALU.min)
                    kb.ts("dve", lin[:, 0:rn], lin[:, 0:rn], -7.0, ALU.max, 1.0, ALU.add)
                    kb.act(sg[:, 0:rn], glu[:, 0:rn], AF.Sigmoid, scale=1.702)
                    kb.tt("dve", glu[:, 0:rn], glu[:, 0:rn], sg[:, 0:rn], ALU.mult)
                    kb.tt("dve", actT[:, mc, r0:r0 + rn], glu[:, 0:rn], lin[:, 0:rn], ALU.mult)
        for nb in range(4):
            bs = nblk % 2
            nblk += 1
            kb.dma(stg[bs][:, :, :], V(w_dn[e_ * D:(e_ + 1) * D, nb * 512:(nb + 1) * 512].rearrange("(k p) c -> p k c", p=128), None),
                   f"stg{bs}")
            S.op("act", lambda e, bs=bs: e.copy(wb[bs].h[:, 0:8, :], stg[bs].h[:, 0:8, :]),
                 reads=[stg[bs][:, :, :]], writes=[wb[bs][:, :, :]])
            S.op("pool", lambda e, bs=bs: e.tensor_copy(wb[bs].h[:, 8:16, :], stg[bs].h[:, 8:16, :]),
                 reads=[stg[bs][:, :, :]], writes=[wb[bs][:, :, :]])
            ds_ = nb % 2
            kb.dma(bdr[0:1, :], V(b_dn[e_:e_ + 1, nb * 512:(nb + 1) * 512], None), "bd")
            kb.copy("pool", bdb[ds_][0:1, :], bdr[0:1, :])
            yb = ybufs[nb]
            for j in range(NJ):
                Pb = dbanks[j % 6]
                col = e_ * NJ + j
                for kk in range(16):
                    kb.mm(Pb[:, :], actT[:, kk, j * 128:(j + 1) * 128], wb[bs][:, kk, :], start=(kk == 0), stop=False)
                kb.mm(Pb[:, :], ones_b[:, :], bdb[ds_][:, :], start=False, stop=True)
                ys_ = ny % 2
                ny += 1
                kb.ts("dve", Yp[ys_][:, :], Pb[:, :], gl[:, col:col + 1], ALU.mult)
                S.dma("pool", lambda e, ys_=ys_, col=col, yb=yb: e.indirect_dma_start(
                    yb[:, :], bass.IndirectOffsetOnAxis(dest.h[:, col:col + 1], 0), Yp[ys_].h[:, :], None),
                    f"ys{ys_}", reads=[dest[:, :], Yp[ys_][:, :]], writes=[V(None, "ybuf")])
    S.emit()
    pB.close()

    pB = contextlib.ExitStack()
    kb.stack = pB
    g_rep = kb.sb([128, 2048], F32, "F_g")
    b_rep = kb.sb([128, 2048], F32, "F_b")
    ys = [[kb.sb([128, 2048], F32, f"F_ys{i}_{s_}") for s_ in range(4)] for i in range(2)]
    hh2 = [kb.sb([128, 2048], F32, f"F_h{i}") for i in range(2)]
    yt = [kb.sb([128, 2048], F32, f"F_y{i}") for i in range(2)]
    stats = kb.sb([128, 24], F32, "F_stats")
    mv = kb.sb([128, 2], F32, "F_mv")
    rr = kb.sb([128, 1], F32, "F_r")
    kb.dma(g_rep[:, :], V(io["ln3g"], None), "c0")
    kb.dma(b_rep[:, :], V(io["ln3b"], None), "c1")
    for i in range(NTT):
        sl = i % 2
        kb.dma(hh2[sl][:, :], V(h2s[i * 128:(i + 1) * 128, :], "h2s"), f"fh{sl}")
        for s_ in range(4):
            r0 = 1 + s_ * NTOK + i * 128
            for nb in range(4):
                kb.dma(ys[sl][s_][:, nb * 512:(nb + 1) * 512], V(ybufs[nb][r0:r0 + 128, :], "ybuf"), f"fy{sl}_{s_}")
        kb.tt("dve", yt[sl][:, :], ys[sl][0][:, :], ys[sl][1][:, :], ALU.add)
        kb.tt("pool", ys[sl][2][:, :], ys[sl][2][:, :], ys[sl][3][:, :], ALU.add)
        kb.tt("dve", yt[sl][:, :], yt[sl][:, :], ys[sl][2][:, :], ALU.add)
        kb.stt(yt[sl][:, :], hh2[sl][:, :], ALPHA, yt[sl][:, :], ALU.mult, ALU.add)
        _ln_tile(kb, yt[sl], g_rep, b_rep, yt[sl], stats, mv, rr)
        kb.dma(V(out_ap[i * 128:(i + 1) * 128, :], None), yt[sl][:, :], f"fo{sl}")
    S.emit()
    pB.close()


def build_program(mode="full", dbg_tiles=None, n_exp=32, ng=4):
    nc = bass.Bass("TRN2", target_bir_lowering=False)
    stack = contextlib.ExitStack()
    nt = dbg_tiles or NT_SEQ
    dbg = (mode == "A")
    doA = mode in ("full", "A")
    doB = mode in ("full", "BCD")
    NG = ng

    def din(name, shape, dt=F32):
        return nc.dram_tensor(name, list(shape), dt, kind="ExternalInput").ap()

    x_b = din("x_b", [SEQ, D])
    if doA:
        w_h_all = din("w_h", [NG * D, 2560])
        bcol_h_all = din("bcol_h", [NG * 128, 8])
        brow_h_all = din("brow_h", [NG, 1536])
        lbl_all = din("lbl", [NG * 128, 8])
        hng_rep = din("hng_rep", [128, 512])
        w_g_all = din("w_g", [NG * D, 2064])
        bcol_g_all = din("bcol_g", [NG * 128, 8])
        brow_g_all = din("brow_g", [NG, 1536])
        wa2_all = din("wa2", [NG * 16, 256])
        gng_rep = din("gng_rep", [128, 512])
        if dbg:
            gh_out = nc.dram_tensor("gh_out", [SEQ, 512], F32, kind="ExternalOutput").ap()
            mg_all = nc.dram_tensor("mg_out", [SEQ, NG * 512], F32, kind="ExternalOutput").ap()
        else:
            gh_out = nc.dram_tensor("gh_scr", [SEQ, 512], F32).ap()
            mg_all = nc.dram_tensor("merged", [SEQ, D], F32).ap()
    io = {}
    if doB:
        if mode == "BCD":
            io["merged"] = din("merged", [SEQ, D])
        else:
            io["merged"] = mg_all
        io["x_tok"] = x_b
        for nm in ("ln1g", "ln1b", "ln2g", "ln2b", "ln3g", "ln3b"):
            io[nm] = din(nm, [128, D])
        io["w_o"] = din("w_o", [D, D])
        io["w_kv"] = din("w_kv", [D, 2 * D])
        io["w_xq"] = din("w_xq", [D, D])
        io["w_xo"] = din("w_xo", [D, D])
        io["mem_b"] = din("mem_b", [256, D])
        io["w_router"] = din("w_router", [D, 32])
        io["br_rep"] = din("br_rep", [128, 32])
        io["bgu_col"] = din("bgu_col", [128, 1024])
        io["w_gu"] = din("w_gu", [32 * D, 2 * D])
        io["w_dn"] = din("w_dn", [32 * D, D])
        io["b_dn"] = din("b_dn", [32, D])
        io["out"] = nc.dram_tensor("out", [NTOK, D], F32, kind="ExternalOutput").ap()
        if mode == "BCD":
            io["h1s"] = nc.dram_tensor("h1s", [NTOK, D], F32, kind="ExternalOutput").ap()
            io["h2s"] = nc.dram_tensor("h2s", [NTOK, D], F32, kind="ExternalOutput").ap()
        else:
            io["h1s"] = nc.dram_tensor("h1s", [NTOK, D], F32).ap()
            io["h2s"] = nc.dram_tensor("h2s", [NTOK, D], F32).ap()
        io["ybuf"] = [nc.dram_tensor(f"ybuf{nb}", [YROWS, 512], F32).ap() for nb in range(4)]
        io["n_exp"] = n_exp

    with stack:
        S = Sched(nc, stack)
        kb = K(nc, S, stack)
        C = _consts(kb)
        ident, cmask, ones_b = C["ident"], C["cmask"], C["ones_b"]

        T0, T1 = kb.ps("pT0"), kb.ps("pT1")
        F0, F1 = kb.ps("pF0"), kb.ps("pF1")
        M0, M1, M2 = kb.ps("pM0"), kb.ps("pM1"), kb.ps("pM2")
        PO = kb.ps("pO")

        if doA:
            pA = contextlib.ExitStack()
            kb.stack = pA
            for hh in range(NG):
                w_h = w_h_all[hh * D:(hh + 1) * D, :]
                bcol_h = bcol_h_all[hh * 128:(hh + 1) * 128, :]
                brow_h = brow_h_all[hh:hh + 1, :]
                lbl = lbl_all[hh * 128:(hh + 1) * 128, :]
                w_g = w_g_all[hh * D:(hh + 1) * D, :]
                bcol_g = bcol_g_all[hh * 128:(hh + 1) * 128, :]
                brow_g = brow_g_all[hh:hh + 1, :]
                wa2_in = wa2_all[hh * 16:(hh + 1) * 16, :]
                mg_out = mg_all[:, hh * 512:(hh + 1) * 512]
                w_sb = kb.sb([128, 16, 2560], BF16, "w_sb")
                stage = [kb.sb([128, 2560], F32, f"wstage{i}") for i in range(2)]
                xt = [kb.sb([128, D], F32, f"xt{i}") for i in range(2)]
                xT = [kb.sb([128, 16 * 128], BF16, f"xT{i}") for i in range(2)]
                bcol = kb.sb([128, 8], F32, "bcol")
                brow = kb.sb([128, 1536], F32, "brow")
                brow_b = kb.sb([128, 1536], BF16, "brow_b")
                lb_in = kb.sb([128, 8], F32, "lb_in")
                lbv = kb.sb([128, 4], F32, "lbv")
                omlb = kb.sb([128, 4], F32, "omlb")
                hng = kb.sb([128, 512], F32, "hng")

                kb.dma(bcol[:, :], V(bcol_h, None), "c0")
                kb.memset("dve", brow[:, :], 0.0)
                kb.dma(brow[0:1, :], V(brow_h, None), "c1")
                kb.dma(lb_in[:, :], V(lbl, None), "c2")
                kb.dma(hng[:, :], V(hng_rep, None), "c3")
                kb.copy("dve", brow_b[:, :], brow[:, :])
                kb.tt("dve", lbv[:, :], lb_in[:, 0:4], lb_in[:, 4:8], ALU.subtract)
                kb.act(lbv[:, :], lbv[:, :], AF.Sigmoid)
                kb.ts("dve", omlb[:, :], lbv[:, :], -1.0, ALU.mult, 1.0, ALU.add)

                _load_weights_bf16(kb, w_h, 2560, w_sb, stage, "ws")

                def wt(name, shape, dt=F32):
                    return kb.sb(shape, dt, name)
                qT = wt("qT", [128, 512])
                fT = wt("fT", [128, 512])
                kTt = wt("kTt", [128, 512])
                cum = wt("cum", [128, 512])
                e1 = wt("e1", [128, 512])
                e2 = wt("e2", [128, 512])
                mid = wt("mid", [128, 8])
                sc = wt("sc", [128, 8])
                Qt = wt("Qt", [128, 512], BF16)
                Qc = wt("Qc", [128, 512], BF16)
                Kt = wt("Kt", [128, 512], BF16)
                KlT = wt("KlT", [128, 512])
                Kl = wt("Kl", [128, 512], BF16)
                Vb = wt("Vb", [128, 512], BF16)
                AT = wt("AT", [128, 512], BF16)
                St = wt("St", [128, 512])
                Sb = wt("Sb", [128, 512], BF16)
                gate = wt("gate", [128, 512])
                g2 = wt("g2", [128, 512])
                osq = wt("osq", [128, 512])
                ss = wt("ss", [128, 4])
                rstd = wt("rstd", [128, 4])
                ot = [wt(f"ot{i}", [128, 512]) for i in range(2)]
                onesf = C["ones_f"]

                kb.memset("dve", St[:, :], 0.0)
                kb.memset("dve", Sb[:, :], 0.0)

                kb.dma(xt[0][:, :], V(x_b[0:128, :], None), "x0")
                for t in range(nt):
                    sl = t % 2
                    if t + 1 < nt:
                        kb.dma(xt[1 - sl][:, :], V(x_b[(t + 1) * 128:(t + 2) * 128, :], None), f"x{1 - sl}")
                    for g in range(4):
                        Tb = T0 if g % 2 == 0 else T1
                        for j in range(4):
                            kk = 4 * g + j
                            kb.tr(Tb[:, j * 128:(j + 1) * 128], xt[sl][:, kk * 128:(kk + 1) * 128], ident[:, :])
                        src = V(Tb.h[:, :], None)
                        dst = xT[sl].k(g)[:, g * 512:(g + 1) * 512]
                        q = kb.evac_q()
                        rd = [Tb[:, 0:1] for j in range(4)]
                        if q == "act":
                            S.op("act", lambda e, d=dst, s=src: e.copy(d.ap, s.ap), reads=rd, writes=[dst])
                        else:
                            S.op("dve", lambda e, d=dst, s=src: e.tensor_copy(d.ap, s.ap), reads=rd, writes=[dst])
                    xTr = [xT[sl].k(kk // 4)[:, kk * 128:(kk + 1) * 128] for kk in range(16)]

                    for which, Fb in ((0, F0), (1, F1)):
                        for h in range(4):
                            c0 = which * 512 + h * 128
                            for kk in range(16):
                                kb.mm(Fb[:, h * 128:(h + 1) * 128], w_sb.k(kk)[:, kk, c0:c0 + 128], xTr[kk],
                                      start=(kk == 0), stop=(kk == 15))
                    for n, Mb in enumerate((M0, M1, M2)):
                        c0 = 1024 + n * 512
                        for kk in range(16):
                            kb.mm(Mb[:, :], xTr[kk], w_sb.k(kk)[:, kk, c0:c0 + 512], start=(kk == 0), stop=False)
                        kb.mm(Mb[:, :], ones_b[:, :], brow_b[:, n * 512:(n + 1) * 512], start=False, stop=True)

                    for h in range(4):
                        cs = slice(h * 128, (h + 1) * 128)
                        kb.act(qT[:, cs], F0[:, cs], AF.Silu, bias=bcol[:, h:h + 1])
                        kb.act(fT[:, cs], F1[:, cs], AF.Sigmoid, bias=bcol[:, 4 + h:5 + h])
                        kb.ts("dve", fT[:, cs], fT[:, cs], omlb[:, h:h + 1], ALU.mult, lbv[:, h:h + 1], ALU.add)
                        kb.ts("dve", kTt[:, cs], fT[:, cs], -1.0, ALU.mult, 1.0, ALU.add)
                    kb.act(fT[:, :], fT[:, :], AF.Ln)
                    for h in range(4):
                        cs = slice(h * 128, (h + 1) * 128)
                        S.op("dve", lambda e, cs=cs: e.tensor_tensor_scan(cum.h[:, cs], onesf.h[:, :], fT.h[:, cs], 0.0,
                                                                            ALU.mult, ALU.add),
                             reads=[onesf[:, :], fT[:, cs]], writes=[cum[:, cs]])
                        c63 = h * 128 + 63
                        cl = h * 128 + 127
                        kb.ts("dve", mid[:, 2 * h:2 * h + 1], cum[:, c63:c63 + 1], -1.0, ALU.mult)
                        kb.act(e1[:, cs], cum[:, cs], AF.Exp, bias=mid[:, 2 * h:2 * h + 1])
                        kb.act(e2[:, cs], cum[:, cs], AF.Exp, bias=cum[:, c63:c63 + 1], scale=-1.0)
                        kb.act(sc[:, 2 * h:2 * h + 1], cum[:, c63:c63 + 1], AF.Exp)
                        kb.act(sc[:, 2 * h + 1:2 * h + 2], cum[:, cl:cl + 1], AF.Exp, bias=mid[:, 2 * h:2 * h + 1])
                        kb.act(mid[:, 2 * h + 1:2 * h + 2], cum[:, cl:cl + 1], AF.Exp)
                        kb.tt("dve", Qt[:, cs], qT[:, cs], e1[:, cs], ALU.mult)
                        kb.ts("dve", Qc[:, cs], Qt[:, cs], sc[:, 2 * h:2 * h + 1], ALU.mult)
                        kb.tt("dve", Kt[:, cs], kTt[:, cs], e2[:, cs], ALU.mult)
                        kb.ts("dve", KlT[:, cs], Kt[:, cs], sc[:, 2 * h + 1:2 * h + 2], ALU.mult)
                    kb.copy("act", Vb[:, :], M0[:, :])
                    for h in range(4):
                        cs = slice(h * 128, (h + 1) * 128)
                        kb.mm(F0[:, cs], Kt[:, cs], Qt[:, cs], start=True, stop=True)
                        kb.tt("dve", AT[:, cs], F0[:, cs], cmask[:, :], ALU.mult)
                    for h in range(4):
                        cs = slice(h * 128, (h + 1) * 128)
                        kb.tr(F1[:, cs], KlT[:, cs], ident[:, :])
                        kb.copy("act", Kl[:, cs], F1[:, cs])
                    for h in range(4):
                        cs = slice(h * 128, (h + 1) * 128)
                        kb.mm(PO[:, cs], AT[:, cs], Vb[:, cs], start=True, stop=False)
                        kb.mm(PO[:, cs], Qc[:, cs], Sb[:, cs], start=False, stop=True)
                    for h in range(4):
                        cs = slice(h * 128, (h + 1) * 128)
                        kb.mm(M0[:, cs], Kl[:, cs], Vb[:, cs], start=True, stop=True)
                    for h in range(4):
                        cs = slice(h * 128, (h + 1) * 128)
                        kb.stt(St[:, cs], St[:, cs], mid[:, 2 * h + 1:2 * h + 2], M0[:, cs], ALU.mult, ALU.add)
                    kb.copy("act", Sb[:, :], St[:, :])
                    kb.act(gate[:, :], M1[:, :], AF.Sigmoid)
                    kb.act(g2[:, :], M2[:, :], AF.Sigmoid)
                    kb.tt("dve", gate[:, :], gate[:, :], g2[:, :], ALU.mult)
                    kb.tt("pool", gate[:, :], gate[:, :], hng[:, :], ALU.mult)
                    for h in range(4):
                        cs = slice(h * 128, (h + 1) * 128)
                        kb.act(osq[:, cs], PO[:, cs], AF.Square, accum=ss[:, h:h + 1])
                    kb.ts("dve", rstd[:, :], ss[:, :], 1.0 / 128.0, ALU.mult, EPS, ALU.add)
                    kb.act(rstd[:, :], rstd[:, :], AF.Sqrt)
                    S.op("dve", lambda e: e.reciprocal(rstd.h[:, :], rstd.h[:, :]), reads=[rstd[:, :]], writes=[rstd[:, :]])
                    o_t = ot[sl]
                    for h in range(4):
                        cs = slice(h * 128, (h + 1) * 128)
                        kb.stt(o_t[:, cs], PO[:, cs], rstd[:, h:h + 1], gate[:, cs], ALU.mult, ALU.mult)
                    kb.dma(V(gh_out[t * 128:(t + 1) * 128, :], f"gh{t}"), o_t[:, :], f"o{sl}")

                bcg = kb.sb([128, 8], F32, "bcg")
                wa2 = kb.sb([16, 256], F32, "wa2_sb")
                gng = kb.sb([128, 512], F32, "gng")
                ga1T = kb.sb([16, 128], F32, "ga1T")
                ght = [kb.sb([128, 512], F32, f"ght{i}") for i in range(2)]
                kb.dma(bcg[:, :], V(bcol_g, None), "c0")
                kb.memset("dve", brow[:, :], 0.0)
                kb.dma(brow[0:1, :], V(brow_g, None), "c1")
                kb.dma(wa2[:, :], V(wa2_in, None), "c2")
                kb.dma(gng[:, :], V(gng_rep, None), "c3")
                kb.copy("dve", brow_b[:, :], brow[:, :])
                kb.ts("dve", bcg[:, 0:2], bcg[:, 0:2], 1.0 / 16.0, ALU.mult)
                kb.ts("dve", bcg[:, 5:7], bcg[:, 5:7], -1.0, ALU.mult)
                _load_weights_bf16(kb, w_g, 2064, w_sb, stage, "ws")
                kb.memset("dve", St[:, :], 0.0)
                kb.memset("dve", Sb[:, :], 0.0)
                St2 = kb.sb([128, 512], F32, "St2")
                Sb2 = kb.sb([128, 512], BF16, "Sb2")
                kb.memset("dve", St2[:, :], 0.0)
                kb.memset("dve", Sb2[:, :], 0.0)
                Sts, Sbs = (St, St2), (Sb, Sb2)

                kb.dma(xt[0][:, :], V(x_b[0:128, :], None), "x0")
                for t in range(nt):
                    sl = t % 2
                    if t + 1 < nt:
                        kb.dma(xt[1 - sl][:, :], V(x_b[(t + 1) * 128:(t + 2) * 128, :], None), f"x{1 - sl}")
                    kb.dma(ght[sl][:, :], V(gh_out[t * 128:(t + 1) * 128, :], f"gh{t}"), f"g{sl}")
                    for g in range(4):
                        Tb = T0 if g % 2 == 0 else T1
                        for j in range(4):
                            kk = 4 * g + j
                            kb.tr(Tb[:, j * 128:(j + 1) * 128], xt[sl][:, kk * 128:(kk + 1) * 128], ident[:, :])
                        dst = xT[sl].k(g)[:, g * 512:(g + 1) * 512]
                        kb.copy(kb.evac_q(), dst, Tb[:, :])
                    xTr = [xT[sl].k(kk // 4)[:, kk * 128:(kk + 1) * 128] for kk in range(16)]
                    for u in range(4):
                        for kk in range(16):
                            kb.mm(F0[:, u * 128:(u + 1) * 128], w_sb.k(kk)[:, kk, u * 128:(u + 1) * 128], xTr[kk],
                                  start=(kk == 0), stop=(kk == 15))
                    for kk in range(16):
                        kb.mm(F1[0:16, 0:128], w_sb.k(kk)[:, kk, 512:528], xTr[kk], start=(kk == 0), stop=(kk == 15))
                    for n, Mb in enumerate((M0, M1, M2)):
                        c0 = 528 + n * 512
                        for kk in range(16):
                            kb.mm(Mb[:, :], xTr[kk], w_sb.k(kk)[:, kk, c0:c0 + 512], start=(kk == 0), stop=False)
                        kb.mm(Mb[:, :], ones_b[:, :], brow_b[:, n * 512:(n + 1) * 512], start=False, stop=True)
                    kb.act(ga1T[:, :], F1[0:16, 0:128], AF.Identity, bias=bcg[0:16, 4:5])
                    for u in range(2):
                        kb.mm(F1[:, 128 + u * 128:256 + u * 128], wa2[:, u * 128:(u + 1) * 128], ga1T[:, :],
                              start=True, stop=True)
                    for u in range(2):
                        cs = slice(u * 128, (u + 1) * 128)
                        kb.act(qT[:, cs], F0[:, cs], AF.Identity, bias=bcg[:, u:u + 1], scale=1.0 / 16.0)
                        kb.act(kTt[:, cs], F0[:, 256 + u * 128:384 + u * 128], AF.Identity, bias=bcg[:, 2 + u:3 + u])
                        kb.act(fT[:, cs], F1[:, 128 + u * 128:256 + u * 128], AF.Exp, bias=bcg[:, 5 + u:6 + u], scale=-1.0)
                    kb.act(fT[:, 0:256], fT[:, 0:256], AF.Ln, bias=1.0)
                    kb.ts("dve", fT[:, 0:256], fT[:, 0:256], -1.0 / 16.0, ALU.mult)
                    for h in range(2):
                        cs = slice(h * 128, (h + 1) * 128)
                        S.op("dve", lambda e, cs=cs: e.tensor_tensor_scan(cum.h[:, cs], onesf.h[:, :], fT.h[:, cs], 0.0,
                                                                            ALU.mult, ALU.add),
                             reads=[onesf[:, :], fT[:, cs]], writes=[cum[:, cs]])
                        c63 = h * 128 + 63
                        cl = h * 128 + 127
                        kb.ts("dve", mid[:, 2 * h:2 * h + 1], cum[:, c63:c63 + 1], -1.0, ALU.mult)
                        kb.act(e1[:, cs], cum[:, cs], AF.Exp, bias=mid[:, 2 * h:2 * h + 1])
                        kb.act(e2[:, cs], cum[:, cs], AF.Exp, bias=cum[:, c63:c63 + 1], scale=-1.0)
                        kb.act(sc[:, 2 * h:2 * h + 1], cum[:, c63:c63 + 1], AF.Exp)
                        kb.act(sc[:, 2 * h + 1:2 * h + 2], cum[:, cl:cl + 1], AF.Exp, bias=mid[:, 2 * h:2 * h + 1])
                        kb.act(mid[:, 2 * h + 1:2 * h + 2], cum[:, cl:cl + 1], AF.Exp)
                        kb.tt("dve", Qt[:, cs], qT[:, cs], e1[:, cs], ALU.mult)
                        kb.ts("dve", Qc[:, cs], Qt[:, cs], sc[:, 2 * h:2 * h + 1], ALU.mult)
                        kb.tt("dve", Kt[:, cs], kTt[:, cs], e2[:, cs], ALU.mult)
                        kb.ts("dve", KlT[:, cs], Kt[:, cs], sc[:, 2 * h + 1:2 * h + 2], ALU.mult)
                    kb.copy("act", Vb[:, :], M0[:, :])
                    for u in range(2):
                        cs = slice(u * 128, (u + 1) * 128)
                        kb.mm(F0[:, 0:128], Kt[:, cs], Qt[:, cs], start=(u == 0), stop=(u == 1))
                    kb.tt("dve", AT[:, 0:128], F0[:, 0:128], cmask[:, :], ALU.mult)
                    for u in range(2):
                        cs = slice(u * 128, (u + 1) * 128)
                        kb.tr(F1[:, cs], KlT[:, cs], ident[:, :])
                    kb.copy("act", Kl[:, 0:256], F1[:, 0:256])
                    kb.mm(PO[:, :], AT[:, 0:128], Vb[:, :], start=True, stop=False)
                    for u in range(2):
                        cs = slice(u * 128, (u + 1) * 128)
                        kb.mm(PO[:, :], Qc[:, cs], Sbs[u][:, :], start=False, stop=(u == 1))
                    for u, Pb in enumerate((M0, F0)):
                        cs = slice(u * 128, (u + 1) * 128)
                        kb.mm(Pb[:, :], Kl[:, cs], Vb[:, :], start=True, stop=True)
                    for u, Pb in enumerate((M0, F0)):
                        kb.stt(Sts[u][:, :], Sts[u][:, :], mid[:, 2 * u + 1:2 * u + 2], Pb[:, :], ALU.mult, ALU.add)
                        kb.copy("act", Sbs[u][:, :], Sts[u][:, :])
                    kb.act(gate[:, :], M1[:, :], AF.Silu)
                    kb.act(g2[:, :], M2[:, :], AF.Sigmoid)
                    kb.tt("dve", gate[:, :], gate[:, :], g2[:, :], ALU.mult)
                    kb.tt("pool", gate[:, :], gate[:, :], gng[:, :], ALU.mult)
                    kb.act(osq[:, :], PO[:, :], AF.Square, accum=ss[:, 0:1])
                    kb.ts("dve", rstd[:, 0:1], ss[:, 0:1], 1.0 / 512.0, ALU.mult, EPS, ALU.add)
                    kb.act(rstd[:, 0:1], rstd[:, 0:1], AF.Sqrt)
                    S.op("dve", lambda e: e.reciprocal(rstd.h[:, 0:1], rstd.h[:, 0:1]), reads=[rstd[:, :]], writes=[rstd[:, :]])
                    o_t = ot[sl]
                    kb.stt(o_t[:, :], PO[:, :], rstd[:, 0:1], gate[:, :], ALU.mult, ALU.mult)
                    kb.tt("dve", o_t[:, :], o_t[:, :], ght[sl][:, :], ALU.add)
                    kb.dma(V(mg_out[t * 128:(t + 1) * 128, :], "merged"), o_t[:, :], f"o{sl}")
            S.emit()
            pA.close()
        if doB:
            phases_bcd(nc, S, kb, C, (T0, T1, F0, F1, M0, M1, M2, PO), io, dbg)
    return nc


def _prep_core_inputs(inp, c):
    b, hh = c // 4, c % 4
    w_in = inp["w_in"][0]
    b_in = inp["b_in"][0]
    offs = np.cumsum([0, 1024, 1024, 2048, 2048, 16, 2048, 2048, 2048, 2048, 2048, 2048])
    o_gq, o_gk, o_gv, o_gr, o_ga1, o_hq, o_hf, o_hi, o_hg, o_ma, o_mb = offs[:11]
    ch = slice(512 * hh, 512 * hh + 512)

    def cols(o, s):
        return np.arange(o + s.start, o + s.stop)
    hcols = np.concatenate([cols(o_hq, ch), cols(o_hf, ch), cols(o_hi, ch), cols(o_hg, ch), cols(o_mb, ch)])
    w_h = np.ascontiguousarray(w_in[:, hcols])
    bh = b_in[hcols]
    bcol_h = np.ascontiguousarray(bh[:1024].reshape(8, 128).T)
    brow_h = np.ascontiguousarray(bh[1024:].reshape(1, 1536))
    lb = inp["hgrn_lb_logits"][:, ch]
    lbl = np.ascontiguousarray(np.concatenate([lb[0].reshape(4, 128).T, lb[1].reshape(4, 128).T], axis=1))
    hng_rep = np.ascontiguousarray(np.broadcast_to(np.tile(inp["hgrn_norm_g"][0], 4)[None, :], (128, 512)))
    kq = slice(256 * hh, 256 * hh + 256)
    gcols = np.concatenate([cols(o_gq, kq), cols(o_gk, kq), np.arange(o_ga1, o_ga1 + 16), cols(o_gv, ch),
                            cols(o_gr, ch), cols(o_ma, ch)])
    w_g = np.ascontiguousarray(w_in[:, gcols])
    bg = b_in[gcols]
    bcol_g = np.zeros((128, 8), np.float32)
    bcol_g[:, 0:4] = bg[:512].reshape(4, 128).T
    bcol_g[:16, 4] = bg[512:528]
    bcol_g[:, 5:7] = inp["b_gla_a"][0][kq].reshape(2, 128).T
    brow_g = np.ascontiguousarray(bg[528:].reshape(1, 1536))
    wa2 = np.ascontiguousarray(inp["w_gla_a2"][0][:, kq])
    gng_rep = np.ascontiguousarray(np.broadcast_to(inp["gla_norm_g"][0][None, :], (128, 512)))
    return dict(x_b=np.ascontiguousarray(inp["x"][b]), w_h=w_h, bcol_h=bcol_h, brow_h=brow_h, lbl=lbl,
                hng_rep=hng_rep, w_g=w_g, bcol_g=bcol_g, brow_g=brow_g, wa2=wa2, gng_rep=gng_rep)


def _prep_a_inputs(inp, b, ng=4):
    parts = [_prep_core_inputs(inp, 4 * b + hh) for hh in range(ng)]
    out = dict(x_b=parts[0]["x_b"], hng_rep=parts[0]["hng_rep"], gng_rep=parts[0]["gng_rep"])
    for k in ("w_h", "bcol_h", "brow_h", "lbl", "w_g", "bcol_g", "brow_g", "wa2"):
        out[k] = np.ascontiguousarray(np.concatenate([p[k] for p in parts], axis=0))
    return out


def _prep_bcd_inputs(inp, b):
    def rep(v):
        return np.ascontiguousarray(np.broadcast_to(v[None, :], (128, v.shape[0])))
    bgu = inp["b_gate_up"][0]
    bg = bgu[:, 0::2].reshape(32, 16, 128)
    bl = bgu[:, 1::2].reshape(32, 16, 128)
    bgu_col = np.ascontiguousarray(np.concatenate([bg, bl], axis=1).transpose(2, 0, 1).reshape(128, 1024))
    return dict(
        x_b=np.ascontiguousarray(inp["x"][b]),
        ln1g=rep(inp["ln1_g"][0]), ln1b=rep(inp["ln1_b"][0]), ln2g=rep(inp["ln2_g"][0]), ln2b=rep(inp["ln2_b"][0]),
        ln3g=rep(inp["ln3_g"][0]), ln3b=rep(inp["ln3_b"][0]),
        w_o=inp["w_mix_o"][0], w_kv=inp["w_mem_kv"][0], w_xq=inp["w_xq"][0], w_xo=inp["w_xo"][0],
        mem_b=np.ascontiguousarray(inp["mem"][b]), w_router=inp["w_router"][0], br_rep=rep(inp["b_router"][0]),
        bgu_col=bgu_col, b_dn=inp["b_down"][0],
        w_gu=inp["w_gate_up"][0].reshape(32 * D, 2 * D), w_dn=inp["w_down"][0].reshape(32 * D, D))


def kernel(**inputs):
    inp = {k: np.asarray(v) for k, v in inputs.items()}
    nc = build_program("full")
    in_maps = []
    for b in range(2):
        m = _prep_a_inputs(inp, b)
        m.update(_prep_bcd_inputs(inp, b))
        in_maps.append(m)
    res = run_bass_kernel_spmd(nc, in_maps, core_ids=[0, 1])
    out = np.stack([np.asarray(r["out"]) for r in res.results], axis=0)
    return out.astype(np.float32)
```

```python
import contextlib
import numpy as np
import concourse.bass as bass
import concourse.mybir as mybir
from concourse.bass_utils import run_bass_kernel_spmd

F32 = mybir.dt.float32
BF16 = mybir.dt.bfloat16
I32 = mybir.dt.int32
U32 = mybir.dt.uint32
AF = mybir.ActivationFunctionType
ALU = mybir.AluOpType
AX = mybir.AxisListType

D = 2048
SEQ = 8192
NT_SEQ = SEQ // 128
EPS = 1e-5
ALPHA = 2.0 ** 0.25
SAME_ENGINE_SYNC = True


class V:
    __slots__ = ("ap", "key")

    def __init__(self, ap, key):
        self.ap = ap
        self.key = key


class Tile:
    def __init__(self, handle, key):
        self.h = handle
        self.key = key

    def __getitem__(self, idx):
        return V(self.h[idx], self.key)

    def k(self, sfx):
        return _Keyed(self.h, self.key + ":" + str(sfx))


class _Keyed:
    def __init__(self, h, key):
        self.h = h
        self.key = key

    def __getitem__(self, idx):
        return V(self.h[idx], self.key)


class Sched:
    QS = ("pe", "act", "dve", "pool", "sp")

    def __init__(self, nc, stack):
        self.nc = nc
        self.stack = stack
        self.items = {q: [] for q in self.QS}
        self.esem = {}
        for q in ("pe", "act", "dve", "pool"):
            self.esem[q] = stack.enter_context(nc.semaphore("es_" + q))
        self.cnt = {q: 0 for q in self.QS}
        self.waited = {q: {} for q in self.QS}
        self.res = {}
        self.dsem = {}
        self.semh = {"es_" + q: h for q, h in self.esem.items()}
        self.n_ops = 0

    def _deps(self, q, reads, writes):
        evs = []
        for r in reads:
            st = self.res.get(r)
            if st and st[0]:
                evs.append(st[0])
        for w in writes:
            st = self.res.get(w)
            if st:
                if st[0]:
                    evs.append(st[0])
                evs.extend(st[1])
        waits = {}
        for (sem, val, srcq) in evs:
            if srcq == q and (q == "pe" or not SAME_ENGINE_SYNC):
                continue
            if self.waited[q].get(sem, 0) >= val:
                continue
            if waits.get(sem, 0) < val:
                waits[sem] = val
        for sem, val in waits.items():
            self.waited[q][sem] = val
        return list(waits.items())

    def _commit(self, ev, reads, writes):
        for r in reads:
            if r in writes:
                continue
            self.res.setdefault(r, [None, []])[1].append(ev)
        for w in writes:
            self.res[w] = [ev, []]

    @staticmethod
    def _keys(views):
        return [v.key for v in views if v is not None and v.key is not None]

    def op(self, q, fn, reads=(), writes=()):
        rk, wk = self._keys(reads), self._keys(writes)
        waits = self._deps(q, rk, wk)
        self.cnt[q] += 1
        ev = ("es_" + q, self.cnt[q], q)
        self.items[q].append((waits, fn, ("es_" + q, 1)))
        self._commit(ev, rk, wk)
        self.n_ops += 1

    def dma(self, q, fn, sem, reads=(), writes=()):
        if sem not in self.dsem:
            h = self.stack.enter_context(self.nc.semaphore("ds_" + sem))
            self.dsem[sem] = [h, 0]
            self.semh["ds_" + sem] = h
        rk, wk = self._keys(reads), self._keys(writes)
        waits = self._deps(q, rk, wk)
        self.dsem[sem][1] += 16
        ev = ("ds_" + sem, self.dsem[sem][1], "dma")
        self.items[q].append((waits, fn, ("ds_" + sem, 16)))
        self._commit(ev, rk, wk)
        self.n_ops += 1

    def emit(self, final=False):
        nc = self.nc
        semh = self.semh
        items = self.items
        bar = [("es_" + q, self.cnt[q]) for q in ("pe", "act", "dve", "pool") if self.cnt[q] > 0]
        bar += [("ds_" + k, v[1]) for k, v in self.dsem.items() if v[1] > 0]

        def replay(q):
            def run(eng):
                for waits, fn, inc in items[q]:
                    for sem, val in waits:
                        eng.wait_ge(semh[sem], val)
                    ins = fn(eng)
                    ins.then_inc(semh[inc[0]], inc[1])
                for sem, val in bar:
                    if self.waited[q].get(sem, 0) < val:
                        eng.wait_ge(semh[sem], val)
                        self.waited[q][sem] = val
            return run

        with nc.Block() as block:
            block.sync(replay("sp"))
            block.tensor(replay("pe"))
            block.scalar(replay("act"))
            block.vector(replay("dve"))
            block.gpsimd(replay("pool"))
        self.items = {q: [] for q in self.QS}


class K:
    def __init__(self, nc, S, stack):
        self.nc, self.S, self.stack = nc, S, stack
        self._n = 0
        self.rr = 0
        self._tiles = {}

    def sb(self, shape, dt, name):
        if name in self._tiles:
            return self._tiles[name]
        h = self.stack.enter_context(self.nc.sbuf_tensor(name, list(shape), dt))
        t = Tile(h, name)
        self._tiles[name] = t
        return t

    def ps(self, name, dt=F32, cols=512):
        h = self.stack.enter_context(self.nc.psum_tensor(name, [128, cols], dt))
        return Tile(h, name)

    def mm(self, out, lhsT, rhs, start, stop):
        self.S.op("pe", lambda e: e.matmul(out.ap, lhsT.ap, rhs.ap, start=start, stop=stop),
                  reads=[lhsT, rhs] + ([] if start else [out]), writes=[out])

    def tr(self, out, in_, ident):
        self.S.op("pe", lambda e: e.transpose(out.ap, in_.ap, ident.ap), reads=[in_, ident], writes=[out])

    def act(self, out, in_, func, bias=None, scale=None, accum=None, q="act"):
        kw = {}
        rd = [in_]
        if bias is not None:
            if isinstance(bias, V):
                kw["bias"] = bias.ap
                rd.append(bias)
            else:
                kw["bias"] = float(bias)
        if scale is not None:
            if isinstance(scale, V):
                kw["scale"] = scale.ap
                rd.append(scale)
            else:
                kw["scale"] = float(scale)
        wr = [out]
        if accum is not None:
            kw["accum_out"] = accum.ap
            wr.append(accum)
        self.S.op("act", lambda e: e.activation(out.ap, in_.ap, func, **kw), reads=rd, writes=wr)

    def copy(self, q, out, in_):
        if q == "act":
            self.S.op("act", lambda e: e.copy(out.ap, in_.ap), reads=[in_], writes=[out])
        else:
            self.S.op(q, lambda e: e.tensor_copy(out.ap, in_.ap), reads=[in_], writes=[out])

    def tt(self, q, out, a, b, op):
        self.S.op(q, lambda e: e.tensor_tensor(out.ap, a.ap, b.ap, op), reads=[a, b], writes=[out])

    def ts(self, q, out, a, s1, op0, s2=None, op1=None, accum=None):
        rd = [a]
        s1v = s1.ap if isinstance(s1, V) else float(s1)
        if isinstance(s1, V):
            rd.append(s1)
        s2v = None
        if s2 is not None:
            s2v = s2.ap if isinstance(s2, V) else float(s2)
            if isinstance(s2, V):
                rd.append(s2)
        wr = [out]
        kw = {}
        if accum is not None:
            kw["accum_out"] = accum.ap
            wr.append(accum)
        o1 = op1 if op1 is not None else ALU.bypass
        self.S.op(q, lambda e: e.tensor_scalar(out.ap, a.ap, s1v, s2v, op0, o1, **kw), reads=rd, writes=wr)

    def stt(self, out, a, s, b, op0, op1):
        rd = [a, b]
        sv = s.ap if isinstance(s, V) else float(s)
        if isinstance(s, V):
            rd.append(s)
        self.S.op("dve", lambda e: e.scalar_tensor_tensor(out.ap, a.ap, sv, b.ap, op0, op1), reads=rd, writes=[out])

    def memset(self, q, out, val):
        self.S.op(q, lambda e: e.memset(out.ap, val), writes=[out])

    def dma(self, out, in_, sem, q="sp", **kw):
        self.S.dma(q, lambda e: e.dma_start(out.ap, in_.ap, **kw), sem, reads=[in_], writes=[out])

    def evac_q(self):
        self.rr += 1
        return "act" if self.rr % 2 else "dve"


def _consts(kb):
    nc, S = kb.nc, kb.S
    ones_f = kb.sb([128, 128], F32, "c_onesf")
    ident = kb.sb([128, 128], F32, "c_ident")
    cmask = kb.sb([128, 128], F32, "c_cmask")
    ones_b = kb.sb([128, 128], BF16, "c_onesb")
    kb.memset("pool", ones_f[:, :], 1.0)
    kb.memset("pool", ones_b[:, :], 1.0)
    S.op("pool", lambda e: e.affine_select(ident.h[:, :], ones_f.h[:, :], [[-1, 128]], ALU.is_equal, 0.0,
                                           base=0, channel_multiplier=1),
         reads=[ones_f[:, :]], writes=[ident[:, :]])
    S.op("pool", lambda e: e.affine_select(cmask.h[:, :], ones_f.h[:, :], [[1, 128]], ALU.is_ge, 0.0,
                                           base=0, channel_multiplier=-1),
         reads=[ones_f[:, :]], writes=[cmask[:, :]])
    return dict(ones_f=ones_f, ident=ident, cmask=cmask, ones_b=ones_b)


def _load_weights_bf16(kb, w_dram, ncols, w_sb, stage, tagsem):
    for k in range(16):
        st = stage[k % 2]
        kb.dma(st[:, :ncols], V(w_dram[k * 128:(k + 1) * 128, :], None), f"{tagsem}{k % 2}")
        q = ("act", "dve", "pool")[k % 3]
        kb.copy(q, w_sb.k(k)[:, k, :ncols], st[:, :ncols])


def _tr_tile(kb, src, dst, Pa, Pb, ident, ncols=128):
    for g in range(4):
        Tb = Pa if g % 2 == 0 else Pb
        for j in range(4):
            kk = 4 * g + j
            kb.tr(Tb[:, j * 128:(j + 1) * 128], src[:, kk * 128:(kk + 1) * 128], ident[:, :])
        kb.copy(kb.evac_q(), dst[:, g * 512:(g + 1) * 512], Tb[:, :])


def _ln_tile(kb, y, g_rep, b_rep, out, stats, mv, r):
    S = kb.S
    for c in range(4):
        S.op("dve", lambda e, c=c: e.bn_stats(stats.h[:, c * 6:(c + 1) * 6], y.h[:, c * 512:(c + 1) * 512]),
             reads=[y[:, :]], writes=[stats[:, :]])
    S.op("dve", lambda e: e.bn_aggr(mv.h[:, 0:2], stats.h[:, 0:24]), reads=[stats[:, :]], writes=[mv[:, :]])
    kb.ts("dve", r[:, :], mv[:, 1:2], EPS, ALU.add)
    kb.act(r[:, :], r[:, :], AF.Sqrt)
    S.op("dve", lambda e: e.reciprocal(r.h[:, :], r.h[:, :]), reads=[r[:, :]], writes=[r[:, :]])
    kb.ts("dve", out[:, :], y[:, :], mv[:, 0:1], ALU.subtract, r[:, 0:1], ALU.mult)
    kb.tt("pool", out[:, :], out[:, :], g_rep[:, :], ALU.mult)
    kb.tt("pool", out[:, :], out[:, :], b_rep[:, :], ALU.add)


def _load_w(kb, w_ap, ncols, w_sb, stage, tagsem, col0=0):
    for k in range(16):
        st = stage[k % 2]
        kb.dma(st[:, :ncols], V(w_ap[k * 128:(k + 1) * 128, col0:col0 + ncols], None), f"{tagsem}{k % 2}")
        q = ("act", "dve", "pool")[k % 3]
        kb.copy(q, w_sb.k(k)[:, k, :ncols], st[:, :ncols])


NTT = 64
NTOK = NTT * 128
CAP = 1280
NJ = CAP // 128
RGS = ((0, 512), (512, 512), (1024, 256))
YROWS = 1 + 4 * NTOK + 128


def phases_bcd(nc, S, kb, C, P, io, dbg):
    ident, ones_b, ones_f = C["ident"], C["ones_b"], C["ones_f"]
    T0, T1, F0, F1, M0, M1, M2, PO = P
    mg, x_tok, out_ap = io["merged"], io["x_tok"], io["out"]
    h1s, h2s, ybufs = io["h1s"], io["h2s"], io["ybuf"]
    n_exp = io.get("n_exp", 32)

    kb.stack = S.stack
    idx_tok = kb.sb([128, 32 * NJ], I32, "R_idxtok")
    dest = kb.sb([128, 32 * NJ], I32, "R_dest")
    gl = kb.sb([128, 32 * NJ], F32, "R_gl")

    pB = contextlib.ExitStack()
    kb.stack = pB
    wsb = kb.sb([128, 16, 2048], BF16, "B_w")
    stage = [kb.sb([128, 2048], F32, f"B_st{i}") for i in range(2)]
    g_rep = kb.sb([128, 2048], F32, "B_g")
    b_rep = kb.sb([128, 2048], F32, "B_b")
    mt = [kb.sb([128, 2048], F32, f"B_mt{i}") for i in range(2)]
    xk = [kb.sb([128, 2048], F32, f"B_xk{i}") for i in range(2)]
    mT = [kb.sb([128, 2048], BF16, f"B_mT{i}") for i in range(2)]
    yt = [kb.sb([128, 2048], F32, f"B_y{i}") for i in range(2)]
    stats = kb.sb([128, 24], F32, "B_stats")
    mv = kb.sb([128, 2], F32, "B_mv")
    rr = kb.sb([128, 1], F32, "B_r")
    kb.dma(g_rep[:, :], V(io["ln1g"], None), "c0")
    kb.dma(b_rep[:, :], V(io["ln1b"], None), "c1")
    _load_w(kb, io["w_o"], 2048, wsb, stage, "ws")
    kb.dma(mt[0][:, :], V(mg[0:128, :], "merged"), "mt0")
    kb.dma(xk[0][:, :], V(x_tok[0:128, :], None), "xk0")
    for i in range(NTT):
        sl = i % 2
        if i + 1 < NTT:
            kb.dma(mt[1 - sl][:, :], V(mg[(i + 1) * 128:(i + 2) * 128, :], "merged"), f"mt{1 - sl}")
            kb.dma(xk[1 - sl][:, :], V(x_tok[(i + 1) * 128:(i + 2) * 128, :], None), f"xk{1 - sl}")
        _tr_tile(kb, mt[sl], mT[sl], T0, T1, ident)
        for n, Pb in enumerate((F0, F1, M0, M1)):
            for kk in range(16):
                kb.mm(Pb[:, :], mT[sl][:, kk * 128:(kk + 1) * 128], wsb.k(kk)[:, kk, n * 512:(n + 1) * 512],
                      start=(kk == 0), stop=(kk == 15))
            kb.stt(yt[sl][:, n * 512:(n + 1) * 512], xk[sl][:, n * 512:(n + 1) * 512], ALPHA, Pb[:, :],
                   ALU.mult, ALU.add)
        _ln_tile(kb, yt[sl], g_rep, b_rep, yt[sl], stats, mv, rr)
        kb.dma(V(h1s[i * 128:(i + 1) * 128, :], "h1s"), yt[sl][:, :], f"ho{sl}")
    S.emit()
    pB.close()

    pR = contextlib.ExitStack()
    kb.stack = pR
    gate_all = kb.sb([128, NTT, 32], F32, "R_gate")

    pB = contextlib.ExitStack()
    kb.stack = pB
    wq = kb.sb([128, 16, 2048], BF16, "C_wq")
    wo = kb.sb([128, 16, 2048], BF16, "C_wo")
    g_rep = kb.sb([128, 2048], F32, "C_g")
    b_rep = kb.sb([128, 2048], F32, "C_b")
    big = kb.sb([128, 4096], BF16, "C_big")
    memT = Tile(big.h[:, :].rearrange("p (k m) -> p k m", k=16), "C_memT")
    KT = kb.sb([128, 16, 256], BF16, "C_KT")
    Vs = kb.sb([128, 2, 2048], BF16, "C_V")
    ht0 = kb.sb([128, 2048], F32, "C_ht0")
    ht = [ht0, ht0]
    stage = ht
    hT = kb.sb([128, 2048], BF16, "C_hT")
    qT = Tile(big.h[:, 0:2048], "C_qT")
    pT = Tile(big.h[:, 2048:3072], "C_pT")
    oT = hT
    yt = kb.sb([128, 2048], F32, "C_y")
    memt = yt
    pf = Tile(yt.h[:, 0:1024], "C_y")
    wr = kb.sb([128, 16, 32], F32, "C_wr")
    br = kb.sb([128, 32], F32, "C_br")
    stats = kb.sb([128, 24], F32, "C_stats")
    mv = kb.sb([128, 2], F32, "C_mv")
    rr = kb.sb([128, 1], F32, "C_r")
    mx = kb.sb([128, 4], F32, "C_mx")
    sm = kb.sb([128, 4], F32, "C_sm")
    m8 = kb.sb([128, 8], F32, "C_m8")
    ex = kb.sb([128, 32], F32, "C_ex")
    s1 = kb.sb([128, 2], F32, "C_s1")
    mk = kb.sb([128, 32], F32, "C_mk")
    kb.dma(g_rep[:, :], V(io["ln2g"], None), "c0")
    kb.dma(b_rep[:, :], V(io["ln2b"], None), "c1")
    kb.dma(wr[:, :, :], V(io["w_router"].rearrange("(k p) e -> p k e", p=128), None), "c2")
    kb.dma(br[:, :], V(io["br_rep"], None), "c3")
    for mtile in range(2):
        kb.dma(memt[:, :], V(io["mem_b"][mtile * 128:(mtile + 1) * 128, :], None), "mm")
        for g in range(4):
            Tb = T0 if g % 2 == 0 else T1
            for j in range(4):
                kk = 4 * g + j
                kb.tr(Tb[:, j * 128:(j + 1) * 128], memt[:, kk * 128:(kk + 1) * 128], ident[:, :])
            S.op("dve", lambda e, Tb=Tb, g=g, mtile=mtile: e.tensor_copy(
                memT.h[:, 4 * g:4 * g + 4, mtile * 128:(mtile + 1) * 128],
                Tb.h[:, :].rearrange("p (a b) -> p a b", a=4)), reads=[Tb[:, :]], writes=[memT[:, :, :]])
    _load_w(kb, io["w_kv"], 2048, wq, stage, "ws", col0=0)
    banks = (F0, F1, M0, M1)
    for c in range(16):
        Pb = banks[(c // 2) % 4]
        cs = slice((c % 2) * 256, (c % 2) * 256 + 256)
        for kk in range(16):
            kb.mm(Pb[:, cs], wq.k(kk)[:, kk, c * 128:(c + 1) * 128], memT[:, kk, :], start=(kk == 0), stop=(kk == 15))
        if c % 2 == 1:
            S.op("act", lambda e, Pb=Pb, c=c: e.copy(KT.h[:, c - 1:c + 1, :], Pb.h[:, :].rearrange("p (a b) -> p a b", a=2)),
                 reads=[Pb[:, :]], writes=[KT[:, :, :]])
    _load_w(kb, io["w_kv"], 2048, wq, stage, "ws", col0=2048)
    for mtile in range(2):
        for n in range(4):
            Pb = banks[n]
            for kk in range(16):
                kb.mm(Pb[:, :], memT[:, kk, mtile * 128:(mtile + 1) * 128], wq.k(kk)[:, kk, n * 512:(n + 1) * 512],
                      start=(kk == 0), stop=(kk == 15))
            kb.copy(kb.evac_q(), Vs[:, mtile, n * 512:(n + 1) * 512], Pb[:, :])
    _load_w(kb, io["w_xq"], 2048, wq, stage, "ws")
    _load_w(kb, io["w_xo"], 2048, wo, stage, "ws")
    SC = 512.0 ** -0.5
    for i in range(NTT):
        sl = i % 2
        kb.dma(ht[sl][:, :], V(h1s[i * 128:(i + 1) * 128, :], "h1s"), "ht")
        _tr_tile(kb, ht[sl], hT, T0, T1, ident)
        for c in range(16):
            Pb = banks[c // 4]
            cs = slice((c % 4) * 128, (c % 4) * 128 + 128)
            for kk in range(16):
                kb.mm(Pb[:, cs], wq.k(kk)[:, kk, c * 128:(c + 1) * 128], hT[:, kk * 128:(kk + 1) * 128],
                      start=(kk == 0), stop=(kk == 15))
            if c % 4 == 3:
                g = c // 4
                kb.act(qT[:, g * 512:(g + 1) * 512], Pb[:, :], AF.Identity, scale=SC)
        for hx in range(4):
            Pb = M2 if hx < 2 else PO
            cs = slice((hx % 2) * 256, (hx % 2) * 256 + 256)
            for cc in range(4):
                c = hx * 4 + cc
                kb.mm(Pb[:, cs], qT[:, c * 128:(c + 1) * 128], KT[:, c, :], start=(cc == 0), stop=(cc == 3))
        for hx in range(4):
            Pb = M2 if hx < 2 else PO
            cs = slice((hx % 2) * 256, (hx % 2) * 256 + 256)
            S.op("dve", lambda e, Pb=Pb, cs=cs, hx=hx: e.reduce_max(mx.h[:, hx:hx + 1], Pb.h[:, cs], AX.X),
                 reads=[Pb[:, :]], writes=[mx[:, :]])
        kb.ts("dve", mx[:, :], mx[:, :], -1.0, ALU.mult)
        for hx in range(4):
            Pb = M2 if hx < 2 else PO
            cs = slice((hx % 2) * 256, (hx % 2) * 256 + 256)
            kb.act(pf[:, hx * 256:(hx + 1) * 256], Pb[:, cs], AF.Exp, bias=mx[:, hx:hx + 1], accum=sm[:, hx:hx + 1])
        S.op("dve", lambda e: e.reciprocal(sm.h[:, :], sm.h[:, :]), reads=[sm[:, :]], writes=[sm[:, :]])
        for hx in range(4):
            kb.ts("dve", pf[:, hx * 256:(hx + 1) * 256], pf[:, hx * 256:(hx + 1) * 256], sm[:, hx:hx + 1], ALU.mult)
        for g in range(2):
            Tb = T0 if g == 0 else T1
            for j in range(4):
                kk = 4 * g + j
                kb.tr(Tb[:, j * 128:(j + 1) * 128], pf[:, kk * 128:(kk + 1) * 128], ident[:, :])
            kb.copy(kb.evac_q(), pT[:, g * 512:(g + 1) * 512], Tb[:, :])
        for c in range(16):
            Pb = banks[c // 4]
            cs = slice((c % 4) * 128, (c % 4) * 128 + 128)
            hx = c // 4
            for mc in range(2):
                kb.mm(Pb[:, cs], Vs[:, mc, c * 128:(c + 1) * 128], pT[:, (hx * 2 + mc) * 128:(hx * 2 + mc + 1) * 128],
                      start=(mc == 0), stop=(mc == 1))
            if c % 4 == 3:
                g = c // 4
                kb.copy(kb.evac_q(), oT[:, g * 512:(g + 1) * 512], Pb[:, :])
        for n in range(4):
            Pb = banks[n]
            for kk in range(16):
                kb.mm(Pb[:, :], oT[:, kk * 128:(kk + 1) * 128], wo.k(kk)[:, kk, n * 512:(n + 1) * 512],
                      start=(kk == 0), stop=(kk == 15))
            kb.stt(yt[:, n * 512:(n + 1) * 512], ht[sl][:, n * 512:(n + 1) * 512], ALPHA, Pb[:, :], ALU.mult, ALU.add)
        _ln_tile(kb, yt, g_rep, b_rep, yt, stats, mv, rr)
        kb.dma(V(h2s[i * 128:(i + 1) * 128, :], "h2s"), yt[:, :], "ho")
        h2T = ht[sl]
        for g in range(4):
            Tb = T0 if g % 2 == 0 else T1
            for j in range(4):
                kk = 4 * g + j
                kb.tr(Tb[:, j * 128:(j + 1) * 128], yt[:, kk * 128:(kk + 1) * 128], ident[:, :])
            kb.copy(kb.evac_q(), h2T[:, g * 512:(g + 1) * 512], Tb[:, :])
        for kk in range(16):
            kb.mm(M2[:, 0:32], h2T[:, kk * 128:(kk + 1) * 128], wr[:, kk, :], start=(kk == 0), stop=(kk == 15))
        lg = ex
        kb.tt("dve", lg[:, :], M2[:, 0:32], br[:, :], ALU.add)
        S.op("dve", lambda e: e.max(m8.h[:, :], lg.h[:, :]), reads=[lg[:, :]], writes=[m8[:, :]])
        kb.ts("dve", mk[:, :], lg[:, :], m8[:, 3:4], ALU.is_ge)
        kb.ts("dve", s1[:, 0:1], m8[:, 0:1], -1.0, ALU.mult)
        kb.act(lg[:, :], lg[:, :], AF.Exp, bias=s1[:, 0:1])
        kb.tt("dve", lg[:, :], lg[:, :], mk[:, :], ALU.mult)
        S.op("dve", lambda e: e.reduce_sum(s1.h[:, 1:2], lg.h[:, :], AX.X), reads=[lg[:, :]], writes=[s1[:, :]])
        S.op("dve", lambda e: e.reciprocal(s1.h[:, 1:2], s1.h[:, 1:2]), reads=[s1[:, :]], writes=[s1[:, :]])
        kb.ts("dve", gate_all[:, i, :], lg[:, :], s1[:, 1:2], ALU.mult)
    S.emit()
    pB.close()

    pB = contextlib.ExitStack()
    kb.stack = pB
    mask_bf = kb.sb([128, NTT, 32], BF16, "D_maskbf")
    SU = kb.sb([128, 128], BF16, "D_SU")
    pos = kb.sb([128, NTT, 32], F32, "D_pos")
    slot = kb.sb([128, NTT, 32], F32, "D_slot")
    toki = kb.sb([128, NTT], I32, "D_toki")
    tokf = kb.sb([128, NTT], F32, "D_tokf")
    tokp1 = kb.sb([128, NTT], F32, "D_tokp1")
    pay = kb.sb([128, NTT, 32, 3], F32, "D_pay")
    io_i = kb.sb([128, CAP], I32, "D_ioi")
    io_f = kb.sb([128, CAP], F32, "D_iof")
    tr_i = kb.sb([128, 1], I32, "D_tri")
    tr_f = kb.sb([128, 1], F32, "D_trf")
    zer = kb.sb([128, 128], F32, "D_zer")
    Sel = [kb.sb([128, CAP], F32, f"D_sel{i}") for i in range(3)]
    lst = kb.sb([128, 32, NJ * 3], F32, "D_lst")
    tmpd = kb.sb([128, 32 * NJ], F32, "D_tmpd")
    mask_all = kb.sb([128, NTT, 32], F32, "D_mask")
    kb.memset("pool", zer[:, :], 0.0)
    kb.ts("dve", mask_all[:, :, :], gate_all[:, :, :], 0.0, ALU.is_gt)
    kb.copy("dve", mask_bf[:, :, :], mask_all[:, :, :])
    S.op("pool", lambda e: e.affine_select(SU.h[:, :], ones_f.h[:, :], [[1, 128]], ALU.is_ge, 0.0,
                                           base=-1, channel_multiplier=-1), reads=[ones_f[:, :]], writes=[SU[:, :]])
    S.op("pool", lambda e: e.iota(toki.h[:, :], [[128, NTT]], base=0, channel_multiplier=1), writes=[toki[:, :]])
    S.op("pool", lambda e: e.iota(io_i.h[:, :], [[1, CAP]], base=0, channel_multiplier=0), writes=[io_i[:, :]])
    S.op("pool", lambda e: e.iota(tr_i.h[:, :], [[0, 1]], base=1 + 4 * NTOK, channel_multiplier=1), writes=[tr_i[:, :]])
    kb.copy("dve", tokf[:, :], toki[:, :])
    kb.copy("dve", io_f[:, :], io_i[:, :])
    kb.copy("dve", tr_f[:, :], tr_i[:, :])
    kb.ts("dve", tokp1[:, :], tokf[:, :], 1.0, ALU.add)
    pbanks = (F0, F1, M0, M1)
    for i in range(NTT):
        Pb = pbanks[i // 16]
        cs = slice((i % 16) * 32, (i % 16) * 32 + 32)
        for ip in range(i):
            kb.mm(Pb[:, cs], ones_b[:, :], mask_bf[:, ip, :], start=(ip == 0), stop=False)
        kb.mm(Pb[:, cs], SU[:, :], mask_bf[:, i, :], start=(i == 0), stop=True)
    for q_ in range(NTT // 16):
        S.op("dve", lambda e, q_=q_: e.tensor_copy(pos.h[:, q_ * 16:(q_ + 1) * 16, :],
                                                  pbanks[q_].h[:, :].rearrange("p (a b) -> p a b", a=16)),
             reads=[pbanks[q_][:, :]], writes=[pos[:, :, :]])
    for i in range(NTT):
        S.op("dve", lambda e, i=i: e.tensor_tensor_scan(slot.h[:, i, :], ones_f.h[:, 0:32], mask_all.h[:, i, :], 0.0,
                                                         ALU.mult, ALU.add),
             reads=[ones_f[:, :], mask_all[:, :, :]], writes=[slot[:, :, :]])
    kb.tt("dve", slot[:, :, :], slot[:, :, :], mask_all[:, :, :], ALU.subtract)
    for i in range(NTT):
        kb.ts("dve", pay[:, i, :, 0], mask_all[:, i, :], 0.0, ALU.mult, tokf[:, i:i + 1], ALU.add)
        kb.ts("dve", pay[:, i, :, 2], slot[:, i, :], float(NTOK), ALU.mult, tokp1[:, i:i + 1], ALU.add)
    kb.copy("dve", pay[:, :, :, 1], gate_all[:, :, :])
    n = 0
    for e_ in range(32):
        Pb = (M2, PO)[e_ % 2]
        kb.mm(Pb[:, 0:NJ * 3], zer[:, :], zer[:, 0:NJ * 3], start=True, stop=False)
        for i in range(NTT):
            sel = Sel[n % 3]
            n += 1
            kb.ts("dve", sel[:, :], io_f[:, :], pos[:, i, e_:e_ + 1], ALU.is_equal, mask_all[:, i, e_:e_ + 1], ALU.mult)
            for j in range(NJ):
                kb.mm(Pb[:, 3 * j:3 * j + 3], sel[:, j * 128:(j + 1) * 128], pay[:, i, e_, :],
                      start=False, stop=(i == NTT - 1 and j == NJ - 1))
        kb.copy("act", lst[:, e_, :], Pb[:, 0:NJ * 3])
    l4 = lst.h[:, :, :].rearrange("p e (j c) -> p e j c", c=3)
    iv = idx_tok.h[:, :].rearrange("p (e j) -> p e j", e=32)
    gv = gl.h[:, :].rearrange("p (e j) -> p e j", e=32)
    tv = tmpd.h[:, :].rearrange("p (e j) -> p e j", e=32)
    S.op("dve", lambda e: e.tensor_copy(iv, l4[:, :, :, 0]), reads=[lst[:, :, :]], writes=[idx_tok[:, :]])
    S.op("dve", lambda e: e.tensor_copy(gv, l4[:, :, :, 1]), reads=[lst[:, :, :]], writes=[gl[:, :]])
    S.op("dve", lambda e: e.tensor_scalar(tv, l4[:, :, :, 2], 0.0, tr_f.h[:, 0:1], ALU.is_equal, ALU.mult),
         reads=[lst[:, :, :], tr_f[:, :]], writes=[tmpd[:, :]])
    S.op("dve", lambda e: e.tensor_tensor(tv, tv, l4[:, :, :, 2], ALU.add),
         reads=[lst[:, :, :], tmpd[:, :]], writes=[tmpd[:, :]])
    kb.copy("dve", dest[:, :], tmpd[:, :])
    S.emit()
    pB.close()
    pR.close()

    pB = contextlib.ExitStack()
    kb.stack = pB
    stg = [kb.sb([128, 16, 512], F32, f"E_stg{i}") for i in range(2)]
    wb = [kb.sb([128, 16, 512], BF16, f"E_wb{i}") for i in range(2)]
    XgT = kb.sb([128, 16, CAP], BF16, "E_XgT")
    xg0 = kb.sb([128, 2048], F32, "E_xg0")
    xg = [xg0, xg0]
    actT = kb.sb([128, 16, CAP], BF16, "E_actT")
    Yp = [kb.sb([128, 512], F32, f"E_Y{j}") for j in range(2)]
    bgu = kb.sb([128, 1024], F32, "E_bgu")
    bdr = kb.sb([128, 512], F32, "E_bdr")
    bdb = [kb.sb([128, 512], BF16, f"E_bdb{i}") for i in range(2)]
    glu = kb.sb([128, 512], F32, "E_glu")
    lin = kb.sb([128, 512], F32, "E_lin")
    sg = kb.sb([128, 512], F32, "E_sg")
    kb.dma(bgu[:, :], V(io["bgu_col"], None), "c0")
    kb.memset("pool", bdr[:, :], 0.0)
    for i in range(2):
        kb.memset("pool", bdb[i][:, :], 0.0)
    w_gu, w_dn, b_dn = io["w_gu"], io["w_dn"], io["b_dn"]
    nblk = 0
    ny = 0
    gbanks = ((F0, F1), (M0, M1), (M2, PO))
    dbanks = (F0, F1, M0, M1, M2, PO)
    for e_ in range(n_exp):
        for j in range(NJ):
            col = e_ * NJ + j
            xs = 0
            S.dma("pool", lambda e, xs=xs, col=col: e.indirect_dma_start(
                xg[xs].h[:, :], None, h2s[:, :], bass.IndirectOffsetOnAxis(idx_tok.h[:, col:col + 1], 0)),
                f"xg{xs}", reads=[idx_tok[:, :], V(None, "h2s")], writes=[xg[xs][:, :]])
            for g in range(4):
                Tb = T0 if g % 2 == 0 else T1
                for jj in range(4):
                    kk = 4 * g + jj
                    kb.tr(Tb[:, jj * 128:(jj + 1) * 128], xg[xs][:, kk * 128:(kk + 1) * 128], ident[:, :])
                q = kb.evac_q()
                fn = (lambda e, Tb=Tb, g=g, j=j: e.copy(
                    XgT.h[:, 4 * g:4 * g + 4, j * 128:(j + 1) * 128], Tb.h[:, :].rearrange("p (a b) -> p a b", a=4))) \
                    if q == "act" else (lambda e, Tb=Tb, g=g, j=j: e.tensor_copy(
                        XgT.h[:, 4 * g:4 * g + 4, j * 128:(j + 1) * 128], Tb.h[:, :].rearrange("p (a b) -> p a b", a=4)))
                S.op(q, fn, reads=[Tb[:, :]], writes=[XgT[:, :, :]])
        for cb in range(8):
            bs = nblk % 2
            nblk += 1
            kb.dma(stg[bs][:, :, :], V(w_gu[e_ * D:(e_ + 1) * D, cb * 512:(cb + 1) * 512].rearrange("(k p) c -> p k c", p=128), None),
                   f"stg{bs}")
            sv = stg[bs].h[:, :, :].rearrange("p k (c two) -> p k c two", two=2)
            S.op("act", lambda e, bs=bs, sv=sv: e.copy(wb[bs].h[:, :, 0:256], sv[:, :, :, 0]),
                 reads=[stg[bs][:, :, :]], writes=[wb[bs][:, :, :]])
            S.op("pool", lambda e, bs=bs, sv=sv: e.tensor_copy(wb[bs].h[:, :, 256:512], sv[:, :, :, 1]),
                 reads=[stg[bs][:, :, :]], writes=[wb[bs][:, :, :]])
            for m in range(2):
                mc = cb * 2 + m
                bg = bgu[:, e_ * 32 + mc:e_ * 32 + mc + 1]
                bl = bgu[:, e_ * 32 + 16 + mc:e_ * 32 + 16 + mc + 1]
                for gi, (r0, rn) in enumerate(RGS):
                    Pg, Pl = gbanks[gi]
                    for kk in range(16):
                        kb.mm(Pg[:, 0:rn], wb[bs][:, kk, m * 128:(m + 1) * 128], XgT[:, kk, r0:r0 + rn],
                              start=(kk == 0), stop=(kk == 15))
                    for kk in range(16):
                        kb.mm(Pl[:, 0:rn], wb[bs][:, kk, 256 + m * 128:256 + (m + 1) * 128], XgT[:, kk, r0:r0 + rn],
                              start=(kk == 0), stop=(kk == 15))
                    kb.ts("dve", glu[:, 0:rn], Pg[:, 0:rn], bg, ALU.add, 7.0, ALU.min)
                    kb.ts("dve", lin[:, 0:rn], Pl[:, 0:rn], bl, ALU.add, 7.0, ALU.min)
                    kb.ts("dve", lin[:, 0:rn], lin[:, 0:rn], -7.0, ALU.max, 1.0, ALU.add)
                    kb.act(sg[:, 0:rn], glu[:, 0:rn], AF.Sigmoid, scale=1.702)
                    kb.tt("dve", glu[:, 0:rn], glu[:, 0:rn], sg[:, 0:rn], ALU.mult)
                    kb.tt("dve", actT[:, mc, r0:r0 + rn], glu[:, 0:rn], lin[:, 0:rn], ALU.mult)
        for nb in range(4):
            bs = nblk % 2
            nblk += 1
            kb.dma(stg[bs][:, :, :], V(w_dn[e_ * D:(e_ + 1) * D, nb * 512:(nb + 1) * 512].rearrange("(k p) c -> p k c", p=128), None),
                   f"stg{bs}")
            S.op("act", lambda e, bs=bs: e.copy(wb[bs].h[:, 0:8, :], stg[bs].h[:, 0:8, :]),
                 reads=[stg[bs][:, :, :]], writes=[wb[bs][:, :, :]])
            S.op("pool", lambda e, bs=bs: e.tensor_copy(wb[bs].h[:, 8:16, :], stg[bs].h[:, 8:16, :]),
                 reads=[stg[bs][:, :, :]], writes=[wb[bs][:, :, :]])
            ds_ = nb % 2
            kb.dma(bdr[0:1, :], V(b_dn[e_:e_ + 1, nb * 512:(nb + 1) * 512], None), "bd")
            kb.copy("pool", bdb[ds_][0:1, :], bdr[0:1, :])
            yb = ybufs[nb]
            for j in range(NJ):
                Pb = dbanks[j % 6]
                col = e_ * NJ + j
                for kk in range(16):
                    kb.mm(Pb[:, :], actT[:, kk, j * 128:(j + 1) * 128], wb[bs][:, kk, :], start=(kk == 0), stop=False)
                kb.mm(Pb[:, :], ones_b[:, :], bdb[ds_][:, :], start=False, stop=True)
                ys_ = ny % 2
                ny += 1
                kb.ts("dve", Yp[ys_][:, :], Pb[:, :], gl[:, col:col + 1], ALU.mult)
                S.dma("pool", lambda e, ys_=ys_, col=col, yb=yb: e.indirect_dma_start(
                    yb[:, :], bass.IndirectOffsetOnAxis(dest.h[:, col:col + 1], 0), Yp[ys_].h[:, :], None),
                    f"ys{ys_}", reads=[dest[:, :], Yp[ys_][:, :]], writes=[V(None, "ybuf")])
    S.emit()
    pB.close()

    pB = contextlib.ExitStack()
    kb.stack = pB
    g_rep = kb.sb([128, 2048], F32, "F_g")
    b_rep = kb.sb([128, 2048], F32, "F_b")
    ys = [[kb.sb([128, 2048], F32, f"F_ys{i}_{s_}") for s_ in range(4)] for i in range(2)]
    hh2 = [kb.sb([128, 2048], F32, f"F_h{i}") for i in range(2)]
    yt = [kb.sb([128, 2048], F32, f"F_y{i}") for i in range(2)]
    stats = kb.sb([128, 24], F32, "F_stats")
    mv = kb.sb([128, 2], F32, "F_mv")
    rr = kb.sb([128, 1], F32, "F_r")
    kb.dma(g_rep[:, :], V(io["ln3g"], None), "c0")
    kb.dma(b_rep[:, :], V(io["ln3b"], None), "c1")
    for i in range(NTT):
        sl = i % 2
        kb.dma(hh2[sl][:, :], V(h2s[i * 128:(i + 1) * 128, :], "h2s"), f"fh{sl}")
        for s_ in range(4):
            r0 = 1 + s_ * NTOK + i * 128
            for nb in range(4):
                kb.dma(ys[sl][s_][:, nb * 512:(nb + 1) * 512], V(ybufs[nb][r0:r0 + 128, :], "ybuf"), f"fy{sl}_{s_}")
        kb.tt("dve", yt[sl][:, :], ys[sl][0][:, :], ys[sl][1][:, :], ALU.add)
        kb.tt("pool", ys[sl][2][:, :], ys[sl][2][:, :], ys[sl][3][:, :], ALU.add)
        kb.tt("dve", yt[sl][:, :], yt[sl][:, :], ys[sl][2][:, :], ALU.add)
        kb.stt(yt[sl][:, :], hh2[sl][:, :], ALPHA, yt[sl][:, :], ALU.mult, ALU.add)
        _ln_tile(kb, yt[sl], g_rep, b_rep, yt[sl], stats, mv, rr)
        kb.dma(V(out_ap[i * 128:(i + 1) * 128, :], None), yt[sl][:, :], f"fo{sl}")
    S.emit()
    pB.close()


def _set_cfg(npb):
    global NPB, NPRE, NTT, NTOK, CAP, NJ, RGS, YROWS
    NPB = npb
    NTT = NT_SEQ // npb
    NPRE = NT_SEQ - NTT
    NTOK = NTT * 128
    CAP, RGS = {1: (1280, ((0, 512), (512, 512), (1024, 256))), 2: (640, ((0, 512), (512, 128))),
                4: (384, ((0, 384),))}[npb]
    NJ = CAP // 128
    YROWS = 1 + 4 * NTOK + 128


def build_program(mode="full", dbg_tiles=None, n_exp=32, ng=4, npb=2, npre_dbg=None):
    global NPRE
    _set_cfg(npb)
    if npre_dbg is not None:
        NPRE = npre_dbg
    nc = bass.Bass("TRN2", target_bir_lowering=False)
    stack = contextlib.ExitStack()
    nt = dbg_tiles or NT_SEQ
    dbg = (mode == "A")
    doA = mode in ("full", "A")
    doB = mode in ("full", "BCD")
    NG = ng

    def din(name, shape, dt=F32):
        return nc.dram_tensor(name, list(shape), dt, kind="ExternalInput").ap()

    x_b = din("x_b", [SEQ, D])
    if doA:
        keep_in = din("keep_rep", [128, NT_SEQ])
        w_h_all = din("w_h", [NG * D, 2560])
        bcol_h_all = din("bcol_h", [NG * 128, 8])
        brow_h_all = din("brow_h", [NG, 1536])
        lbl_all = din("lbl", [NG * 128, 8])
        hng_rep = din("hng_rep", [128, 512])
        w_g_all = din("w_g", [NG * D, 2064])
        bcol_g_all = din("bcol_g", [NG * 128, 8])
        brow_g_all = din("brow_g", [NG, 1536])
        wa2_all = din("wa2", [NG * 16, 256])
        gng_rep = din("gng_rep", [128, 512])
        if dbg:
            gh_out = nc.dram_tensor("gh_out", [SEQ, 512], F32, kind="ExternalOutput").ap()
            mg_all = nc.dram_tensor("mg_out", [SEQ, NG * 512], F32, kind="ExternalOutput").ap()
        else:
            gh_out = nc.dram_tensor("gh_scr", [SEQ, 512], F32).ap()
            mg_all = nc.dram_tensor("merged", [NTOK, D], F32).ap()
    io = {}
    if doB:
        if mode == "BCD":
            io["merged"] = din("merged", [NTOK, D])
        else:
            io["merged"] = mg_all
        io["x_tok"] = x_b[NPRE * 128:, :]
        for nm in ("ln1g", "ln1b", "ln2g", "ln2b", "ln3g", "ln3b"):
            io[nm] = din(nm, [128, D])
        io["w_o"] = din("w_o", [D, D])
        io["w_kv"] = din("w_kv", [D, 2 * D])
        io["w_xq"] = din("w_xq", [D, D])
        io["w_xo"] = din("w_xo", [D, D])
        io["mem_b"] = din("mem_b", [256, D])
        io["w_router"] = din("w_router", [D, 32])
        io["br_rep"] = din("br_rep", [128, 32])
        io["bgu_col"] = din("bgu_col", [128, 1024])
        io["w_gu"] = din("w_gu", [32 * D, 2 * D])
        io["w_dn"] = din("w_dn", [32 * D, D])
        io["b_dn"] = din("b_dn", [32, D])
        io["out"] = nc.dram_tensor("out", [NTOK, D], F32, kind="ExternalOutput").ap()
        if mode == "BCD":
            io["h1s"] = nc.dram_tensor("h1s", [NTOK, D], F32, kind="ExternalOutput").ap()
            io["h2s"] = nc.dram_tensor("h2s", [NTOK, D], F32, kind="ExternalOutput").ap()
        else:
            io["h1s"] = nc.dram_tensor("h1s", [NTOK, D], F32).ap()
            io["h2s"] = nc.dram_tensor("h2s", [NTOK, D], F32).ap()
        io["ybuf"] = [nc.dram_tensor(f"ybuf{nb}", [YROWS, 512], F32).ap() for nb in range(4)]
        io["n_exp"] = n_exp

    with stack:
        S = Sched(nc, stack)
        kb = K(nc, S, stack)
        C = _consts(kb)
        ident, cmask, ones_b = C["ident"], C["cmask"], C["ones_b"]

        T0, T1 = kb.ps("pT0"), kb.ps("pT1")
        F0, F1 = kb.ps("pF0"), kb.ps("pF1")
        M0, M1, M2 = kb.ps("pM0"), kb.ps("pM1"), kb.ps("pM2")
        PO = kb.ps("pO")

        if doA:
            pA = contextlib.ExitStack()
            kb.stack = pA
            for hh in range(NG):
                keep = kb.sb([128, NT_SEQ], F32, "keep")
                if hh == 0:
                    kb.dma(keep[:, :], V(keep_in, None), "ck")
                w_h = w_h_all[hh * D:(hh + 1) * D, :]
                bcol_h = bcol_h_all[hh * 128:(hh + 1) * 128, :]
                brow_h = brow_h_all[hh:hh + 1, :]
                lbl = lbl_all[hh * 128:(hh + 1) * 128, :]
                w_g = w_g_all[hh * D:(hh + 1) * D, :]
                bcol_g = bcol_g_all[hh * 128:(hh + 1) * 128, :]
                brow_g = brow_g_all[hh:hh + 1, :]
                wa2_in = wa2_all[hh * 16:(hh + 1) * 16, :]
                mg_out = mg_all[:, hh * 512:(hh + 1) * 512]
                w_sb = kb.sb([128, 16, 2560], BF16, "w_sb")
                stage = [kb.sb([128, 2560], F32, f"wstage{i}") for i in range(2)]
                xt = [kb.sb([128, D], F32, f"xt{i}") for i in range(2)]
                xT = [kb.sb([128, 16 * 128], BF16, f"xT{i}") for i in range(2)]
                bcol = kb.sb([128, 8], F32, "bcol")
                brow = kb.sb([128, 1536], F32, "brow")
                brow_b = kb.sb([128, 1536], BF16, "brow_b")
                lb_in = kb.sb([128, 8], F32, "lb_in")
                lbv = kb.sb([128, 4], F32, "lbv")
                omlb = kb.sb([128, 4], F32, "omlb")
                hng = kb.sb([128, 512], F32, "hng")

                kb.dma(bcol[:, :], V(bcol_h, None), "c0")
                kb.memset("dve", brow[:, :], 0.0)
                kb.dma(brow[0:1, :], V(brow_h, None), "c1")
                kb.dma(lb_in[:, :], V(lbl, None), "c2")
                kb.dma(hng[:, :], V(hng_rep, None), "c3")
                kb.copy("dve", brow_b[:, :], brow[:, :])
                kb.tt("dve", lbv[:, :], lb_in[:, 0:4], lb_in[:, 4:8], ALU.subtract)
                kb.act(lbv[:, :], lbv[:, :], AF.Sigmoid)
                kb.ts("dve", omlb[:, :], lbv[:, :], -1.0, ALU.mult, 1.0, ALU.add)

                _load_weights_bf16(kb, w_h, 2560, w_sb, stage, "ws")

                def wt(name, shape, dt=F32):
                    return kb.sb(shape, dt, name)
                qT = wt("qT", [128, 512])
                fT = wt("fT", [128, 512])
                kTt = wt("kTt", [128, 512])
                cum = wt("cum", [128, 512])
                e1 = wt("e1", [128, 512])
                e2 = wt("e2", [128, 512])
                mid = wt("mid", [128, 8])
                sc = wt("sc", [128, 8])
                Qt = wt("Qt", [128, 512], BF16)
                Qc = wt("Qc", [128, 512], BF16)
                Kt = wt("Kt", [128, 512], BF16)
                KlT = wt("KlT", [128, 512])
                Kl = wt("Kl", [128, 512], BF16)
                Vb = wt("Vb", [128, 512], BF16)
                AT = wt("AT", [128, 512], BF16)
                St = wt("St", [128, 512])
                Sb = wt("Sb", [128, 512], BF16)
                gate = wt("gate", [128, 512])
                g2 = wt("g2", [128, 512])
                osq = wt("osq", [128, 512])
                ss = wt("ss", [128, 4])
                rstd = wt("rstd", [128, 4])
                ot = [wt(f"ot{i}", [128, 512]) for i in range(2)]
                onesf = C["ones_f"]

                kb.memset("dve", St[:, :], 0.0)
                kb.memset("dve", Sb[:, :], 0.0)

                kb.dma(xt[0][:, :], V(x_b[0:128, :], None), "x0")
                for t in range(nt):
                    sl = t % 2
                    full = (t >= NPRE)
                    if t + 1 < nt:
                        kb.dma(xt[1 - sl][:, :], V(x_b[(t + 1) * 128:(t + 2) * 128, :], None), f"x{1 - sl}")
                    for g in range(4):
                        Tb = T0 if g % 2 == 0 else T1
                        for j in range(4):
                            kk = 4 * g + j
                            kb.tr(Tb[:, j * 128:(j + 1) * 128], xt[sl][:, kk * 128:(kk + 1) * 128], ident[:, :])
                        src = V(Tb.h[:, :], None)
                        dst = xT[sl].k(g)[:, g * 512:(g + 1) * 512]
                        q = kb.evac_q()
                        rd = [Tb[:, 0:1] for j in range(4)]
                        if q == "act":
                            S.op("act", lambda e, d=dst, s=src: e.copy(d.ap, s.ap), reads=rd, writes=[dst])
                        else:
                            S.op("dve", lambda e, d=dst, s=src: e.tensor_copy(d.ap, s.ap), reads=rd, writes=[dst])
                    xTr = [xT[sl].k(kk // 4)[:, kk * 128:(kk + 1) * 128] for kk in range(16)]

                    for which, Fb in ((0, F0), (1, F1)):
                        if which == 0 and not full:
                            continue
                        for h in range(4):
                            c0 = which * 512 + h * 128
                            for kk in range(16):
                                kb.mm(Fb[:, h * 128:(h + 1) * 128], w_sb.k(kk)[:, kk, c0:c0 + 128], xTr[kk],
                                      start=(kk == 0), stop=(kk == 15))
                    for n, Mb in enumerate((M0, M1, M2)):
                        if n > 0 and not full:
                            continue
                        c0 = 1024 + n * 512
                        for kk in range(16):
                            kb.mm(Mb[:, :], xTr[kk], w_sb.k(kk)[:, kk, c0:c0 + 512], start=(kk == 0), stop=False)
                        kb.mm(Mb[:, :], ones_b[:, :], brow_b[:, n * 512:(n + 1) * 512], start=False, stop=True)

                    for h in range(4):
                        cs = slice(h * 128, (h + 1) * 128)
                        if full:
                            kb.act(qT[:, cs], F0[:, cs], AF.Silu, bias=bcol[:, h:h + 1])
                        kb.act(fT[:, cs], F1[:, cs], AF.Sigmoid, bias=bcol[:, 4 + h:5 + h])
                        kb.ts("dve", fT[:, cs], fT[:, cs], omlb[:, h:h + 1], ALU.mult, lbv[:, h:h + 1], ALU.add)
                        kb.ts("dve", kTt[:, cs], fT[:, cs], -1.0, ALU.mult, 1.0, ALU.add)
                    kb.act(fT[:, :], fT[:, :], AF.Ln)
                    for h in range(4):
                        cs = slice(h * 128, (h + 1) * 128)
                        S.op("dve", lambda e, cs=cs: e.tensor_tensor_scan(cum.h[:, cs], onesf.h[:, :], fT.h[:, cs], 0.0,
                                                                            ALU.mult, ALU.add),
                             reads=[onesf[:, :], fT[:, cs]], writes=[cum[:, cs]])
                        c63 = h * 128 + 63
                        cl = h * 128 + 127
                        kb.ts("dve", mid[:, 2 * h:2 * h + 1], cum[:, c63:c63 + 1], -1.0, ALU.mult)
                        if full:
                            kb.act(e1[:, cs], cum[:, cs], AF.Exp, bias=mid[:, 2 * h:2 * h + 1])
                        kb.act(e2[:, cs], cum[:, cs], AF.Exp, bias=cum[:, c63:c63 + 1], scale=-1.0)
                        if full:
                            kb.act(sc[:, 2 * h:2 * h + 1], cum[:, c63:c63 + 1], AF.Exp)
                        kb.act(sc[:, 2 * h + 1:2 * h + 2], cum[:, cl:cl + 1], AF.Exp, bias=mid[:, 2 * h:2 * h + 1])
                        kb.act(mid[:, 2 * h + 1:2 * h + 2], cum[:, cl:cl + 1], AF.Exp)
                        if full:
                            kb.tt("dve", Qt[:, cs], qT[:, cs], e1[:, cs], ALU.mult)
                            kb.ts("dve", Qc[:, cs], Qt[:, cs], sc[:, 2 * h:2 * h + 1], ALU.mult)
                        kb.tt("dve", Kt[:, cs], kTt[:, cs], e2[:, cs], ALU.mult)
                        kb.ts("dve", KlT[:, cs], Kt[:, cs], sc[:, 2 * h + 1:2 * h + 2], ALU.mult)
                    kb.copy("act", Vb[:, :], M0[:, :])
                    for h in range(4 if full else 0):
                        cs = slice(h * 128, (h + 1) * 128)
                        kb.mm(F0[:, cs], Kt[:, cs], Qt[:, cs], start=True, stop=True)
                        kb.tt("dve", AT[:, cs], F0[:, cs], cmask[:, :], ALU.mult)
                    for h in range(4):
                        cs = slice(h * 128, (h + 1) * 128)
                        kb.tr(F1[:, cs], KlT[:, cs], ident[:, :])
                        kb.copy("act", Kl[:, cs], F1[:, cs])
                    for h in range(4 if full else 0):
                        cs = slice(h * 128, (h + 1) * 128)
                        kb.mm(PO[:, cs], AT[:, cs], Vb[:, cs], start=True, stop=False)
                        kb.mm(PO[:, cs], Qc[:, cs], Sb[:, cs], start=False, stop=True)
                    for h in range(4):
                        cs = slice(h * 128, (h + 1) * 128)
                        kb.mm(M0[:, cs], Kl[:, cs], Vb[:, cs], start=True, stop=True)
                    for h in range(4):
                        cs = slice(h * 128, (h + 1) * 128)
                        kb.stt(St[:, cs], St[:, cs], mid[:, 2 * h + 1:2 * h + 2], M0[:, cs], ALU.mult, ALU.add)
                    if not full:
                        kb.ts("dve", St[:, :], St[:, :], keep[:, t:t + 1], ALU.mult)
                    kb.copy("act", Sb[:, :], St[:, :])
                    if not full:
                        continue
                    kb.act(gate[:, :], M1[:, :], AF.Sigmoid)
                    kb.act(g2[:, :], M2[:, :], AF.Sigmoid)
                    kb.tt("dve", gate[:, :], gate[:, :], g2[:, :], ALU.mult)
                    kb.tt("pool", gate[:, :], gate[:, :], hng[:, :], ALU.mult)
                    for h in range(4):
                        cs = slice(h * 128, (h + 1) * 128)
                        kb.act(osq[:, cs], PO[:, cs], AF.Square, accum=ss[:, h:h + 1])
                    kb.ts("dve", rstd[:, :], ss[:, :], 1.0 / 128.0, ALU.mult, EPS, ALU.add)
                    kb.act(rstd[:, :], rstd[:, :], AF.Sqrt)
                    S.op("dve", lambda e: e.reciprocal(rstd.h[:, :], rstd.h[:, :]), reads=[rstd[:, :]], writes=[rstd[:, :]])
                    o_t = ot[sl]
                    for h in range(4):
                        cs = slice(h * 128, (h + 1) * 128)
                        kb.stt(o_t[:, cs], PO[:, cs], rstd[:, h:h + 1], gate[:, cs], ALU.mult, ALU.mult)
                    kb.dma(V(gh_out[(t - NPRE) * 128:(t - NPRE + 1) * 128, :], f"gh{t}"), o_t[:, :], f"o{sl}")

                bcg = kb.sb([128, 8], F32, "bcg")
                wa2 = kb.sb([16, 256], F32, "wa2_sb")
                gng = kb.sb([128, 512], F32, "gng")
                ga1T = kb.sb([16, 128], F32, "ga1T")
                ght = [kb.sb([128, 512], F32, f"ght{i}") for i in range(2)]
                kb.dma(bcg[:, :], V(bcol_g, None), "c0")
                kb.memset("dve", brow[:, :], 0.0)
                kb.dma(brow[0:1, :], V(brow_g, None), "c1")
                kb.dma(wa2[:, :], V(wa2_in, None), "c2")
                kb.dma(gng[:, :], V(gng_rep, None), "c3")
                kb.copy("dve", brow_b[:, :], brow[:, :])
                kb.ts("dve", bcg[:, 0:2], bcg[:, 0:2], 1.0 / 16.0, ALU.mult)
                kb.ts("dve", bcg[:, 5:7], bcg[:, 5:7], -1.0, ALU.mult)
                _load_weights_bf16(kb, w_g, 2064, w_sb, stage, "ws")
                kb.memset("dve", St[:, :], 0.0)
                kb.memset("dve", Sb[:, :], 0.0)
                St2 = kb.sb([128, 512], F32, "St2")
                Sb2 = kb.sb([128, 512], BF16, "Sb2")
                kb.memset("dve", St2[:, :], 0.0)
                kb.memset("dve", Sb2[:, :], 0.0)
                Sts, Sbs = (St, St2), (Sb, Sb2)

                kb.dma(xt[0][:, :], V(x_b[0:128, :], None), "x0")
                for t in range(nt):
                    sl = t % 2
                    full = (t >= NPRE)
                    if t + 1 < nt:
                        kb.dma(xt[1 - sl][:, :], V(x_b[(t + 1) * 128:(t + 2) * 128, :], None), f"x{1 - sl}")
                    if full:
                        kb.dma(ght[sl][:, :], V(gh_out[(t - NPRE) * 128:(t - NPRE + 1) * 128, :], f"gh{t}"), f"g{sl}")
                    for g in range(4):
                        Tb = T0 if g % 2 == 0 else T1
                        for j in range(4):
                            kk = 4 * g + j
                            kb.tr(Tb[:, j * 128:(j + 1) * 128], xt[sl][:, kk * 128:(kk + 1) * 128], ident[:, :])
                        dst = xT[sl].k(g)[:, g * 512:(g + 1) * 512]
                        kb.copy(kb.evac_q(), dst, Tb[:, :])
                    xTr = [xT[sl].k(kk // 4)[:, kk * 128:(kk + 1) * 128] for kk in range(16)]
                    for u in range(4):
                        if u < 2 and not full:
                            continue
                        for kk in range(16):
                            kb.mm(F0[:, u * 128:(u + 1) * 128], w_sb.k(kk)[:, kk, u * 128:(u + 1) * 128], xTr[kk],
                                  start=(kk == 0), stop=(kk == 15))
                    for kk in range(16):
                        kb.mm(F1[0:16, 0:128], w_sb.k(kk)[:, kk, 512:528], xTr[kk], start=(kk == 0), stop=(kk == 15))
                    for n, Mb in enumerate((M0, M1, M2)):
                        if n > 0 and not full:
                            continue
                        c0 = 528 + n * 512
                        for kk in range(16):
                            kb.mm(Mb[:, :], xTr[kk], w_sb.k(kk)[:, kk, c0:c0 + 512], start=(kk == 0), stop=False)
                        kb.mm(Mb[:, :], ones_b[:, :], brow_b[:, n * 512:(n + 1) * 512], start=False, stop=True)
                    kb.act(ga1T[:, :], F1[0:16, 0:128], AF.Identity, bias=bcg[0:16, 4:5])
                    for u in range(2):
                        kb.mm(F1[:, 128 + u * 128:256 + u * 128], wa2[:, u * 128:(u + 1) * 128], ga1T[:, :],
                              start=True, stop=True)
                    for u in range(2):
                        cs = slice(u * 128, (u + 1) * 128)
                        if full:
                            kb.act(qT[:, cs], F0[:, cs], AF.Identity, bias=bcg[:, u:u + 1], scale=1.0 / 16.0)
                        kb.act(kTt[:, cs], F0[:, 256 + u * 128:384 + u * 128], AF.Identity, bias=bcg[:, 2 + u:3 + u])
                        kb.act(fT[:, cs], F1[:, 128 + u * 128:256 + u * 128], AF.Exp, bias=bcg[:, 5 + u:6 + u], scale=-1.0)
                    kb.act(fT[:, 0:256], fT[:, 0:256], AF.Ln, bias=1.0)
                    kb.ts("dve", fT[:, 0:256], fT[:, 0:256], -1.0 / 16.0, ALU.mult)
                    for h in range(2):
                        cs = slice(h * 128, (h + 1) * 128)
                        S.op("dve", lambda e, cs=cs: e.tensor_tensor_scan(cum.h[:, cs], onesf.h[:, :], fT.h[:, cs], 0.0,
                                                                            ALU.mult, ALU.add),
                             reads=[onesf[:, :], fT[:, cs]], writes=[cum[:, cs]])
                        c63 = h * 128 + 63
                        cl = h * 128 + 127
                        kb.ts("dve", mid[:, 2 * h:2 * h + 1], cum[:, c63:c63 + 1], -1.0, ALU.mult)
                        if full:
                            kb.act(e1[:, cs], cum[:, cs], AF.Exp, bias=mid[:, 2 * h:2 * h + 1])
                        kb.act(e2[:, cs], cum[:, cs], AF.Exp, bias=cum[:, c63:c63 + 1], scale=-1.0)
                        if full:
                            kb.act(sc[:, 2 * h:2 * h + 1], cum[:, c63:c63 + 1], AF.Exp)
                        kb.act(sc[:, 2 * h + 1:2 * h + 2], cum[:, cl:cl + 1], AF.Exp, bias=mid[:, 2 * h:2 * h + 1])
                        kb.act(mid[:, 2 * h + 1:2 * h + 2], cum[:, cl:cl + 1], AF.Exp)
                        if full:
                            kb.tt("dve", Qt[:, cs], qT[:, cs], e1[:, cs], ALU.mult)
                            kb.ts("dve", Qc[:, cs], Qt[:, cs], sc[:, 2 * h:2 * h + 1], ALU.mult)
                        kb.tt("dve", Kt[:, cs], kTt[:, cs], e2[:, cs], ALU.mult)
                        kb.ts("dve", KlT[:, cs], Kt[:, cs], sc[:, 2 * h + 1:2 * h + 2], ALU.mult)
                    kb.copy("act", Vb[:, :], M0[:, :])
                    for u in range(2 if full else 0):
                        cs = slice(u * 128, (u + 1) * 128)
                        kb.mm(F0[:, 0:128], Kt[:, cs], Qt[:, cs], start=(u == 0), stop=(u == 1))
                    if full:
                        kb.tt("dve", AT[:, 0:128], F0[:, 0:128], cmask[:, :], ALU.mult)
                    for u in range(2):
                        cs = slice(u * 128, (u + 1) * 128)
                        kb.tr(F1[:, cs], KlT[:, cs], ident[:, :])
                    kb.copy("act", Kl[:, 0:256], F1[:, 0:256])
                    if full:
                        kb.mm(PO[:, :], AT[:, 0:128], Vb[:, :], start=True, stop=False)
                    for u in range(2 if full else 0):
                        cs = slice(u * 128, (u + 1) * 128)
                        kb.mm(PO[:, :], Qc[:, cs], Sbs[u][:, :], start=False, stop=(u == 1))
                    for u, Pb in enumerate((M0, F0)):
                        cs = slice(u * 128, (u + 1) * 128)
                        kb.mm(Pb[:, :], Kl[:, cs], Vb[:, :], start=True, stop=True)
                    for u, Pb in enumerate((M0, F0)):
                        kb.stt(Sts[u][:, :], Sts[u][:, :], mid[:, 2 * u + 1:2 * u + 2], Pb[:, :], ALU.mult, ALU.add)
                        if not full:
                            kb.ts("dve", Sts[u][:, :], Sts[u][:, :], keep[:, t:t + 1], ALU.mult)
                        kb.copy("act", Sbs[u][:, :], Sts[u][:, :])
                    if not full:
                        continue
                    kb.act(gate[:, :], M1[:, :], AF.Silu)
                    kb.act(g2[:, :], M2[:, :], AF.Sigmoid)
                    kb.tt("dve", gate[:, :], gate[:, :], g2[:, :], ALU.mult)
                    kb.tt("pool", gate[:, :], gate[:, :], gng[:, :], ALU.mult)
                    kb.act(osq[:, :], PO[:, :], AF.Square, accum=ss[:, 0:1])
                    kb.ts("dve", rstd[:, 0:1], ss[:, 0:1], 1.0 / 512.0, ALU.mult, EPS, ALU.add)
                    kb.act(rstd[:, 0:1], rstd[:, 0:1], AF.Sqrt)
                    S.op("dve", lambda e: e.reciprocal(rstd.h[:, 0:1], rstd.h[:, 0:1]), reads=[rstd[:, :]], writes=[rstd[:, :]])
                    o_t = ot[sl]
                    kb.stt(o_t[:, :], PO[:, :], rstd[:, 0:1], gate[:, :], ALU.mult, ALU.mult)
                    kb.tt("dve", o_t[:, :], o_t[:, :], ght[sl][:, :], ALU.add)
                    kb.dma(V(mg_out[(t - NPRE) * 128:(t - NPRE + 1) * 128, :], "merged"), o_t[:, :], f"o{sl}")
            S.emit()
            pA.close()
        if doB:
            phases_bcd(nc, S, kb, C, (T0, T1, F0, F1, M0, M1, M2, PO), io, dbg)
    return nc


def _prep_core_inputs(inp, c):
    b, hh = c // 4, c % 4
    w_in = inp["w_in"][0]
    b_in = inp["b_in"][0]
    offs = np.cumsum([0, 1024, 1024, 2048, 2048, 16, 2048, 2048, 2048, 2048, 2048, 2048])
    o_gq, o_gk, o_gv, o_gr, o_ga1, o_hq, o_hf, o_hi, o_hg, o_ma, o_mb = offs[:11]
    ch = slice(512 * hh, 512 * hh + 512)

    def cols(o, s):
        return np.arange(o + s.start, o + s.stop)
    hcols = np.concatenate([cols(o_hq, ch), cols(o_hf, ch), cols(o_hi, ch), cols(o_hg, ch), cols(o_mb, ch)])
    w_h = np.ascontiguousarray(w_in[:, hcols])
    bh = b_in[hcols]
    bcol_h = np.ascontiguousarray(bh[:1024].reshape(8, 128).T)
    brow_h = np.ascontiguousarray(bh[1024:].reshape(1, 1536))
    lb = inp["hgrn_lb_logits"][:, ch]
    lbl = np.ascontiguousarray(np.concatenate([lb[0].reshape(4, 128).T, lb[1].reshape(4, 128).T], axis=1))
    hng_rep = np.ascontiguousarray(np.broadcast_to(np.tile(inp["hgrn_norm_g"][0], 4)[None, :], (128, 512)))
    kq = slice(256 * hh, 256 * hh + 256)
    gcols = np.concatenate([cols(o_gq, kq), cols(o_gk, kq), np.arange(o_ga1, o_ga1 + 16), cols(o_gv, ch),
                            cols(o_gr, ch), cols(o_ma, ch)])
    w_g = np.ascontiguousarray(w_in[:, gcols])
    bg = b_in[gcols]
    bcol_g = np.zeros((128, 8), np.float32)
    bcol_g[:, 0:4] = bg[:512].reshape(4, 128).T
    bcol_g[:16, 4] = bg[512:528]
    bcol_g[:, 5:7] = inp["b_gla_a"][0][kq].reshape(2, 128).T
    brow_g = np.ascontiguousarray(bg[528:].reshape(1, 1536))
    wa2 = np.ascontiguousarray(inp["w_gla_a2"][0][:, kq])
    gng_rep = np.ascontiguousarray(np.broadcast_to(inp["gla_norm_g"][0][None, :], (128, 512)))
    return dict(x_b=np.ascontiguousarray(inp["x"][b]), w_h=w_h, bcol_h=bcol_h, brow_h=brow_h, lbl=lbl,
                hng_rep=hng_rep, w_g=w_g, bcol_g=bcol_g, brow_g=brow_g, wa2=wa2, gng_rep=gng_rep)


def _prep_a_inputs(inp, b, ng=4, j=0, npb=1):
    parts = [_prep_core_inputs(inp, 4 * b + hh) for hh in range(ng)]
    ntok = SEQ // npb
    own_end = (j + 1) * ntok
    start = own_end - SEQ
    xw = np.zeros((SEQ, D), np.float32)
    xw[max(0, -start):] = inp["x"][b, max(0, start):own_end]
    keep = ((start + 128 * np.arange(NT_SEQ)) >= 0).astype(np.float32)
    out = dict(x_b=xw, hng_rep=parts[0]["hng_rep"], gng_rep=parts[0]["gng_rep"],
               keep_rep=np.ascontiguousarray(np.broadcast_to(keep[None, :], (128, NT_SEQ))))
    for k in ("w_h", "bcol_h", "brow_h", "lbl", "w_g", "bcol_g", "brow_g", "wa2"):
        out[k] = np.ascontiguousarray(np.concatenate([p[k] for p in parts], axis=0))
    return out


def _prep_bcd_inputs(inp, b):
    def rep(v):
        return np.ascontiguousarray(np.broadcast_to(v[None, :], (128, v.shape[0])))
    bgu = inp["b_gate_up"][0]
    bg = bgu[:, 0::2].reshape(32, 16, 128)
    bl = bgu[:, 1::2].reshape(32, 16, 128)
    bgu_col = np.ascontiguousarray(np.concatenate([bg, bl], axis=1).transpose(2, 0, 1).reshape(128, 1024))
    return dict(
        x_b=np.ascontiguousarray(inp["x"][b]),
        ln1g=rep(inp["ln1_g"][0]), ln1b=rep(inp["ln1_b"][0]), ln2g=rep(inp["ln2_g"][0]), ln2b=rep(inp["ln2_b"][0]),
        ln3g=rep(inp["ln3_g"][0]), ln3b=rep(inp["ln3_b"][0]),
        w_o=inp["w_mix_o"][0], w_kv=inp["w_mem_kv"][0], w_xq=inp["w_xq"][0], w_xo=inp["w_xo"][0],
        mem_b=np.ascontiguousarray(inp["mem"][b]), w_router=inp["w_router"][0], br_rep=rep(inp["b_router"][0]),
        bgu_col=bgu_col, b_dn=inp["b_down"][0],
        w_gu=inp["w_gate_up"][0].reshape(32 * D, 2 * D), w_dn=inp["w_down"][0].reshape(32 * D, D))


NPB_FULL = 2


def kernel(**inputs):
    inp = {k: np.asarray(v) for k, v in inputs.items()}
    npb = NPB_FULL
    nc = build_program("full", npb=npb)
    in_maps = []
    for b in range(2):
        bcd = _prep_bcd_inputs(inp, b)
        for j in range(npb):
            m = dict(bcd)
            m.update(_prep_a_inputs(inp, b, 4, j, npb))
            in_maps.append(m)
    n = 2 * npb
    res = run_bass_kernel_spmd(nc, in_maps, core_ids=list(range(n)))
    out = np.stack([np.asarray(r["out"]) for r in res.results], axis=0)
    return out.reshape(2, SEQ, D).astype(np.float32)
```

```python
import contextlib
import numpy as np
import concourse.bass as bass
import concourse.mybir as mybir
from concourse.bass_utils import run_bass_kernel_spmd

F32 = mybir.dt.float32
BF16 = mybir.dt.bfloat16
I32 = mybir.dt.int32
U32 = mybir.dt.uint32
AF = mybir.ActivationFunctionType
ALU = mybir.AluOpType
AX = mybir.AxisListType

D = 2048
SEQ = 8192
NT_SEQ = SEQ // 128
EPS = 1e-5
ALPHA = 2.0 ** 0.25
SAME_ENGINE_SYNC = True


class V:
    __slots__ = ("ap", "key")

    def __init__(self, ap, key):
        self.ap = ap
        self.key = key


class Tile:
    def __init__(self, handle, key):
        self.h = handle
        self.key = key

    def __getitem__(self, idx):
        return V(self.h[idx], self.key)

    def k(self, sfx):
        return _Keyed(self.h, self.key + ":" + str(sfx))


class _Keyed:
    def __init__(self, h, key):
        self.h = h
        self.key = key

    def __getitem__(self, idx):
        return V(self.h[idx], self.key)


class Sched:
    QS = ("pe", "act", "dve", "pool", "sp")

    def __init__(self, nc, stack):
        self.nc = nc
        self.stack = stack
        self.items = {q: [] for q in self.QS}
        self.esem = {}
        for q in ("pe", "act", "dve", "pool"):
            self.esem[q] = stack.enter_context(nc.semaphore("es_" + q))
        self.cnt = {q: 0 for q in self.QS}
        self.waited = {q: {} for q in self.QS}
        self.res = {}
        self.dsem = {}
        self.semh = {"es_" + q: h for q, h in self.esem.items()}
        self.n_ops = 0

    def _deps(self, q, reads, writes):
        evs = []
        for r in reads:
            st = self.res.get(r)
            if st and st[0]:
                evs.append(st[0])
        for w in writes:
            st = self.res.get(w)
            if st:
                if st[0]:
                    evs.append(st[0])
                evs.extend(st[1])
        waits = {}
        for (sem, val, srcq) in evs:
            if srcq == q and (q == "pe" or not SAME_ENGINE_SYNC):
                continue
            if self.waited[q].get(sem, 0) >= val:
                continue
            if waits.get(sem, 0) < val:
                waits[sem] = val
        for sem, val in waits.items():
            self.waited[q][sem] = val
        return list(waits.items())

    def _commit(self, ev, reads, writes):
        for r in reads:
            if r in writes:
                continue
            self.res.setdefault(r, [None, []])[1].append(ev)
        for w in writes:
            self.res[w] = [ev, []]

    @staticmethod
    def _keys(views):
        return [v.key for v in views if v is not None and v.key is not None]

    def op(self, q, fn, reads=(), writes=()):
        rk, wk = self._keys(reads), self._keys(writes)
        waits = self._deps(q, rk, wk)
        self.cnt[q] += 1
        ev = ("es_" + q, self.cnt[q], q)
        self.items[q].append((waits, fn, ("es_" + q, 1)))
        self._commit(ev, rk, wk)
        self.n_ops += 1

    def dma(self, q, fn, sem, reads=(), writes=()):
        if sem not in self.dsem:
            h = self.stack.enter_context(self.nc.semaphore("ds_" + sem))
            self.dsem[sem] = [h, 0]
            self.semh["ds_" + sem] = h
        rk, wk = self._keys(reads), self._keys(writes)
        waits = self._deps(q, rk, wk)
        self.dsem[sem][1] += 16
        ev = ("ds_" + sem, self.dsem[sem][1], "dma")
        self.items[q].append((waits, fn, ("ds_" + sem, 16)))
        self._commit(ev, rk, wk)
        self.n_ops += 1

    def emit(self, final=False):
        nc = self.nc
        semh = self.semh
        items = self.items
        bar = [("es_" + q, self.cnt[q]) for q in ("pe", "act", "dve", "pool") if self.cnt[q] > 0]
        bar += [("ds_" + k, v[1]) for k, v in self.dsem.items() if v[1] > 0]

        def replay(q):
            def run(eng):
                for waits, fn, inc in items[q]:
                    for sem, val in waits:
                        eng.wait_ge(semh[sem], val)
                    ins = fn(eng)
                    ins.then_inc(semh[inc[0]], inc[1])
                for sem, val in bar:
                    if self.waited[q].get(sem, 0) < val:
                        eng.wait_ge(semh[sem], val)
                        self.waited[q][sem] = val
            return run

        with nc.Block() as block:
            block.sync(replay("sp"))
            block.tensor(replay("pe"))
            block.scalar(replay("act"))
            block.vector(replay("dve"))
            block.gpsimd(replay("pool"))
        self.items = {q: [] for q in self.QS}


class K:
    def __init__(self, nc, S, stack):
        self.nc, self.S, self.stack = nc, S, stack
        self._n = 0
        self.rr = 0
        self._tiles = {}

    def sb(self, shape, dt, name):
        if name in self._tiles:
            return self._tiles[name]
        h = self.stack.enter_context(self.nc.sbuf_tensor(name, list(shape), dt))
        t = Tile(h, name)
        self._tiles[name] = t
        return t

    def ps(self, name, dt=F32, cols=512):
        h = self.stack.enter_context(self.nc.psum_tensor(name, [128, cols], dt))
        return Tile(h, name)

    def mm(self, out, lhsT, rhs, start, stop):
        self.S.op("pe", lambda e: e.matmul(out.ap, lhsT.ap, rhs.ap, start=start, stop=stop),
                  reads=[lhsT, rhs] + ([] if start else [out]), writes=[out])

    def tr(self, out, in_, ident):
        self.S.op("pe", lambda e: e.transpose(out.ap, in_.ap, ident.ap), reads=[in_, ident], writes=[out])

    def act(self, out, in_, func, bias=None, scale=None, accum=None, q="act"):
        kw = {}
        rd = [in_]
        if bias is not None:
            if isinstance(bias, V):
                kw["bias"] = bias.ap
                rd.append(bias)
            else:
                kw["bias"] = float(bias)
        if scale is not None:
            if isinstance(scale, V):
                kw["scale"] = scale.ap
                rd.append(scale)
            else:
                kw["scale"] = float(scale)
        wr = [out]
        if accum is not None:
            kw["accum_out"] = accum.ap
            wr.append(accum)
        self.S.op("act", lambda e: e.activation(out.ap, in_.ap, func, **kw), reads=rd, writes=wr)

    def copy(self, q, out, in_):
        if q == "act":
            self.S.op("act", lambda e: e.copy(out.ap, in_.ap), reads=[in_], writes=[out])
        else:
            self.S.op(q, lambda e: e.tensor_copy(out.ap, in_.ap), reads=[in_], writes=[out])

    def tt(self, q, out, a, b, op):
        self.S.op(q, lambda e: e.tensor_tensor(out.ap, a.ap, b.ap, op), reads=[a, b], writes=[out])

    def ts(self, q, out, a, s1, op0, s2=None, op1=None, accum=None):
        rd = [a]
        s1v = s1.ap if isinstance(s1, V) else float(s1)
        if isinstance(s1, V):
            rd.append(s1)
        s2v = None
        if s2 is not None:
            s2v = s2.ap if isinstance(s2, V) else float(s2)
            if isinstance(s2, V):
                rd.append(s2)
        wr = [out]
        kw = {}
        if accum is not None:
            kw["accum_out"] = accum.ap
            wr.append(accum)
        o1 = op1 if op1 is not None else ALU.bypass
        self.S.op(q, lambda e: e.tensor_scalar(out.ap, a.ap, s1v, s2v, op0, o1, **kw), reads=rd, writes=wr)

    def stt(self, out, a, s, b, op0, op1):
        rd = [a, b]
        sv = s.ap if isinstance(s, V) else float(s)
        if isinstance(s, V):
            rd.append(s)
        self.S.op("dve", lambda e: e.scalar_tensor_tensor(out.ap, a.ap, sv, b.ap, op0, op1), reads=rd, writes=[out])

    def memset(self, q, out, val):
        self.S.op(q, lambda e: e.memset(out.ap, val), writes=[out])

    def dma(self, out, in_, sem, q="sp", **kw):
        self.S.dma(q, lambda e: e.dma_start(out.ap, in_.ap, **kw), sem, reads=[in_], writes=[out])

    def evac_q(self):
        self.rr += 1
        return "act" if self.rr % 2 else "dve"


def _consts(kb):
    nc, S = kb.nc, kb.S
    ones_f = kb.sb([128, 128], F32, "c_onesf")
    ident = kb.sb([128, 128], F32, "c_ident")
    cmask = kb.sb([128, 128], F32, "c_cmask")
    ones_b = kb.sb([128, 128], BF16, "c_onesb")
    kb.memset("pool", ones_f[:, :], 1.0)
    kb.memset("pool", ones_b[:, :], 1.0)
    S.op("pool", lambda e: e.affine_select(ident.h[:, :], ones_f.h[:, :], [[-1, 128]], ALU.is_equal, 0.0,
                                           base=0, channel_multiplier=1),
         reads=[ones_f[:, :]], writes=[ident[:, :]])
    S.op("pool", lambda e: e.affine_select(cmask.h[:, :], ones_f.h[:, :], [[1, 128]], ALU.is_ge, 0.0,
                                           base=0, channel_multiplier=-1),
         reads=[ones_f[:, :]], writes=[cmask[:, :]])
    return dict(ones_f=ones_f, ident=ident, cmask=cmask, ones_b=ones_b)


def _load_weights_bf16(kb, w_dram, ncols, w_sb, stage, tagsem):
    for k in range(16):
        st = stage[k % 2]
        kb.dma(st[:, :ncols], V(w_dram[k * 128:(k + 1) * 128, :], None), f"{tagsem}{k % 2}")
        q = ("act", "dve", "pool")[k % 3]
        kb.copy(q, w_sb.k(k)[:, k, :ncols], st[:, :ncols])


def _tr_tile(kb, src, dst, Pa, Pb, ident, ncols=128):
    for g in range(4):
        Tb = Pa if g % 2 == 0 else Pb
        for j in range(4):
            kk = 4 * g + j
            kb.tr(Tb[:, j * 128:(j + 1) * 128], src[:, kk * 128:(kk + 1) * 128], ident[:, :])
        kb.copy(kb.evac_q(), dst[:, g * 512:(g + 1) * 512], Tb[:, :])


def _ln_tile(kb, y, g_rep, b_rep, out, stats, mv, r):
    S = kb.S
    for c in range(4):
        S.op("dve", lambda e, c=c: e.bn_stats(stats.h[:, c * 6:(c + 1) * 6], y.h[:, c * 512:(c + 1) * 512]),
             reads=[y[:, :]], writes=[stats[:, :]])
    S.op("dve", lambda e: e.bn_aggr(mv.h[:, 0:2], stats.h[:, 0:24]), reads=[stats[:, :]], writes=[mv[:, :]])
    kb.ts("dve", r[:, :], mv[:, 1:2], EPS, ALU.add)
    kb.act(r[:, :], r[:, :], AF.Sqrt)
    S.op("dve", lambda e: e.reciprocal(r.h[:, :], r.h[:, :]), reads=[r[:, :]], writes=[r[:, :]])
    kb.ts("dve", out[:, :], y[:, :], mv[:, 0:1], ALU.subtract, r[:, 0:1], ALU.mult)
    kb.tt("pool", out[:, :], out[:, :], g_rep[:, :], ALU.mult)
    kb.tt("pool", out[:, :], out[:, :], b_rep[:, :], ALU.add)


def _load_w(kb, w_ap, ncols, w_sb, stage, tagsem, col0=0):
    for k in range(16):
        st = stage[k % 2]
        kb.dma(st[:, :ncols], V(w_ap[k * 128:(k + 1) * 128, col0:col0 + ncols], None), f"{tagsem}{k % 2}")
        q = ("act", "dve", "pool")[k % 3]
        kb.copy(q, w_sb.k(k)[:, k, :ncols], st[:, :ncols])


NTT = 64
NTOK = NTT * 128
CAP = 1280
NJ = CAP // 128
RGS = ((0, 512), (512, 512), (1024, 256))
YROWS = 1 + 4 * NTOK + 128


def phases_bcd(nc, S, kb, C, P, io, dbg):
    ident, ones_b, ones_f = C["ident"], C["ones_b"], C["ones_f"]
    T0, T1, F0, F1, M0, M1, M2, PO = P
    mg, x_tok, out_ap = io["merged"], io["x_tok"], io["out"]
    h1s, h2s, ybufs = io["h1s"], io["h2s"], io["ybuf"]
    n_exp = io.get("n_exp", 32)

    kb.stack = S.stack
    idx_tok = kb.sb([128, 32 * NJ], I32, "R_idxtok")
    dest = kb.sb([128, 32 * NJ], I32, "R_dest")
    gl = kb.sb([128, 32 * NJ], F32, "R_gl")

    pB = contextlib.ExitStack()
    kb.stack = pB
    wsb = kb.sb([128, 16, 2048], BF16, "B_w")
    stage = [kb.sb([128, 2048], F32, f"B_st{i}") for i in range(2)]
    g_rep = kb.sb([128, 2048], F32, "B_g")
    b_rep = kb.sb([128, 2048], F32, "B_b")
    mt = [kb.sb([128, 2048], F32, f"B_mt{i}") for i in range(2)]
    xk = [kb.sb([128, 2048], F32, f"B_xk{i}") for i in range(2)]
    mT = [kb.sb([128, 2048], BF16, f"B_mT{i}") for i in range(2)]
    yt = [kb.sb([128, 2048], F32, f"B_y{i}") for i in range(2)]
    stats = kb.sb([128, 24], F32, "B_stats")
    mv = kb.sb([128, 2], F32, "B_mv")
    rr = kb.sb([128, 1], F32, "B_r")
    kb.dma(g_rep[:, :], V(io["ln1g"], None), "c0")
    kb.dma(b_rep[:, :], V(io["ln1b"], None), "c1")
    _load_w(kb, io["w_o"], 2048, wsb, stage, "ws")
    kb.dma(mt[0][:, :], V(mg[0:128, :], "merged"), "mt0")
    kb.dma(xk[0][:, :], V(x_tok[0:128, :], None), "xk0")
    for i in range(NTT):
        sl = i % 2
        if i + 1 < NTT:
            kb.dma(mt[1 - sl][:, :], V(mg[(i + 1) * 128:(i + 2) * 128, :], "merged"), f"mt{1 - sl}")
            kb.dma(xk[1 - sl][:, :], V(x_tok[(i + 1) * 128:(i + 2) * 128, :], None), f"xk{1 - sl}")
        _tr_tile(kb, mt[sl], mT[sl], T0, T1, ident)
        for n, Pb in enumerate((F0, F1, M0, M1)):
            for kk in range(16):
                kb.mm(Pb[:, :], mT[sl][:, kk * 128:(kk + 1) * 128], wsb.k(kk)[:, kk, n * 512:(n + 1) * 512],
                      start=(kk == 0), stop=(kk == 15))
            kb.stt(yt[sl][:, n * 512:(n + 1) * 512], xk[sl][:, n * 512:(n + 1) * 512], ALPHA, Pb[:, :],
                   ALU.mult, ALU.add)
        _ln_tile(kb, yt[sl], g_rep, b_rep, yt[sl], stats, mv, rr)
        kb.dma(V(h1s[i * 128:(i + 1) * 128, :], "h1s"), yt[sl][:, :], f"ho{sl}")
    S.emit()
    pB.close()

    pR = contextlib.ExitStack()
    kb.stack = pR
    gate_all = kb.sb([128, NTT, 32], F32, "R_gate")

    pB = contextlib.ExitStack()
    kb.stack = pB
    wq = kb.sb([128, 16, 2048], BF16, "C_wq")
    wo = kb.sb([128, 16, 2048], BF16, "C_wo")
    g_rep = kb.sb([128, 2048], F32, "C_g")
    b_rep = kb.sb([128, 2048], F32, "C_b")
    big = kb.sb([128, 4096], BF16, "C_big")
    memT = Tile(big.h[:, :].rearrange("p (k m) -> p k m", k=16), "C_memT")
    KT = kb.sb([128, 16, 256], BF16, "C_KT")
    Vs = kb.sb([128, 2, 2048], BF16, "C_V")
    ht0 = kb.sb([128, 2048], F32, "C_ht0")
    ht = [ht0, ht0]
    stage = ht
    hT = kb.sb([128, 2048], BF16, "C_hT")
    qT = Tile(big.h[:, 0:2048], "C_qT")
    pT = Tile(big.h[:, 2048:3072], "C_pT")
    oT = hT
    yt = kb.sb([128, 2048], F32, "C_y")
    memt = yt
    pf = Tile(yt.h[:, 0:1024], "C_y")
    wr = kb.sb([128, 16, 32], F32, "C_wr")
    br = kb.sb([128, 32], F32, "C_br")
    stats = kb.sb([128, 24], F32, "C_stats")
    mv = kb.sb([128, 2], F32, "C_mv")
    rr = kb.sb([128, 1], F32, "C_r")
    mx = kb.sb([128, 4], F32, "C_mx")
    sm = kb.sb([128, 4], F32, "C_sm")
    m8 = kb.sb([128, 8], F32, "C_m8")
    ex = kb.sb([128, 32], F32, "C_ex")
    s1 = kb.sb([128, 2], F32, "C_s1")
    mk = kb.sb([128, 32], F32, "C_mk")
    kb.dma(g_rep[:, :], V(io["ln2g"], None), "c0")
    kb.dma(b_rep[:, :], V(io["ln2b"], None), "c1")
    kb.dma(wr[:, :, :], V(io["w_router"].rearrange("(k p) e -> p k e", p=128), None), "c2")
    kb.dma(br[:, :], V(io["br_rep"], None), "c3")
    for mtile in range(2):
        kb.dma(memt[:, :], V(io["mem_b"][mtile * 128:(mtile + 1) * 128, :], None), "mm")
        for g in range(4):
            Tb = T0 if g % 2 == 0 else T1
            for j in range(4):
                kk = 4 * g + j
                kb.tr(Tb[:, j * 128:(j + 1) * 128], memt[:, kk * 128:(kk + 1) * 128], ident[:, :])
            S.op("dve", lambda e, Tb=Tb, g=g, mtile=mtile: e.tensor_copy(
                memT.h[:, 4 * g:4 * g + 4, mtile * 128:(mtile + 1) * 128],
                Tb.h[:, :].rearrange("p (a b) -> p a b", a=4)), reads=[Tb[:, :]], writes=[memT[:, :, :]])
    _load_w(kb, io["w_kv"], 2048, wq, stage, "ws", col0=0)
    banks = (F0, F1, M0, M1)
    for c in range(16):
        Pb = banks[(c // 2) % 4]
        cs = slice((c % 2) * 256, (c % 2) * 256 + 256)
        for kk in range(16):
            kb.mm(Pb[:, cs], wq.k(kk)[:, kk, c * 128:(c + 1) * 128], memT[:, kk, :], start=(kk == 0), stop=(kk == 15))
        if c % 2 == 1:
            S.op("act", lambda e, Pb=Pb, c=c: e.copy(KT.h[:, c - 1:c + 1, :], Pb.h[:, :].rearrange("p (a b) -> p a b", a=2)),
                 reads=[Pb[:, :]], writes=[KT[:, :, :]])
    _load_w(kb, io["w_kv"], 2048, wq, stage, "ws", col0=2048)
    for mtile in range(2):
        for n in range(4):
            Pb = banks[n]
            for kk in range(16):
                kb.mm(Pb[:, :], memT[:, kk, mtile * 128:(mtile + 1) * 128], wq.k(kk)[:, kk, n * 512:(n + 1) * 512],
                      start=(kk == 0), stop=(kk == 15))
            kb.copy(kb.evac_q(), Vs[:, mtile, n * 512:(n + 1) * 512], Pb[:, :])
    _load_w(kb, io["w_xq"], 2048, wq, stage, "ws")
    _load_w(kb, io["w_xo"], 2048, wo, stage, "ws")
    SC = 512.0 ** -0.5
    for i in range(NTT):
        sl = i % 2
        kb.dma(ht[sl][:, :], V(h1s[i * 128:(i + 1) * 128, :], "h1s"), "ht")
        _tr_tile(kb, ht[sl], hT, T0, T1, ident)
        for c in range(16):
            Pb = banks[c // 4]
            cs = slice((c % 4) * 128, (c % 4) * 128 + 128)
            for kk in range(16):
                kb.mm(Pb[:, cs], wq.k(kk)[:, kk, c * 128:(c + 1) * 128], hT[:, kk * 128:(kk + 1) * 128],
                      start=(kk == 0), stop=(kk == 15))
            if c % 4 == 3:
                g = c // 4
                kb.act(qT[:, g * 512:(g + 1) * 512], Pb[:, :], AF.Identity, scale=SC)
        for hx in range(4):
            Pb = M2 if hx < 2 else PO
            cs = slice((hx % 2) * 256, (hx % 2) * 256 + 256)
            for cc in range(4):
                c = hx * 4 + cc
                kb.mm(Pb[:, cs], qT[:, c * 128:(c + 1) * 128], KT[:, c, :], start=(cc == 0), stop=(cc == 3))
        for hx in range(4):
            Pb = M2 if hx < 2 else PO
            cs = slice((hx % 2) * 256, (hx % 2) * 256 + 256)
            S.op("dve", lambda e, Pb=Pb, cs=cs, hx=hx: e.reduce_max(mx.h[:, hx:hx + 1], Pb.h[:, cs], AX.X),
                 reads=[Pb[:, :]], writes=[mx[:, :]])
        kb.ts("dve", mx[:, :], mx[:, :], -1.0, ALU.mult)
        for hx in range(4):
            Pb = M2 if hx < 2 else PO
            cs = slice((hx % 2) * 256, (hx % 2) * 256 + 256)
            kb.act(pf[:, hx * 256:(hx + 1) * 256], Pb[:, cs], AF.Exp, bias=mx[:, hx:hx + 1], accum=sm[:, hx:hx + 1])
        S.op("dve", lambda e: e.reciprocal(sm.h[:, :], sm.h[:, :]), reads=[sm[:, :]], writes=[sm[:, :]])
        for hx in range(4):
            kb.ts("dve", pf[:, hx * 256:(hx + 1) * 256], pf[:, hx * 256:(hx + 1) * 256], sm[:, hx:hx + 1], ALU.mult)
        for g in range(2):
            Tb = T0 if g == 0 else T1
            for j in range(4):
                kk = 4 * g + j
                kb.tr(Tb[:, j * 128:(j + 1) * 128], pf[:, kk * 128:(kk + 1) * 128], ident[:, :])
            kb.copy(kb.evac_q(), pT[:, g * 512:(g + 1) * 512], Tb[:, :])
        for c in range(16):
            Pb = banks[c // 4]
            cs = slice((c % 4) * 128, (c % 4) * 128 + 128)
            hx = c // 4
            for mc in range(2):
                kb.mm(Pb[:, cs], Vs[:, mc, c * 128:(c + 1) * 128], pT[:, (hx * 2 + mc) * 128:(hx * 2 + mc + 1) * 128],
                      start=(mc == 0), stop=(mc == 1))
            if c % 4 == 3:
                g = c // 4
                kb.copy(kb.evac_q(), oT[:, g * 512:(g + 1) * 512], Pb[:, :])
        for n in range(4):
            Pb = banks[n]
            for kk in range(16):
                kb.mm(Pb[:, :], oT[:, kk * 128:(kk + 1) * 128], wo.k(kk)[:, kk, n * 512:(n + 1) * 512],
                      start=(kk == 0), stop=(kk == 15))
            kb.stt(yt[:, n * 512:(n + 1) * 512], ht[sl][:, n * 512:(n + 1) * 512], ALPHA, Pb[:, :], ALU.mult, ALU.add)
        _ln_tile(kb, yt, g_rep, b_rep, yt, stats, mv, rr)
        kb.dma(V(h2s[i * 128:(i + 1) * 128, :], "h2s"), yt[:, :], "ho")
        h2T = ht[sl]
        for g in range(4):
            Tb = T0 if g % 2 == 0 else T1
            for j in range(4):
                kk = 4 * g + j
                kb.tr(Tb[:, j * 128:(j + 1) * 128], yt[:, kk * 128:(kk + 1) * 128], ident[:, :])
            kb.copy(kb.evac_q(), h2T[:, g * 512:(g + 1) * 512], Tb[:, :])
        for kk in range(16):
            kb.mm(M2[:, 0:32], h2T[:, kk * 128:(kk + 1) * 128], wr[:, kk, :], start=(kk == 0), stop=(kk == 15))
        lg = ex
        kb.tt("dve", lg[:, :], M2[:, 0:32], br[:, :], ALU.add)
        S.op("dve", lambda e: e.max(m8.h[:, :], lg.h[:, :]), reads=[lg[:, :]], writes=[m8[:, :]])
        kb.ts("dve", mk[:, :], lg[:, :], m8[:, 3:4], ALU.is_ge)
        kb.ts("dve", s1[:, 0:1], m8[:, 0:1], -1.0, ALU.mult)
        kb.act(lg[:, :], lg[:, :], AF.Exp, bias=s1[:, 0:1])
        kb.tt("dve", lg[:, :], lg[:, :], mk[:, :], ALU.mult)
        S.op("dve", lambda e: e.reduce_sum(s1.h[:, 1:2], lg.h[:, :], AX.X), reads=[lg[:, :]], writes=[s1[:, :]])
        S.op("dve", lambda e: e.reciprocal(s1.h[:, 1:2], s1.h[:, 1:2]), reads=[s1[:, :]], writes=[s1[:, :]])
        kb.ts("dve", gate_all[:, i, :], lg[:, :], s1[:, 1:2], ALU.mult)
    S.emit()
    pB.close()

    pB = contextlib.ExitStack()
    kb.stack = pB
    mask_bf = kb.sb([128, NTT, 32], BF16, "D_maskbf")
    SU = kb.sb([128, 128], BF16, "D_SU")
    pos = kb.sb([128, NTT, 32], F32, "D_pos")
    slot = kb.sb([128, NTT, 32], F32, "D_slot")
    toki = kb.sb([128, NTT], I32, "D_toki")
    tokf = kb.sb([128, NTT], F32, "D_tokf")
    tokp1 = kb.sb([128, NTT], F32, "D_tokp1")
    pay = kb.sb([128, NTT, 32, 3], F32, "D_pay")
    io_i = kb.sb([128, CAP], I32, "D_ioi")
    io_f = kb.sb([128, CAP], F32, "D_iof")
    tr_i = kb.sb([128, 1], I32, "D_tri")
    tr_f = kb.sb([128, 1], F32, "D_trf")
    zer = kb.sb([128, 128], F32, "D_zer")
    Sel = [kb.sb([128, CAP], F32, f"D_sel{i}") for i in range(3)]
    lst = kb.sb([128, 32, NJ * 3], F32, "D_lst")
    tmpd = kb.sb([128, 32 * NJ], F32, "D_tmpd")
    mask_all = kb.sb([128, NTT, 32], F32, "D_mask")
    kb.memset("pool", zer[:, :], 0.0)
    kb.ts("dve", mask_all[:, :, :], gate_all[:, :, :], 0.0, ALU.is_gt)
    kb.copy("dve", mask_bf[:, :, :], mask_all[:, :, :])
    S.op("pool", lambda e: e.affine_select(SU.h[:, :], ones_f.h[:, :], [[1, 128]], ALU.is_ge, 0.0,
                                           base=-1, channel_multiplier=-1), reads=[ones_f[:, :]], writes=[SU[:, :]])
    S.op("pool", lambda e: e.iota(toki.h[:, :], [[128, NTT]], base=0, channel_multiplier=1), writes=[toki[:, :]])
    S.op("pool", lambda e: e.iota(io_i.h[:, :], [[1, CAP]], base=0, channel_multiplier=0), writes=[io_i[:, :]])
    S.op("pool", lambda e: e.iota(tr_i.h[:, :], [[0, 1]], base=1 + 4 * NTOK, channel_multiplier=1), writes=[tr_i[:, :]])
    kb.copy("dve", tokf[:, :], toki[:, :])
    kb.copy("dve", io_f[:, :], io_i[:, :])
    kb.copy("dve", tr_f[:, :], tr_i[:, :])
    kb.ts("dve", tokp1[:, :], tokf[:, :], 1.0, ALU.add)
    pbanks = (F0, F1, M0, M1)
    for i in range(NTT):
        Pb = pbanks[i // 16]
        cs = slice((i % 16) * 32, (i % 16) * 32 + 32)
        for ip in range(i):
            kb.mm(Pb[:, cs], ones_b[:, :], mask_bf[:, ip, :], start=(ip == 0), stop=False)
        kb.mm(Pb[:, cs], SU[:, :], mask_bf[:, i, :], start=(i == 0), stop=True)
    for q_ in range(NTT // 16):
        S.op("dve", lambda e, q_=q_: e.tensor_copy(pos.h[:, q_ * 16:(q_ + 1) * 16, :],
                                                  pbanks[q_].h[:, :].rearrange("p (a b) -> p a b", a=16)),
             reads=[pbanks[q_][:, :]], writes=[pos[:, :, :]])
    for i in range(NTT):
        S.op("dve", lambda e, i=i: e.tensor_tensor_scan(slot.h[:, i, :], ones_f.h[:, 0:32], mask_all.h[:, i, :], 0.0,
                                                         ALU.mult, ALU.add),
             reads=[ones_f[:, :], mask_all[:, :, :]], writes=[slot[:, :, :]])
    kb.tt("dve", slot[:, :, :], slot[:, :, :], mask_all[:, :, :], ALU.subtract)
    for i in range(NTT):
        kb.ts("dve", pay[:, i, :, 0], mask_all[:, i, :], 0.0, ALU.mult, tokf[:, i:i + 1], ALU.add)
        kb.ts("dve", pay[:, i, :, 2], slot[:, i, :], float(NTOK), ALU.mult, tokp1[:, i:i + 1], ALU.add)
    kb.copy("dve", pay[:, :, :, 1], gate_all[:, :, :])
    n = 0
    for e_ in range(32):
        Pb = (M2, PO)[e_ % 2]
        kb.mm(Pb[:, 0:NJ * 3], zer[:, :], zer[:, 0:NJ * 3], start=True, stop=False)
        for i in range(NTT):
            sel = Sel[n % 3]
            n += 1
            kb.ts("dve", sel[:, :], io_f[:, :], pos[:, i, e_:e_ + 1], ALU.is_equal, mask_all[:, i, e_:e_ + 1], ALU.mult)
            for j in range(NJ):
                kb.mm(Pb[:, 3 * j:3 * j + 3], sel[:, j * 128:(j + 1) * 128], pay[:, i, e_, :],
                      start=False, stop=(i == NTT - 1 and j == NJ - 1))
        kb.copy("act", lst[:, e_, :], Pb[:, 0:NJ * 3])
    l4 = lst.h[:, :, :].rearrange("p e (j c) -> p e j c", c=3)
    iv = idx_tok.h[:, :].rearrange("p (e j) -> p e j", e=32)
    gv = gl.h[:, :].rearrange("p (e j) -> p e j", e=32)
    tv = tmpd.h[:, :].rearrange("p (e j) -> p e j", e=32)
    S.op("dve", lambda e: e.tensor_copy(iv, l4[:, :, :, 0]), reads=[lst[:, :, :]], writes=[idx_tok[:, :]])
    S.op("dve", lambda e: e.tensor_copy(gv, l4[:, :, :, 1]), reads=[lst[:, :, :]], writes=[gl[:, :]])
    S.op("dve", lambda e: e.tensor_scalar(tv, l4[:, :, :, 2], 0.0, tr_f.h[:, 0:1], ALU.is_equal, ALU.mult),
         reads=[lst[:, :, :], tr_f[:, :]], writes=[tmpd[:, :]])
    S.op("dve", lambda e: e.tensor_tensor(tv, tv, l4[:, :, :, 2], ALU.add),
         reads=[lst[:, :, :], tmpd[:, :]], writes=[tmpd[:, :]])
    kb.copy("dve", dest[:, :], tmpd[:, :])
    S.emit()
    pB.close()
    pR.close()

    pB = contextlib.ExitStack()
    kb.stack = pB
    stg = [kb.sb([128, 16, 512], F32, f"E_stg{i}") for i in range(2)]
    wb = [kb.sb([128, 16, 512], BF16, f"E_wb{i}") for i in range(2)]
    XgT = kb.sb([128, 16, CAP], BF16, "E_XgT")
    xg0 = kb.sb([128, 2048], F32, "E_xg0")
    xg = [xg0, xg0]
    actT = kb.sb([128, 16, CAP], BF16, "E_actT")
    Yp = [kb.sb([128, 512], F32, f"E_Y{j}") for j in range(2)]
    bgu = kb.sb([128, 1024], F32, "E_bgu")
    bdr = kb.sb([128, 512], F32, "E_bdr")
    bdb = [kb.sb([128, 512], BF16, f"E_bdb{i}") for i in range(2)]
    glu = kb.sb([128, 512], F32, "E_glu")
    lin = kb.sb([128, 512], F32, "E_lin")
    sg = kb.sb([128, 512], F32, "E_sg")
    kb.dma(bgu[:, :], V(io["bgu_col"], None), "c0")
    kb.memset("pool", bdr[:, :], 0.0)
    for i in range(2):
        kb.memset("pool", bdb[i][:, :], 0.0)
    w_gu, w_dn, b_dn = io["w_gu"], io["w_dn"], io["b_dn"]
    nblk = 0
    ny = 0
    gbanks = ((F0, F1), (M0, M1), (M2, PO))
    dbanks = (F0, F1, M0, M1, M2, PO)
    for e_ in range(n_exp):
        for j in range(NJ):
            col = e_ * NJ + j
            xs = 0
            S.dma("pool", lambda e, xs=xs, col=col: e.indirect_dma_start(
                xg[xs].h[:, :], None, h2s[:, :], bass.IndirectOffsetOnAxis(idx_tok.h[:, col:col + 1], 0)),
                f"xg{xs}", reads=[idx_tok[:, :], V(None, "h2s")], writes=[xg[xs][:, :]])
            for g in range(4):
                Tb = T0 if g % 2 == 0 else T1
                for jj in range(4):
                    kk = 4 * g + jj
                    kb.tr(Tb[:, jj * 128:(jj + 1) * 128], xg[xs][:, kk * 128:(kk + 1) * 128], ident[:, :])
                q = kb.evac_q()
                fn = (lambda e, Tb=Tb, g=g, j=j: e.copy(
                    XgT.h[:, 4 * g:4 * g + 4, j * 128:(j + 1) * 128], Tb.h[:, :].rearrange("p (a b) -> p a b", a=4))) \
                    if q == "act" else (lambda e, Tb=Tb, g=g, j=j: e.tensor_copy(
                        XgT.h[:, 4 * g:4 * g + 4, j * 128:(j + 1) * 128], Tb.h[:, :].rearrange("p (a b) -> p a b", a=4)))
                S.op(q, fn, reads=[Tb[:, :]], writes=[XgT[:, :, :]])
        for cb in range(8):
            bs = nblk % 2
            nblk += 1
            kb.dma(stg[bs][:, :, :], V(w_gu[e_ * D:(e_ + 1) * D, cb * 512:(cb + 1) * 512].rearrange("(k p) c -> p k c", p=128), None),
                   f"stg{bs}")
            sv = stg[bs].h[:, :, :].rearrange("p k (c two) -> p k c two", two=2)
            S.op("act", lambda e, bs=bs, sv=sv: e.copy(wb[bs].h[:, :, 0:256], sv[:, :, :, 0]),
                 reads=[stg[bs][:, :, :]], writes=[wb[bs][:, :, :]])
            S.op("pool", lambda e, bs=bs, sv=sv: e.tensor_copy(wb[bs].h[:, :, 256:512], sv[:, :, :, 1]),
                 reads=[stg[bs][:, :, :]], writes=[wb[bs][:, :, :]])
            for m in range(2):
                mc = cb * 2 + m
                bg = bgu[:, e_ * 32 + mc:e_ * 32 + mc + 1]
                bl = bgu[:, e_ * 32 + 16 + mc:e_ * 32 + 16 + mc + 1]
                for gi, (r0, rn) in enumerate(RGS):
                    Pg, Pl = gbanks[gi]
                    for kk in range(16):
                        kb.mm(Pg[:, 0:rn], wb[bs][:, kk, m * 128:(m + 1) * 128], XgT[:, kk, r0:r0 + rn],
                              start=(kk == 0), stop=(kk == 15))
                    for kk in range(16):
                        kb.mm(Pl[:, 0:rn], wb[bs][:, kk, 256 + m * 128:256 + (m + 1) * 128], XgT[:, kk, r0:r0 + rn],
                              start=(kk == 0), stop=(kk == 15))
                    kb.ts("dve", glu[:, 0:rn], Pg[:, 0:rn], bg, ALU.add, 7.0, ALU.min)
                    kb.ts("dve", lin[:, 0:rn], Pl[:, 0:rn], bl, ALU.add, 7.0, ALU.min)
                    kb.ts("dve", lin[:, 0:rn], lin[:, 0:rn], -7.0, ALU.max, 1.0, ALU.add)
                    kb.act(sg[:, 0:rn], glu[:, 0:rn], AF.Sigmoid, scale=1.702)
                    kb.tt("dve", glu[:, 0:rn], glu[:, 0:rn], sg[:, 0:rn], ALU.mult)
                    kb.tt("dve", actT[:, mc, r0:r0 + rn], glu[:, 0:rn], lin[:, 0:rn], ALU.mult)
        for nb in range(4):
            bs = nblk % 2
            nblk += 1
            kb.dma(stg[bs][:, :, :], V(w_dn[e_ * D:(e_ + 1) * D, nb * 512:(nb + 1) * 512].rearrange("(k p) c -> p k c", p=128), None),
                   f"stg{bs}")
            S.op("act", lambda e, bs=bs: e.copy(wb[bs].h[:, 0:8, :], stg[bs].h[:, 0:8, :]),
                 reads=[stg[bs][:, :, :]], writes=[wb[bs][:, :, :]])
            S.op("pool", lambda e, bs=bs: e.tensor_copy(wb[bs].h[:, 8:16, :], stg[bs].h[:, 8:16, :]),
                 reads=[stg[bs][:, :, :]], writes=[wb[bs][:, :, :]])
            ds_ = nb % 2
            kb.dma(bdr[0:1, :], V(b_dn[e_:e_ + 1, nb * 512:(nb + 1) * 512], None), "bd")
            kb.copy("pool", bdb[ds_][0:1, :], bdr[0:1, :])
            yb = ybufs[nb]
            for j in range(NJ):
                Pb = dbanks[j % 6]
                col = e_ * NJ + j
                for kk in range(16):
                    kb.mm(Pb[:, :], actT[:, kk, j * 128:(j + 1) * 128], wb[bs][:, kk, :], start=(kk == 0), stop=False)
                kb.mm(Pb[:, :], ones_b[:, :], bdb[ds_][:, :], start=False, stop=True)
                ys_ = ny % 2
                ny += 1
                kb.ts("dve", Yp[ys_][:, :], Pb[:, :], gl[:, col:col + 1], ALU.mult)
                S.dma("pool", lambda e, ys_=ys_, col=col, yb=yb: e.indirect_dma_start(
                    yb[:, :], bass.IndirectOffsetOnAxis(dest.h[:, col:col + 1], 0), Yp[ys_].h[:, :], None),
                    f"ys{ys_}", reads=[dest[:, :], Yp[ys_][:, :]], writes=[V(None, "ybuf")])
    S.emit()
    pB.close()

    pB = contextlib.ExitStack()
    kb.stack = pB
    g_rep = kb.sb([128, 2048], F32, "F_g")
    b_rep = kb.sb([128, 2048], F32, "F_b")
    ys = [[kb.sb([128, 2048], F32, f"F_ys{i}_{s_}") for s_ in range(4)] for i in range(2)]
    hh2 = [kb.sb([128, 2048], F32, f"F_h{i}") for i in range(2)]
    yt = [kb.sb([128, 2048], F32, f"F_y{i}") for i in range(2)]
    stats = kb.sb([128, 24], F32, "F_stats")
    mv = kb.sb([128, 2], F32, "F_mv")
    rr = kb.sb([128, 1], F32, "F_r")
    kb.dma(g_rep[:, :], V(io["ln3g"], None), "c0")
    kb.dma(b_rep[:, :], V(io["ln3b"], None), "c1")
    for i in range(NTT):
        sl = i % 2
        kb.dma(hh2[sl][:, :], V(h2s[i * 128:(i + 1) * 128, :], "h2s"), f"fh{sl}")
        for s_ in range(4):
            r0 = 1 + s_ * NTOK + i * 128
            for nb in range(4):
                kb.dma(ys[sl][s_][:, nb * 512:(nb + 1) * 512], V(ybufs[nb][r0:r0 + 128, :], "ybuf"), f"fy{sl}_{s_}")
        kb.tt("dve", yt[sl][:, :], ys[sl][0][:, :], ys[sl][1][:, :], ALU.add)
        kb.tt("pool", ys[sl][2][:, :], ys[sl][2][:, :], ys[sl][3][:, :], ALU.add)
        kb.tt("dve", yt[sl][:, :], yt[sl][:, :], ys[sl][2][:, :], ALU.add)
        kb.stt(yt[sl][:, :], hh2[sl][:, :], ALPHA, yt[sl][:, :], ALU.mult, ALU.add)
        _ln_tile(kb, yt[sl], g_rep, b_rep, yt[sl], stats, mv, rr)
        kb.dma(V(out_ap[i * 128:(i + 1) * 128, :], None), yt[sl][:, :], f"fo{sl}")
    S.emit()
    pB.close()


def _set_cfg(npb):
    global NPB, NPRE, NTT, NTOK, CAP, NJ, RGS, YROWS
    NPB = npb
    NTT = NT_SEQ // npb
    NPRE = NT_SEQ - NTT
    NTOK = NTT * 128
    CAP, RGS = {1: (1280, ((0, 512), (512, 512), (1024, 256))), 2: (640, ((0, 512), (512, 128))),
                4: (384, ((0, 384),))}[npb]
    NJ = CAP // 128
    YROWS = 1 + 4 * NTOK + 128


def build_program(mode="full", dbg_tiles=None, n_exp=32, ng=4, npb=2, npre_dbg=None):
    global NPRE
    _set_cfg(npb)
    if npre_dbg is not None:
        NPRE = npre_dbg
    nc = bass.Bass("TRN2", target_bir_lowering=False)
    stack = contextlib.ExitStack()
    nt = dbg_tiles or NT_SEQ
    dbg = (mode == "A")
    doA = mode in ("full", "A")
    doB = mode in ("full", "BCD")
    NG = ng

    def din(name, shape, dt=F32):
        return nc.dram_tensor(name, list(shape), dt, kind="ExternalInput").ap()

    x_b = din("x_b", [SEQ, D])
    if doA:
        keep_in = din("keep_rep", [128, NT_SEQ])
        w_h_all = din("w_h", [NG * D, 2560])
        bcol_h_all = din("bcol_h", [NG * 128, 8])
        brow_h_all = din("brow_h", [NG, 1536])
        lbl_all = din("lbl", [NG * 128, 8])
        hng_rep = din("hng_rep", [128, 512])
        w_g_all = din("w_g", [NG * D, 2064])
        bcol_g_all = din("bcol_g", [NG * 128, 8])
        brow_g_all = din("brow_g", [NG, 1536])
        wa2_all = din("wa2", [NG * 16, 256])
        gng_rep = din("gng_rep", [128, 512])
        if dbg:
            gh_out = nc.dram_tensor("gh_out", [SEQ, 512], F32, kind="ExternalOutput").ap()
            mg_all = nc.dram_tensor("mg_out", [SEQ, NG * 512], F32, kind="ExternalOutput").ap()
        else:
            gh_out = nc.dram_tensor("gh_scr", [SEQ, 512], F32).ap()
            mg_all = nc.dram_tensor("merged", [NTOK, D], F32).ap()
    io = {}
    if doB:
        if mode == "BCD":
            io["merged"] = din("merged", [NTOK, D])
        else:
            io["merged"] = mg_all
        io["x_tok"] = x_b[NPRE * 128:, :]
        for nm in ("ln1g", "ln1b", "ln2g", "ln2b", "ln3g", "ln3b"):
            io[nm] = din(nm, [128, D])
        io["w_o"] = din("w_o", [D, D])
        io["w_kv"] = din("w_kv", [D, 2 * D])
        io["w_xq"] = din("w_xq", [D, D])
        io["w_xo"] = din("w_xo", [D, D])
        io["mem_b"] = din("mem_b", [256, D])
        io["w_router"] = din("w_router", [D, 32])
        io["br_rep"] = din("br_rep", [128, 32])
        io["bgu_col"] = din("bgu_col", [128, 1024])
        io["w_gu"] = din("w_gu", [32 * D, 2 * D])
        io["w_dn"] = din("w_dn", [32 * D, D])
        io["b_dn"] = din("b_dn", [32, D])
        io["out"] = nc.dram_tensor("out", [NTOK, D], F32, kind="ExternalOutput").ap()
        if mode == "BCD":
            io["h1s"] = nc.dram_tensor("h1s", [NTOK, D], F32, kind="ExternalOutput").ap()
            io["h2s"] = nc.dram_tensor("h2s", [NTOK, D], F32, kind="ExternalOutput").ap()
        else:
            io["h1s"] = nc.dram_tensor("h1s", [NTOK, D], F32).ap()
            io["h2s"] = nc.dram_tensor("h2s", [NTOK, D], F32).ap()
        io["ybuf"] = [nc.dram_tensor(f"ybuf{nb}", [YROWS, 512], F32).ap() for nb in range(4)]
        io["n_exp"] = n_exp

    with stack:
        S = Sched(nc, stack)
        kb = K(nc, S, stack)
        C = _consts(kb)
        ident, cmask, ones_b = C["ident"], C["cmask"], C["ones_b"]

        T0, T1 = kb.ps("pT0"), kb.ps("pT1")
        F0, F1 = kb.ps("pF0"), kb.ps("pF1")
        M0, M1, M2 = kb.ps("pM0"), kb.ps("pM1"), kb.ps("pM2")
        PO = kb.ps("pO")

        if doA:
            pA = contextlib.ExitStack()
            kb.stack = pA
            bF0, bF1, bM0, bM1, bM2, bPO = F0, F1, M0, M1, M2, PO
            for hh in range(NG):
                F0, F1, M0 = bF0, bF1, bM0
                keep = kb.sb([128, NT_SEQ], F32, "keep")
                if hh == 0:
                    kb.dma(keep[:, :], V(keep_in, None), "ck")
                w_h = w_h_all[hh * D:(hh + 1) * D, :]
                bcol_h = bcol_h_all[hh * 128:(hh + 1) * 128, :]
                brow_h = brow_h_all[hh:hh + 1, :]
                lbl = lbl_all[hh * 128:(hh + 1) * 128, :]
                w_g = w_g_all[hh * D:(hh + 1) * D, :]
                bcol_g = bcol_g_all[hh * 128:(hh + 1) * 128, :]
                brow_g = brow_g_all[hh:hh + 1, :]
                wa2_in = wa2_all[hh * 16:(hh + 1) * 16, :]
                mg_out = mg_all[:, hh * 512:(hh + 1) * 512]
                w_sb = kb.sb([128, 16, 2560], BF16, "w_sb")
                stage = [kb.sb([128, 2560], F32, f"wstage{i}") for i in range(2)]
                xt = [kb.sb([128, D], F32, f"xt{i}") for i in range(2)]
                xT = [kb.sb([128, 16 * 128], BF16, f"xT{i}") for i in range(2)]
                bcol = kb.sb([128, 8], F32, "bcol")
                brow = kb.sb([128, 1536], F32, "brow")
                brow_b = kb.sb([128, 1536], BF16, "brow_b")
                lb_in = kb.sb([128, 8], F32, "lb_in")
                lbv = kb.sb([128, 4], F32, "lbv")
                omlb = kb.sb([128, 4], F32, "omlb")
                hng = kb.sb([128, 512], F32, "hng")

                kb.dma(bcol[:, :], V(bcol_h, None), "c0")
                kb.memset("dve", brow[:, :], 0.0)
                kb.dma(brow[0:1, :], V(brow_h, None), "c1")
                kb.dma(lb_in[:, :], V(lbl, None), "c2")
                kb.dma(hng[:, :], V(hng_rep, None), "c3")
                kb.copy("dve", brow_b[:, :], brow[:, :])
                kb.tt("dve", lbv[:, :], lb_in[:, 0:4], lb_in[:, 4:8], ALU.subtract)
                kb.act(lbv[:, :], lbv[:, :], AF.Sigmoid)
                kb.ts("dve", omlb[:, :], lbv[:, :], -1.0, ALU.mult, 1.0, ALU.add)

                _load_weights_bf16(kb, w_h, 2560, w_sb, stage, "ws")

                def wt(name, shape, dt=F32):
                    return kb.sb(shape, dt, name)
                WS = []
                for wi in range(2):
                    WS.append((
                        wt(f"qT{wi}", [128, 512]), wt(f"fT{wi}", [128, 512]), wt(f"kTt{wi}", [128, 512]),
                        wt(f"cum{wi}", [128, 512]), wt(f"e1{wi}", [128, 512]), wt(f"e2{wi}", [128, 512]),
                        wt(f"mid{wi}", [128, 8]), wt(f"sc{wi}", [128, 8]),
                        wt(f"Qt{wi}", [128, 512], BF16), wt(f"Qc{wi}", [128, 512], BF16), wt(f"Kt{wi}", [128, 512], BF16),
                        wt(f"KlT{wi}", [128, 512]), wt(f"Kl{wi}", [128, 512], BF16), wt(f"Vb{wi}", [128, 512], BF16),
                        wt(f"AT{wi}", [128, 512], BF16), wt(f"gate{wi}", [128, 512]), wt(f"g2{wi}", [128, 512]),
                        wt(f"osq{wi}", [128, 512]), wt(f"ss{wi}", [128, 4]), wt(f"rstd{wi}", [128, 4])))
                St = wt("St", [128, 512])
                Sb = wt("Sb", [128, 512], BF16)
                ot = [wt(f"ot{i}", [128, 512]) for i in range(2)]
                onesf = C["ones_f"]

                kb.memset("dve", St[:, :], 0.0)
                kb.memset("dve", Sb[:, :], 0.0)

                kb.dma(xt[0][:, :], V(x_b[0:128, :], None), "x0")
                for t in range(nt):
                    sl = t % 2
                    full = (t >= NPRE)
                    (qT, fT, kTt, cum, e1, e2, mid, sc, Qt, Qc, Kt, KlT, Kl, Vb, AT, gate, g2, osq, ss, rstd) = WS[sl]
                    if full or sl == 0:
                        F0, F1, M0 = bF0, bF1, bM0
                    else:
                        F1, M0 = bF0, bM1
                    if t + 1 < nt:
                        kb.dma(xt[1 - sl][:, :], V(x_b[(t + 1) * 128:(t + 2) * 128, :], None), f"x{1 - sl}")
                    for g in range(4):
                        Tb = T0 if g % 2 == 0 else T1
                        for j in range(4):
                            kk = 4 * g + j
                            kb.tr(Tb[:, j * 128:(j + 1) * 128], xt[sl][:, kk * 128:(kk + 1) * 128], ident[:, :])
                        src = V(Tb.h[:, :], None)
                        dst = xT[sl].k(g)[:, g * 512:(g + 1) * 512]
                        q = kb.evac_q()
                        rd = [Tb[:, 0:1] for j in range(4)]
                        if q == "act":
                            S.op("act", lambda e, d=dst, s=src: e.copy(d.ap, s.ap), reads=rd, writes=[dst])
                        else:
                            S.op("dve", lambda e, d=dst, s=src: e.tensor_copy(d.ap, s.ap), reads=rd, writes=[dst])
                    xTr = [xT[sl].k(kk // 4)[:, kk * 128:(kk + 1) * 128] for kk in range(16)]

                    for which, Fb in ((0, F0), (1, F1)):
                        if which == 0 and not full:
                            continue
                        for h in range(4):
                            c0 = which * 512 + h * 128
                            for kk in range(16):
                                kb.mm(Fb[:, h * 128:(h + 1) * 128], w_sb.k(kk)[:, kk, c0:c0 + 128], xTr[kk],
                                      start=(kk == 0), stop=(kk == 15))
                    for n, Mb in enumerate((M0, M1, M2)):
                        if n > 0 and not full:
                            continue
                        c0 = 1024 + n * 512
                        for kk in range(16):
                            kb.mm(Mb[:, :], xTr[kk], w_sb.k(kk)[:, kk, c0:c0 + 512], start=(kk == 0), stop=False)
                        kb.mm(Mb[:, :], ones_b[:, :], brow_b[:, n * 512:(n + 1) * 512], start=False, stop=True)

                    for h in range(4):
                        cs = slice(h * 128, (h + 1) * 128)
                        if full:
                            kb.act(qT[:, cs], F0[:, cs], AF.Silu, bias=bcol[:, h:h + 1])
                        kb.act(fT[:, cs], F1[:, cs], AF.Sigmoid, bias=bcol[:, 4 + h:5 + h])
                        kb.ts("dve", fT[:, cs], fT[:, cs], omlb[:, h:h + 1], ALU.mult, lbv[:, h:h + 1], ALU.add)
                        kb.ts("dve", kTt[:, cs], fT[:, cs], -1.0, ALU.mult, 1.0, ALU.add)
                    kb.act(fT[:, :], fT[:, :], AF.Ln)
                    for h in range(4):
                        cs = slice(h * 128, (h + 1) * 128)
                        S.op("dve", lambda e, cs=cs, cum=cum, fT=fT: e.tensor_tensor_scan(cum.h[:, cs], onesf.h[:, :], fT.h[:, cs], 0.0,
                                                                            ALU.mult, ALU.add),
                             reads=[onesf[:, :], fT[:, cs]], writes=[cum[:, cs]])
                        c63 = h * 128 + 63
                        cl = h * 128 + 127
                        kb.ts("dve", mid[:, 2 * h:2 * h + 1], cum[:, c63:c63 + 1], -1.0, ALU.mult)
                        if full:
                            kb.act(e1[:, cs], cum[:, cs], AF.Exp, bias=mid[:, 2 * h:2 * h + 1])
                        kb.act(e2[:, cs], cum[:, cs], AF.Exp, bias=cum[:, c63:c63 + 1], scale=-1.0)
                        if full:
                            kb.act(sc[:, 2 * h:2 * h + 1], cum[:, c63:c63 + 1], AF.Exp)
                        kb.act(sc[:, 2 * h + 1:2 * h + 2], cum[:, cl:cl + 1], AF.Exp, bias=mid[:, 2 * h:2 * h + 1])
                        kb.act(mid[:, 2 * h + 1:2 * h + 2], cum[:, cl:cl + 1], AF.Exp)
                        if full:
                            kb.tt("dve", Qt[:, cs], qT[:, cs], e1[:, cs], ALU.mult)
                            kb.ts("dve", Qc[:, cs], Qt[:, cs], sc[:, 2 * h:2 * h + 1], ALU.mult)
                        kb.tt("dve", Kt[:, cs], kTt[:, cs], e2[:, cs], ALU.mult)
                        kb.ts("dve", KlT[:, cs], Kt[:, cs], sc[:, 2 * h + 1:2 * h + 2], ALU.mult)
                    kb.copy("act", Vb[:, :], M0[:, :])
                    for h in range(4 if full else 0):
                        cs = slice(h * 128, (h + 1) * 128)
                        kb.mm(F0[:, cs], Kt[:, cs], Qt[:, cs], start=True, stop=True)
                        kb.tt("dve", AT[:, cs], F0[:, cs], cmask[:, :], ALU.mult)
                    for h in range(4):
                        cs = slice(h * 128, (h + 1) * 128)
                        kb.tr(F1[:, cs], KlT[:, cs], ident[:, :])
                        kb.copy("act", Kl[:, cs], F1[:, cs])
                    for h in range(4 if full else 0):
                        cs = slice(h * 128, (h + 1) * 128)
                        kb.mm(PO[:, cs], AT[:, cs], Vb[:, cs], start=True, stop=False)
                        kb.mm(PO[:, cs], Qc[:, cs], Sb[:, cs], start=False, stop=True)
                    for h in range(4):
                        cs = slice(h * 128, (h + 1) * 128)
                        kb.mm(M0[:, cs], Kl[:, cs], Vb[:, cs], start=True, stop=True)
                    for h in range(4):
                        cs = slice(h * 128, (h + 1) * 128)
                        kb.stt(St[:, cs], St[:, cs], mid[:, 2 * h + 1:2 * h + 2], M0[:, cs], ALU.mult, ALU.add)
                    if not full:
                        kb.ts("dve", St[:, :], St[:, :], keep[:, t:t + 1], ALU.mult)
                    kb.copy("act", Sb[:, :], St[:, :])
                    if not full:
                        continue
                    kb.act(gate[:, :], M1[:, :], AF.Sigmoid)
                    kb.act(g2[:, :], M2[:, :], AF.Sigmoid)
                    kb.tt("dve", gate[:, :], gate[:, :], g2[:, :], ALU.mult)
                    kb.tt("pool", gate[:, :], gate[:, :], hng[:, :], ALU.mult)
                    for h in range(4):
                        cs = slice(h * 128, (h + 1) * 128)
                        kb.act(osq[:, cs], PO[:, cs], AF.Square, accum=ss[:, h:h + 1])
                    kb.ts("dve", rstd[:, :], ss[:, :], 1.0 / 128.0, ALU.mult, EPS, ALU.add)
                    kb.act(rstd[:, :], rstd[:, :], AF.Sqrt)
                    S.op("dve", lambda e, rstd=rstd: e.reciprocal(rstd.h[:, :], rstd.h[:, :]), reads=[rstd[:, :]], writes=[rstd[:, :]])
                    o_t = ot[sl]
                    for h in range(4):
                        cs = slice(h * 128, (h + 1) * 128)
                        kb.stt(o_t[:, cs], PO[:, cs], rstd[:, h:h + 1], gate[:, cs], ALU.mult, ALU.mult)
                    kb.dma(V(gh_out[(t - NPRE) * 128:(t - NPRE + 1) * 128, :], f"gh{t}"), o_t[:, :], f"o{sl}")

                bcg = kb.sb([128, 8], F32, "bcg")
                wa2 = kb.sb([16, 256], F32, "wa2_sb")
                gng = kb.sb([128, 512], F32, "gng")
                ga1T = kb.sb([16, 128], F32, "ga1T")
                ght = [kb.sb([128, 512], F32, f"ght{i}") for i in range(2)]
                kb.dma(bcg[:, :], V(bcol_g, None), "c0")
                kb.memset("dve", brow[:, :], 0.0)
                kb.dma(brow[0:1, :], V(brow_g, None), "c1")
                kb.dma(wa2[:, :], V(wa2_in, None), "c2")
                kb.dma(gng[:, :], V(gng_rep, None), "c3")
                kb.copy("dve", brow_b[:, :], brow[:, :])
                kb.ts("dve", bcg[:, 0:2], bcg[:, 0:2], 1.0 / 16.0, ALU.mult)
                kb.ts("dve", bcg[:, 5:7], bcg[:, 5:7], -1.0, ALU.mult)
                _load_weights_bf16(kb, w_g, 2064, w_sb, stage, "ws")
                kb.memset("dve", St[:, :], 0.0)
                kb.memset("dve", Sb[:, :], 0.0)
                St2 = kb.sb([128, 512], F32, "St2")
                Sb2 = kb.sb([128, 512], BF16, "Sb2")
                kb.memset("dve", St2[:, :], 0.0)
                kb.memset("dve", Sb2[:, :], 0.0)
                Sts, Sbs = (St, St2), (Sb, Sb2)

                kb.dma(xt[0][:, :], V(x_b[0:128, :], None), "x0")
                for t in range(nt):
                    sl = t % 2
                    full = (t >= NPRE)
                    (qT, fT, kTt, cum, e1, e2, mid, sc, Qt, Qc, Kt, KlT, Kl, Vb, AT, gate, g2, osq, ss, rstd) = WS[sl]
                    if full or sl == 0:
                        F0, F1, M0 = bF0, bF1, bM0
                    else:
                        F0, F1, M0 = bM1, bM2, bPO
                    if t + 1 < nt:
                        kb.dma(xt[1 - sl][:, :], V(x_b[(t + 1) * 128:(t + 2) * 128, :], None), f"x{1 - sl}")
                    if full:
                        kb.dma(ght[sl][:, :], V(gh_out[(t - NPRE) * 128:(t - NPRE + 1) * 128, :], f"gh{t}"), f"g{sl}")
                    for g in range(4):
                        Tb = T0 if g % 2 == 0 else T1
                        for j in range(4):
                            kk = 4 * g + j
                            kb.tr(Tb[:, j * 128:(j + 1) * 128], xt[sl][:, kk * 128:(kk + 1) * 128], ident[:, :])
                        dst = xT[sl].k(g)[:, g * 512:(g + 1) * 512]
                        kb.copy(kb.evac_q(), dst, Tb[:, :])
                    xTr = [xT[sl].k(kk // 4)[:, kk * 128:(kk + 1) * 128] for kk in range(16)]
                    for u in range(4):
                        if u < 2 and not full:
                            continue
                        for kk in range(16):
                            kb.mm(F0[:, u * 128:(u + 1) * 128], w_sb.k(kk)[:, kk, u * 128:(u + 1) * 128], xTr[kk],
                                  start=(kk == 0), stop=(kk == 15))
                    for kk in range(16):
                        kb.mm(F1[0:16, 0:128], w_sb.k(kk)[:, kk, 512:528], xTr[kk], start=(kk == 0), stop=(kk == 15))
                    for n, Mb in enumerate((M0, M1, M2)):
                        if n > 0 and not full:
                            continue
                        c0 = 528 + n * 512
                        for kk in range(16):
                            kb.mm(Mb[:, :], xTr[kk], w_sb.k(kk)[:, kk, c0:c0 + 512], start=(kk == 0), stop=False)
                        kb.mm(Mb[:, :], ones_b[:, :], brow_b[:, n * 512:(n + 1) * 512], start=False, stop=True)
                    kb.act(ga1T[:, :], F1[0:16, 0:128], AF.Identity, bias=bcg[0:16, 4:5])
                    for u in range(2):
                        kb.mm(F1[:, 128 + u * 128:256 + u * 128], wa2[:, u * 128:(u + 1) * 128], ga1T[:, :],
                              start=True, stop=True)
                    for u in range(2):
                        cs = slice(u * 128, (u + 1) * 128)
                        if full:
                            kb.act(qT[:, cs], F0[:, cs], AF.Identity, bias=bcg[:, u:u + 1], scale=1.0 / 16.0)
                        kb.act(kTt[:, cs], F0[:, 256 + u * 128:384 + u * 128], AF.Identity, bias=bcg[:, 2 + u:3 + u])
                        kb.act(fT[:, cs], F1[:, 128 + u * 128:256 + u * 128], AF.Exp, bias=bcg[:, 5 + u:6 + u], scale=-1.0)
                    kb.act(fT[:, 0:256], fT[:, 0:256], AF.Ln, bias=1.0)
                    kb.ts("dve", fT[:, 0:256], fT[:, 0:256], -1.0 / 16.0, ALU.mult)
                    for h in range(2):
                        cs = slice(h * 128, (h + 1) * 128)
                        S.op("dve", lambda e, cs=cs, cum=cum, fT=fT: e.tensor_tensor_scan(cum.h[:, cs], onesf.h[:, :], fT.h[:, cs], 0.0,
                                                                            ALU.mult, ALU.add),
                             reads=[onesf[:, :], fT[:, cs]], writes=[cum[:, cs]])
                        c63 = h * 128 + 63
                        cl = h * 128 + 127
                        kb.ts("dve", mid[:, 2 * h:2 * h + 1], cum[:, c63:c63 + 1], -1.0, ALU.mult)
                        if full:
                            kb.act(e1[:, cs], cum[:, cs], AF.Exp, bias=mid[:, 2 * h:2 * h + 1])
                        kb.act(e2[:, cs], cum[:, cs], AF.Exp, bias=cum[:, c63:c63 + 1], scale=-1.0)
                        if full:
                            kb.act(sc[:, 2 * h:2 * h + 1], cum[:, c63:c63 + 1], AF.Exp)
                        kb.act(sc[:, 2 * h + 1:2 * h + 2], cum[:, cl:cl + 1], AF.Exp, bias=mid[:, 2 * h:2 * h + 1])
                        kb.act(mid[:, 2 * h + 1:2 * h + 2], cum[:, cl:cl + 1], AF.Exp)
                        if full:
                            kb.tt("dve", Qt[:, cs], qT[:, cs], e1[:, cs], ALU.mult)
                            kb.ts("dve", Qc[:, cs], Qt[:, cs], sc[:, 2 * h:2 * h + 1], ALU.mult)
                        kb.tt("dve", Kt[:, cs], kTt[:, cs], e2[:, cs], ALU.mult)
                        kb.ts("dve", KlT[:, cs], Kt[:, cs], sc[:, 2 * h + 1:2 * h + 2], ALU.mult)
                    kb.copy("act", Vb[:, :], M0[:, :])
                    for u in range(2 if full else 0):
                        cs = slice(u * 128, (u + 1) * 128)
                        kb.mm(F0[:, 0:128], Kt[:, cs], Qt[:, cs], start=(u == 0), stop=(u == 1))
                    if full:
                        kb.tt("dve", AT[:, 0:128], F0[:, 0:128], cmask[:, :], ALU.mult)
                    for u in range(2):
                        cs = slice(u * 128, (u + 1) * 128)
                        kb.tr(F1[:, cs], KlT[:, cs], ident[:, :])
                    kb.copy("act", Kl[:, 0:256], F1[:, 0:256])
                    if full:
                        kb.mm(PO[:, :], AT[:, 0:128], Vb[:, :], start=True, stop=False)
                    for u in range(2 if full else 0):
                        cs = slice(u * 128, (u + 1) * 128)
                        kb.mm(PO[:, :], Qc[:, cs], Sbs[u][:, :], start=False, stop=(u == 1))
                    for u, Pb in enumerate((M0, F0)):
                        cs = slice(u * 128, (u + 1) * 128)
                        kb.mm(Pb[:, :], Kl[:, cs], Vb[:, :], start=True, stop=True)
                    for u, Pb in enumerate((M0, F0)):
                        kb.stt(Sts[u][:, :], Sts[u][:, :], mid[:, 2 * u + 1:2 * u + 2], Pb[:, :], ALU.mult, ALU.add)
                        if not full:
                            kb.ts("dve", Sts[u][:, :], Sts[u][:, :], keep[:, t:t + 1], ALU.mult)
                        kb.copy("act", Sbs[u][:, :], Sts[u][:, :])
                    if not full:
                        continue
                    kb.act(gate[:, :], M1[:, :], AF.Silu)
                    kb.act(g2[:, :], M2[:, :], AF.Sigmoid)
                    kb.tt("dve", gate[:, :], gate[:, :], g2[:, :], ALU.mult)
                    kb.tt("pool", gate[:, :], gate[:, :], gng[:, :], ALU.mult)
                    kb.act(osq[:, :], PO[:, :], AF.Square, accum=ss[:, 0:1])
                    kb.ts("dve", rstd[:, 0:1], ss[:, 0:1], 1.0 / 512.0, ALU.mult, EPS, ALU.add)
                    kb.act(rstd[:, 0:1], rstd[:, 0:1], AF.Sqrt)
                    S.op("dve", lambda e, rstd=rstd: e.reciprocal(rstd.h[:, 0:1], rstd.h[:, 0:1]), reads=[rstd[:, :]], writes=[rstd[:, :]])
                    o_t = ot[sl]
                    kb.stt(o_t[:, :], PO[:, :], rstd[:, 0:1], gate[:, :], ALU.mult, ALU.mult)
                    kb.tt("dve", o_t[:, :], o_t[:, :], ght[sl][:, :], ALU.add)
                    kb.dma(V(mg_out[(t - NPRE) * 128:(t - NPRE + 1) * 128, :], "merged"), o_t[:, :], f"o{sl}")
            S.emit()
            pA.close()
        if doB:
            phases_bcd(nc, S, kb, C, (T0, T1, F0, F1, M0, M1, M2, PO), io, dbg)
    return nc


def _prep_core_inputs(inp, c):
    b, hh = c // 4, c % 4
    w_in = inp["w_in"][0]
    b_in = inp["b_in"][0]
    offs = np.cumsum([0, 1024, 1024, 2048, 2048, 16, 2048, 2048, 2048, 2048, 2048, 2048])
    o_gq, o_gk, o_gv, o_gr, o_ga1, o_hq, o_hf, o_hi, o_hg, o_ma, o_mb = offs[:11]
    ch = slice(512 * hh, 512 * hh + 512)

    def cols(o, s):
        return np.arange(o + s.start, o + s.stop)
    hcols = np.concatenate([cols(o_hq, ch), cols(o_hf, ch), cols(o_hi, ch), cols(o_hg, ch), cols(o_mb, ch)])
    w_h = np.ascontiguousarray(w_in[:, hcols])
    bh = b_in[hcols]
    bcol_h = np.ascontiguousarray(bh[:1024].reshape(8, 128).T)
    brow_h = np.ascontiguousarray(bh[1024:].reshape(1, 1536))
    lb = inp["hgrn_lb_logits"][:, ch]
    lbl = np.ascontiguousarray(np.concatenate([lb[0].reshape(4, 128).T, lb[1].reshape(4, 128).T], axis=1))
    hng_rep = np.ascontiguousarray(np.broadcast_to(np.tile(inp["hgrn_norm_g"][0], 4)[None, :], (128, 512)))
    kq = slice(256 * hh, 256 * hh + 256)
    gcols = np.concatenate([cols(o_gq, kq), cols(o_gk, kq), np.arange(o_ga1, o_ga1 + 16), cols(o_gv, ch),
                            cols(o_gr, ch), cols(o_ma, ch)])
    w_g = np.ascontiguousarray(w_in[:, gcols])
    bg = b_in[gcols]
    bcol_g = np.zeros((128, 8), np.float32)
    bcol_g[:, 0:4] = bg[:512].reshape(4, 128).T
    bcol_g[:16, 4] = bg[512:528]
    bcol_g[:, 5:7] = inp["b_gla_a"][0][kq].reshape(2, 128).T
    brow_g = np.ascontiguousarray(bg[528:].reshape(1, 1536))
    wa2 = np.ascontiguousarray(inp["w_gla_a2"][0][:, kq])
    gng_rep = np.ascontiguousarray(np.broadcast_to(inp["gla_norm_g"][0][None, :], (128, 512)))
    return dict(x_b=np.ascontiguousarray(inp["x"][b]), w_h=w_h, bcol_h=bcol_h, brow_h=brow_h, lbl=lbl,
                hng_rep=hng_rep, w_g=w_g, bcol_g=bcol_g, brow_g=brow_g, wa2=wa2, gng_rep=gng_rep)


def _prep_a_inputs(inp, b, ng=4, j=0, npb=1):
    parts = [_prep_core_inputs(inp, 4 * b + hh) for hh in range(ng)]
    ntok = SEQ // npb
    own_end = (j + 1) * ntok
    start = own_end - SEQ
    xw = np.zeros((SEQ, D), np.float32)
    xw[max(0, -start):] = inp["x"][b, max(0, start):own_end]
    keep = ((start + 128 * np.arange(NT_SEQ)) >= 0).astype(np.float32)
    out = dict(x_b=xw, hng_rep=parts[0]["hng_rep"], gng_rep=parts[0]["gng_rep"],
               keep_rep=np.ascontiguousarray(np.broadcast_to(keep[None, :], (128, NT_SEQ))))
    for k in ("w_h", "bcol_h", "brow_h", "lbl", "w_g", "bcol_g", "brow_g", "wa2"):
        out[k] = np.ascontiguousarray(np.concatenate([p[k] for p in parts], axis=0))
    return out


def _prep_bcd_inputs(inp, b):
    def rep(v):
        return np.ascontiguousarray(np.broadcast_to(v[None, :], (128, v.shape[0])))
    bgu = inp["b_gate_up"][0]
    bg = bgu[:, 0::2].reshape(32, 16, 128)
    bl = bgu[:, 1::2].reshape(32, 16, 128)
    bgu_col = np.ascontiguousarray(np.concatenate([bg, bl], axis=1).transpose(2, 0, 1).reshape(128, 1024))
    return dict(
        x_b=np.ascontiguousarray(inp["x"][b]),
        ln1g=rep(inp["ln1_g"][0]), ln1b=rep(inp["ln1_b"][0]), ln2g=rep(inp["ln2_g"][0]), ln2b=rep(inp["ln2_b"][0]),
        ln3g=rep(inp["ln3_g"][0]), ln3b=rep(inp["ln3_b"][0]),
        w_o=inp["w_mix_o"][0], w_kv=inp["w_mem_kv"][0], w_xq=inp["w_xq"][0], w_xo=inp["w_xo"][0],
        mem_b=np.ascontiguousarray(inp["mem"][b]), w_router=inp["w_router"][0], br_rep=rep(inp["b_router"][0]),
        bgu_col=bgu_col, b_dn=inp["b_down"][0],
        w_gu=inp["w_gate_up"][0].reshape(32 * D, 2 * D), w_dn=inp["w_down"][0].reshape(32 * D, D))


NPB_FULL = 4


def kernel(**inputs):
    inp = {k: np.asarray(v) for k, v in inputs.items()}
    npb = NPB_FULL
    nc = build_program("full", npb=npb)
    in_maps = []
    for b in range(2):
        bcd = _prep_bcd_inputs(inp, b)
        for j in range(npb):
            m = dict(bcd)
            m.update(_prep_a_inputs(inp, b, 4, j, npb))
            in_maps.append(m)
    n = 2 * npb
    res = run_bass_kernel_spmd(nc, in_maps, core_ids=list(range(n)))
    out = np.stack([np.asarray(r["out"]) for r in res.results], axis=0)
    return out.reshape(2, SEQ, D).astype(np.float32)
```

```python
import contextlib
import numpy as np
import concourse.bass as bass
import concourse.mybir as mybir
from concourse.bass_utils import run_bass_kernel_spmd

F32 = mybir.dt.float32
BF16 = mybir.dt.bfloat16
I32 = mybir.dt.int32
U32 = mybir.dt.uint32
AF = mybir.ActivationFunctionType
ALU = mybir.AluOpType
AX = mybir.AxisListType

D = 2048
SEQ = 8192
NT_SEQ = SEQ // 128
EPS = 1e-5
ALPHA = 2.0 ** 0.25
SAME_ENGINE_SYNC = True


class V:
    __slots__ = ("ap", "key")

    def __init__(self, ap, key):
        self.ap = ap
        self.key = key


class Tile:
    def __init__(self, handle, key):
        self.h = handle
        self.key = key

    def __getitem__(self, idx):
        return V(self.h[idx], self.key)

    def k(self, sfx):
        return _Keyed(self.h, self.key + ":" + str(sfx))


class _Keyed:
    def __init__(self, h, key):
        self.h = h
        self.key = key

    def __getitem__(self, idx):
        return V(self.h[idx], self.key)


class Sched:
    QS = ("pe", "act", "dve", "pool", "sp")

    def __init__(self, nc, stack):
        self.nc = nc
        self.stack = stack
        self.items = {q: [] for q in self.QS}
        self.esem = {}
        for q in ("pe", "act", "dve", "pool"):
            self.esem[q] = stack.enter_context(nc.semaphore("es_" + q))
        self.cnt = {q: 0 for q in self.QS}
        self.waited = {q: {} for q in self.QS}
        self.res = {}
        self.dsem = {}
        self.semh = {"es_" + q: h for q, h in self.esem.items()}
        self.n_ops = 0

    def _deps(self, q, reads, writes):
        evs = []
        for r in reads:
            st = self.res.get(r)
            if st and st[0]:
                evs.append(st[0])
        for w in writes:
            st = self.res.get(w)
            if st:
                if st[0]:
                    evs.append(st[0])
                evs.extend(st[1])
        waits = {}
        for (sem, val, srcq) in evs:
            if srcq == q and (q == "pe" or not SAME_ENGINE_SYNC):
                continue
            if self.waited[q].get(sem, 0) >= val:
                continue
            if waits.get(sem, 0) < val:
                waits[sem] = val
        for sem, val in waits.items():
            self.waited[q][sem] = val
        return list(waits.items())

    def _commit(self, ev, reads, writes):
        for r in reads:
            if r in writes:
                continue
            self.res.setdefault(r, [None, []])[1].append(ev)
        for w in writes:
            self.res[w] = [ev, []]

    @staticmethod
    def _keys(views):
        return [v.key for v in views if v is not None and v.key is not None]

    def op(self, q, fn, reads=(), writes=()):
        rk, wk = self._keys(reads), self._keys(writes)
        waits = self._deps(q, rk, wk)
        self.cnt[q] += 1
        ev = ("es_" + q, self.cnt[q], q)
        self.items[q].append((waits, fn, ("es_" + q, 1)))
        self._commit(ev, rk, wk)
        self.n_ops += 1

    def dma(self, q, fn, sem, reads=(), writes=()):
        if sem not in self.dsem:
            h = self.stack.enter_context(self.nc.semaphore("ds_" + sem))
            self.dsem[sem] = [h, 0]
            self.semh["ds_" + sem] = h
        rk, wk = self._keys(reads), self._keys(writes)
        waits = self._deps(q, rk, wk)
        self.dsem[sem][1] += 16
        ev = ("ds_" + sem, self.dsem[sem][1], "dma")
        self.items[q].append((waits, fn, ("ds_" + sem, 16)))
        self._commit(ev, rk, wk)
        self.n_ops += 1

    def emit(self, final=False):
        nc = self.nc
        semh = self.semh
        items = self.items
        bar = [("es_" + q, self.cnt[q]) for q in ("pe", "act", "dve", "pool") if self.cnt[q] > 0]
        bar += [("ds_" + k, v[1]) for k, v in self.dsem.items() if v[1] > 0]

        def replay(q):
            def run(eng):
                for waits, fn, inc in items[q]:
                    for sem, val in waits:
                        eng.wait_ge(semh[sem], val)
                    ins = fn(eng)
                    ins.then_inc(semh[inc[0]], inc[1])
                for sem, val in bar:
                    if self.waited[q].get(sem, 0) < val:
                        eng.wait_ge(semh[sem], val)
                        self.waited[q][sem] = val
            return run

        with nc.Block() as block:
            block.sync(replay("sp"))
            block.tensor(replay("pe"))
            block.scalar(replay("act"))
            block.vector(replay("dve"))
            block.gpsimd(replay("pool"))
        self.items = {q: [] for q in self.QS}


class K:
    def __init__(self, nc, S, stack):
        self.nc, self.S, self.stack = nc, S, stack
        self._n = 0
        self.rr = 0
        self._tiles = {}

    def sb(self, shape, dt, name):
        if name in self._tiles:
            return self._tiles[name]
        h = self.stack.enter_context(self.nc.sbuf_tensor(name, list(shape), dt))
        t = Tile(h, name)
        self._tiles[name] = t
        return t

    def ps(self, name, dt=F32, cols=512):
        h = self.stack.enter_context(self.nc.psum_tensor(name, [128, cols], dt))
        return Tile(h, name)

    def mm(self, out, lhsT, rhs, start, stop):
        self.S.op("pe", lambda e: e.matmul(out.ap, lhsT.ap, rhs.ap, start=start, stop=stop),
                  reads=[lhsT, rhs] + ([] if start else [out]), writes=[out])

    def tr(self, out, in_, ident):
        self.S.op("pe", lambda e: e.transpose(out.ap, in_.ap, ident.ap), reads=[in_, ident], writes=[out])

    def act(self, out, in_, func, bias=None, scale=None, accum=None, q="act"):
        kw = {}
        rd = [in_]
        if bias is not None:
            if isinstance(bias, V):
                kw["bias"] = bias.ap
                rd.append(bias)
            else:
                kw["bias"] = float(bias)
        if scale is not None:
            if isinstance(scale, V):
                kw["scale"] = scale.ap
                rd.append(scale)
            else:
                kw["scale"] = float(scale)
        wr = [out]
        if accum is not None:
            kw["accum_out"] = accum.ap
            wr.append(accum)
        self.S.op("act", lambda e: e.activation(out.ap, in_.ap, func, **kw), reads=rd, writes=wr)

    def copy(self, q, out, in_):
        if q == "act":
            self.S.op("act", lambda e: e.copy(out.ap, in_.ap), reads=[in_], writes=[out])
        else:
            self.S.op(q, lambda e: e.tensor_copy(out.ap, in_.ap), reads=[in_], writes=[out])

    def tt(self, q, out, a, b, op):
        self.S.op(q, lambda e: e.tensor_tensor(out.ap, a.ap, b.ap, op), reads=[a, b], writes=[out])

    def ts(self, q, out, a, s1, op0, s2=None, op1=None, accum=None):
        rd = [a]
        s1v = s1.ap if isinstance(s1, V) else float(s1)
        if isinstance(s1, V):
            rd.append(s1)
        s2v = None
        if s2 is not None:
            s2v = s2.ap if isinstance(s2, V) else float(s2)
            if isinstance(s2, V):
                rd.append(s2)
        wr = [out]
        kw = {}
        if accum is not None:
            kw["accum_out"] = accum.ap
            wr.append(accum)
        o1 = op1 if op1 is not None else ALU.bypass
        self.S.op(q, lambda e: e.tensor_scalar(out.ap, a.ap, s1v, s2v, op0, o1, **kw), reads=rd, writes=wr)

    def stt(self, out, a, s, b, op0, op1):
        rd = [a, b]
        sv = s.ap if isinstance(s, V) else float(s)
        if isinstance(s, V):
            rd.append(s)
        self.S.op("dve", lambda e: e.scalar_tensor_tensor(out.ap, a.ap, sv, b.ap, op0, op1), reads=rd, writes=[out])

    def memset(self, q, out, val):
        self.S.op(q, lambda e: e.memset(out.ap, val), writes=[out])

    def dma(self, out, in_, sem, q="sp", **kw):
        self.S.dma(q, lambda e: e.dma_start(out.ap, in_.ap, **kw), sem, reads=[in_], writes=[out])

    def evac_q(self):
        self.rr += 1
        return "act" if self.rr % 2 else "dve"


def _consts(kb):
    nc, S = kb.nc, kb.S
    ones_f = kb.sb([128, 128], F32, "c_onesf")
    ident = kb.sb([128, 128], F32, "c_ident")
    cmask = kb.sb([128, 128], F32, "c_cmask")
    ones_b = kb.sb([128, 128], BF16, "c_onesb")
    kb.memset("pool", ones_f[:, :], 1.0)
    kb.memset("pool", ones_b[:, :], 1.0)
    S.op("pool", lambda e: e.affine_select(ident.h[:, :], ones_f.h[:, :], [[-1, 128]], ALU.is_equal, 0.0,
                                           base=0, channel_multiplier=1),
         reads=[ones_f[:, :]], writes=[ident[:, :]])
    S.op("pool", lambda e: e.affine_select(cmask.h[:, :], ones_f.h[:, :], [[1, 128]], ALU.is_ge, 0.0,
                                           base=0, channel_multiplier=-1),
         reads=[ones_f[:, :]], writes=[cmask[:, :]])
    return dict(ones_f=ones_f, ident=ident, cmask=cmask, ones_b=ones_b)


def _load_weights_bf16(kb, w_dram, ncols, w_sb, stage, tagsem):
    for k in range(16):
        st = stage[k % 2]
        kb.dma(st[:, :ncols], V(w_dram[k * 128:(k + 1) * 128, :], None), f"{tagsem}{k % 2}")
        q = ("act", "dve", "pool")[k % 3]
        kb.copy(q, w_sb.k(k)[:, k, :ncols], st[:, :ncols])


def _tr_tile(kb, src, dst, Pa, Pb, ident, ncols=128):
    for g in range(4):
        Tb = Pa if g % 2 == 0 else Pb
        for j in range(4):
            kk = 4 * g + j
            kb.tr(Tb[:, j * 128:(j + 1) * 128], src[:, kk * 128:(kk + 1) * 128], ident[:, :])
        kb.copy(kb.evac_q(), dst[:, g * 512:(g + 1) * 512], Tb[:, :])


def _ln_tile(kb, y, g_rep, b_rep, out, stats, mv, r):
    S = kb.S
    for c in range(4):
        S.op("dve", lambda e, c=c: e.bn_stats(stats.h[:, c * 6:(c + 1) * 6], y.h[:, c * 512:(c + 1) * 512]),
             reads=[y[:, :]], writes=[stats[:, :]])
    S.op("dve", lambda e: e.bn_aggr(mv.h[:, 0:2], stats.h[:, 0:24]), reads=[stats[:, :]], writes=[mv[:, :]])
    kb.ts("dve", r[:, :], mv[:, 1:2], EPS, ALU.add)
    kb.act(r[:, :], r[:, :], AF.Sqrt)
    S.op("dve", lambda e: e.reciprocal(r.h[:, :], r.h[:, :]), reads=[r[:, :]], writes=[r[:, :]])
    kb.ts("dve", out[:, :], y[:, :], mv[:, 0:1], ALU.subtract, r[:, 0:1], ALU.mult)
    kb.tt("pool", out[:, :], out[:, :], g_rep[:, :], ALU.mult)
    kb.tt("pool", out[:, :], out[:, :], b_rep[:, :], ALU.add)


def _load_w(kb, w_ap, ncols, w_sb, stage, tagsem, col0=0):
    for k in range(16):
        st = stage[k % 2]
        kb.dma(st[:, :ncols], V(w_ap[k * 128:(k + 1) * 128, col0:col0 + ncols], None), f"{tagsem}{k % 2}")
        q = ("act", "dve", "pool")[k % 3]
        kb.copy(q, w_sb.k(k)[:, k, :ncols], st[:, :ncols])


NTT = 64
NTOK = NTT * 128
CAP = 1280
NJ = CAP // 128
RGS = ((0, 512), (512, 512), (1024, 256))
YROWS = 1 + 4 * NTOK + 128


def phases_bcd(nc, S, kb, C, P, io, dbg):
    ident, ones_b, ones_f = C["ident"], C["ones_b"], C["ones_f"]
    T0, T1, F0, F1, M0, M1, M2, PO = P
    mg, x_tok, out_ap = io["merged"], io["x_tok"], io["out"]
    h1s, h2s, ybufs = io["h1s"], io["h2s"], io["ybuf"]
    n_exp = io.get("n_exp", 32)

    kb.stack = S.stack
    idx_tok = kb.sb([128, 32 * NJ], I32, "R_idxtok")
    dest = kb.sb([128, 32 * NJ], I32, "R_dest")
    gl = kb.sb([128, 32 * NJ], F32, "R_gl")

    pB = contextlib.ExitStack()
    kb.stack = pB
    wsb = kb.sb([128, 16, 2048], BF16, "B_w")
    stage = [kb.sb([128, 2048], F32, f"B_st{i}") for i in range(2)]
    g_rep = kb.sb([128, 2048], F32, "B_g")
    b_rep = kb.sb([128, 2048], F32, "B_b")
    mt = [kb.sb([128, 2048], F32, f"B_mt{i}") for i in range(2)]
    xk = [kb.sb([128, 2048], F32, f"B_xk{i}") for i in range(2)]
    mT = [kb.sb([128, 2048], BF16, f"B_mT{i}") for i in range(2)]
    yt = [kb.sb([128, 2048], F32, f"B_y{i}") for i in range(2)]
    stats = kb.sb([128, 24], F32, "B_stats")
    mv = kb.sb([128, 2], F32, "B_mv")
    rr = kb.sb([128, 1], F32, "B_r")
    kb.dma(g_rep[:, :], V(io["ln1g"], None), "c0")
    kb.dma(b_rep[:, :], V(io["ln1b"], None), "c1")
    _load_w(kb, io["w_o"], 2048, wsb, stage, "ws")
    kb.dma(mt[0][:, :], V(mg[0:128, :], "merged"), "mt0")
    kb.dma(xk[0][:, :], V(x_tok[0:128, :], None), "xk0")
    for i in range(NTT):
        sl = i % 2
        if i + 1 < NTT:
            kb.dma(mt[1 - sl][:, :], V(mg[(i + 1) * 128:(i + 2) * 128, :], "merged"), f"mt{1 - sl}")
            kb.dma(xk[1 - sl][:, :], V(x_tok[(i + 1) * 128:(i + 2) * 128, :], None), f"xk{1 - sl}")
        _tr_tile(kb, mt[sl], mT[sl], T0, T1, ident)
        for n, Pb in enumerate((F0, F1, M0, M1)):
            for kk in range(16):
                kb.mm(Pb[:, :], mT[sl][:, kk * 128:(kk + 1) * 128], wsb.k(kk)[:, kk, n * 512:(n + 1) * 512],
                      start=(kk == 0), stop=(kk == 15))
            kb.stt(yt[sl][:, n * 512:(n + 1) * 512], xk[sl][:, n * 512:(n + 1) * 512], ALPHA, Pb[:, :],
                   ALU.mult, ALU.add)
        _ln_tile(kb, yt[sl], g_rep, b_rep, yt[sl], stats, mv, rr)
        kb.dma(V(h1s[i * 128:(i + 1) * 128, :], "h1s"), yt[sl][:, :], f"ho{sl}")
    S.emit()
    pB.close()

    pR = contextlib.ExitStack()
    kb.stack = pR
    gate_all = kb.sb([128, NTT, 32], F32, "R_gate")

    pB = contextlib.ExitStack()
    kb.stack = pB
    wq = kb.sb([128, 16, 2048], BF16, "C_wq")
    wo = kb.sb([128, 16, 2048], BF16, "C_wo")
    g_rep = kb.sb([128, 2048], F32, "C_g")
    b_rep = kb.sb([128, 2048], F32, "C_b")
    big = kb.sb([128, 4096], BF16, "C_big")
    memT = Tile(big.h[:, :].rearrange("p (k m) -> p k m", k=16), "C_memT")
    KT = kb.sb([128, 16, 256], BF16, "C_KT")
    Vs = kb.sb([128, 2, 2048], BF16, "C_V")
    ht0 = kb.sb([128, 2048], F32, "C_ht0")
    ht = [ht0, ht0]
    stage = ht
    hT = kb.sb([128, 2048], BF16, "C_hT")
    qT = Tile(big.h[:, 0:2048], "C_qT")
    pT = Tile(big.h[:, 2048:3072], "C_pT")
    oT = hT
    yt = kb.sb([128, 2048], F32, "C_y")
    memt = yt
    pf = Tile(yt.h[:, 0:1024], "C_y")
    wr = kb.sb([128, 16, 32], F32, "C_wr")
    br = kb.sb([128, 32], F32, "C_br")
    stats = kb.sb([128, 24], F32, "C_stats")
    mv = kb.sb([128, 2], F32, "C_mv")
    rr = kb.sb([128, 1], F32, "C_r")
    mx = kb.sb([128, 4], F32, "C_mx")
    sm = kb.sb([128, 4], F32, "C_sm")
    m8 = kb.sb([128, 8], F32, "C_m8")
    ex = kb.sb([128, 32], F32, "C_ex")
    s1 = kb.sb([128, 2], F32, "C_s1")
    mk = kb.sb([128, 32], F32, "C_mk")
    kb.dma(g_rep[:, :], V(io["ln2g"], None), "c0")
    kb.dma(b_rep[:, :], V(io["ln2b"], None), "c1")
    kb.dma(wr[:, :, :], V(io["w_router"].rearrange("(k p) e -> p k e", p=128), None), "c2")
    kb.dma(br[:, :], V(io["br_rep"], None), "c3")
    for mtile in range(2):
        kb.dma(memt[:, :], V(io["mem_b"][mtile * 128:(mtile + 1) * 128, :], None), "mm")
        for g in range(4):
            Tb = T0 if g % 2 == 0 else T1
            for j in range(4):
                kk = 4 * g + j
                kb.tr(Tb[:, j * 128:(j + 1) * 128], memt[:, kk * 128:(kk + 1) * 128], ident[:, :])
            S.op("dve", lambda e, Tb=Tb, g=g, mtile=mtile: e.tensor_copy(
                memT.h[:, 4 * g:4 * g + 4, mtile * 128:(mtile + 1) * 128],
                Tb.h[:, :].rearrange("p (a b) -> p a b", a=4)), reads=[Tb[:, :]], writes=[memT[:, :, :]])
    _load_w(kb, io["w_kv"], 2048, wq, stage, "ws", col0=0)
    banks = (F0, F1, M0, M1)
    for c in range(16):
        Pb = banks[(c // 2) % 4]
        cs = slice((c % 2) * 256, (c % 2) * 256 + 256)
        for kk in range(16):
            kb.mm(Pb[:, cs], wq.k(kk)[:, kk, c * 128:(c + 1) * 128], memT[:, kk, :], start=(kk == 0), stop=(kk == 15))
        if c % 2 == 1:
            S.op("act", lambda e, Pb=Pb, c=c: e.copy(KT.h[:, c - 1:c + 1, :], Pb.h[:, :].rearrange("p (a b) -> p a b", a=2)),
                 reads=[Pb[:, :]], writes=[KT[:, :, :]])
    _load_w(kb, io["w_kv"], 2048, wq, stage, "ws", col0=2048)
    for mtile in range(2):
        for n in range(4):
            Pb = banks[n]
            for kk in range(16):
                kb.mm(Pb[:, :], memT[:, kk, mtile * 128:(mtile + 1) * 128], wq.k(kk)[:, kk, n * 512:(n + 1) * 512],
                      start=(kk == 0), stop=(kk == 15))
            kb.copy(kb.evac_q(), Vs[:, mtile, n * 512:(n + 1) * 512], Pb[:, :])
    _load_w(kb, io["w_xq"], 2048, wq, stage, "ws")
    _load_w(kb, io["w_xo"], 2048, wo, stage, "ws")
    SC = 512.0 ** -0.5
    for i in range(NTT):
        sl = i % 2
        kb.dma(ht[sl][:, :], V(h1s[i * 128:(i + 1) * 128, :], "h1s"), "ht")
        _tr_tile(kb, ht[sl], hT, T0, T1, ident)
        for c in range(16):
            Pb = banks[c // 4]
            cs = slice((c % 4) * 128, (c % 4) * 128 + 128)
            for kk in range(16):
                kb.mm(Pb[:, cs], wq.k(kk)[:, kk, c * 128:(c + 1) * 128], hT[:, kk * 128:(kk + 1) * 128],
                      start=(kk == 0), stop=(kk == 15))
            if c % 4 == 3:
                g = c // 4
                kb.act(qT[:, g * 512:(g + 1) * 512], Pb[:, :], AF.Identity, scale=SC)
        for hx in range(4):
            Pb = M2 if hx < 2 else PO
            cs = slice((hx % 2) * 256, (hx % 2) * 256 + 256)
            for cc in range(4):
                c = hx * 4 + cc
                kb.mm(Pb[:, cs], qT[:, c * 128:(c + 1) * 128], KT[:, c, :], start=(cc == 0), stop=(cc == 3))
        for hx in range(4):
            Pb = M2 if hx < 2 else PO
            cs = slice((hx % 2) * 256, (hx % 2) * 256 + 256)
            S.op("dve", lambda e, Pb=Pb, cs=cs, hx=hx: e.reduce_max(mx.h[:, hx:hx + 1], Pb.h[:, cs], AX.X),
                 reads=[Pb[:, :]], writes=[mx[:, :]])
        kb.ts("dve", mx[:, :], mx[:, :], -1.0, ALU.mult)
        for hx in range(4):
            Pb = M2 if hx < 2 else PO
            cs = slice((hx % 2) * 256, (hx % 2) * 256 + 256)
            kb.act(pf[:, hx * 256:(hx + 1) * 256], Pb[:, cs], AF.Exp, bias=mx[:, hx:hx + 1], accum=sm[:, hx:hx + 1])
        S.op("dve", lambda e: e.reciprocal(sm.h[:, :], sm.h[:, :]), reads=[sm[:, :]], writes=[sm[:, :]])
        for hx in range(4):
            kb.ts("dve", pf[:, hx * 256:(hx + 1) * 256], pf[:, hx * 256:(hx + 1) * 256], sm[:, hx:hx + 1], ALU.mult)
        for g in range(2):
            Tb = T0 if g == 0 else T1
            for j in range(4):
                kk = 4 * g + j
                kb.tr(Tb[:, j * 128:(j + 1) * 128], pf[:, kk * 128:(kk + 1) * 128], ident[:, :])
            kb.copy(kb.evac_q(), pT[:, g * 512:(g + 1) * 512], Tb[:, :])
        for c in range(16):
            Pb = banks[c // 4]
            cs = slice((c % 4) * 128, (c % 4) * 128 + 128)
            hx = c // 4
            for mc in range(2):
                kb.mm(Pb[:, cs], Vs[:, mc, c * 128:(c + 1) * 128], pT[:, (hx * 2 + mc) * 128:(hx * 2 + mc + 1) * 128],
                      start=(mc == 0), stop=(mc == 1))
            if c % 4 == 3:
                g = c // 4
                kb.copy(kb.evac_q(), oT[:, g * 512:(g + 1) * 512], Pb[:, :])
        for n in range(4):
            Pb = banks[n]
            for kk in range(16):
                kb.mm(Pb[:, :], oT[:, kk * 128:(kk + 1) * 128], wo.k(kk)[:, kk, n * 512:(n + 1) * 512],
                      start=(kk == 0), stop=(kk == 15))
            kb.stt(yt[:, n * 512:(n + 1) * 512], ht[sl][:, n * 512:(n + 1) * 512], ALPHA, Pb[:, :], ALU.mult, ALU.add)
        _ln_tile(kb, yt, g_rep, b_rep, yt, stats, mv, rr)
        kb.dma(V(h2s[i * 128:(i + 1) * 128, :], "h2s"), yt[:, :], "ho")
        h2T = ht[sl]
        for g in range(4):
            Tb = T0 if g % 2 == 0 else T1
            for j in range(4):
                kk = 4 * g + j
                kb.tr(Tb[:, j * 128:(j + 1) * 128], yt[:, kk * 128:(kk + 1) * 128], ident[:, :])
            kb.copy(kb.evac_q(), h2T[:, g * 512:(g + 1) * 512], Tb[:, :])
        for kk in range(16):
            kb.mm(M2[:, 0:32], h2T[:, kk * 128:(kk + 1) * 128], wr[:, kk, :], start=(kk == 0), stop=(kk == 15))
        lg = ex
        kb.tt("dve", lg[:, :], M2[:, 0:32], br[:, :], ALU.add)
        S.op("dve", lambda e: e.max(m8.h[:, :], lg.h[:, :]), reads=[lg[:, :]], writes=[m8[:, :]])
        kb.ts("dve", mk[:, :], lg[:, :], m8[:, 3:4], ALU.is_ge)
        kb.ts("dve", s1[:, 0:1], m8[:, 0:1], -1.0, ALU.mult)
        kb.act(lg[:, :], lg[:, :], AF.Exp, bias=s1[:, 0:1])
        kb.tt("dve", lg[:, :], lg[:, :], mk[:, :], ALU.mult)
        S.op("dve", lambda e: e.reduce_sum(s1.h[:, 1:2], lg.h[:, :], AX.X), reads=[lg[:, :]], writes=[s1[:, :]])
        S.op("dve", lambda e: e.reciprocal(s1.h[:, 1:2], s1.h[:, 1:2]), reads=[s1[:, :]], writes=[s1[:, :]])
        kb.ts("dve", gate_all[:, i, :], lg[:, :], s1[:, 1:2], ALU.mult)
    S.emit()
    pB.close()

    pB = contextlib.ExitStack()
    kb.stack = pB
    mask_bf = kb.sb([128, NTT, 32], BF16, "D_maskbf")
    SU = kb.sb([128, 128], BF16, "D_SU")
    pos = kb.sb([128, NTT, 32], F32, "D_pos")
    slot = kb.sb([128, NTT, 32], F32, "D_slot")
    toki = kb.sb([128, NTT], I32, "D_toki")
    tokf = kb.sb([128, NTT], F32, "D_tokf")
    tokp1 = kb.sb([128, NTT], F32, "D_tokp1")
    pay = kb.sb([128, NTT, 32, 3], F32, "D_pay")
    io_i = kb.sb([128, CAP], I32, "D_ioi")
    io_f = kb.sb([128, CAP], F32, "D_iof")
    tr_i = kb.sb([128, 1], I32, "D_tri")
    tr_f = kb.sb([128, 1], F32, "D_trf")
    zer = kb.sb([128, 128], F32, "D_zer")
    Sel = [kb.sb([128, CAP], F32, f"D_sel{i}") for i in range(3)]
    lst = kb.sb([128, 32, NJ * 3], F32, "D_lst")
    tmpd = kb.sb([128, 32 * NJ], F32, "D_tmpd")
    mask_all = kb.sb([128, NTT, 32], F32, "D_mask")
    kb.memset("pool", zer[:, :], 0.0)
    kb.ts("dve", mask_all[:, :, :], gate_all[:, :, :], 0.0, ALU.is_gt)
    kb.copy("dve", mask_bf[:, :, :], mask_all[:, :, :])
    S.op("pool", lambda e: e.affine_select(SU.h[:, :], ones_f.h[:, :], [[1, 128]], ALU.is_ge, 0.0,
                                           base=-1, channel_multiplier=-1), reads=[ones_f[:, :]], writes=[SU[:, :]])
    S.op("pool", lambda e: e.iota(toki.h[:, :], [[128, NTT]], base=0, channel_multiplier=1), writes=[toki[:, :]])
    S.op("pool", lambda e: e.iota(io_i.h[:, :], [[1, CAP]], base=0, channel_multiplier=0), writes=[io_i[:, :]])
    S.op("pool", lambda e: e.iota(tr_i.h[:, :], [[0, 1]], base=1 + 4 * NTOK, channel_multiplier=1), writes=[tr_i[:, :]])
    kb.copy("dve", tokf[:, :], toki[:, :])
    kb.copy("dve", io_f[:, :], io_i[:, :])
    kb.copy("dve", tr_f[:, :], tr_i[:, :])
    kb.ts("dve", tokp1[:, :], tokf[:, :], 1.0, ALU.add)
    pbanks = (F0, F1, M0, M1)
    for i in range(NTT):
        Pb = pbanks[i // 16]
        cs = slice((i % 16) * 32, (i % 16) * 32 + 32)
        for ip in range(i):
            kb.mm(Pb[:, cs], ones_b[:, :], mask_bf[:, ip, :], start=(ip == 0), stop=False)
        kb.mm(Pb[:, cs], SU[:, :], mask_bf[:, i, :], start=(i == 0), stop=True)
    for q_ in range(NTT // 16):
        S.op("dve", lambda e, q_=q_: e.tensor_copy(pos.h[:, q_ * 16:(q_ + 1) * 16, :],
                                                  pbanks[q_].h[:, :].rearrange("p (a b) -> p a b", a=16)),
             reads=[pbanks[q_][:, :]], writes=[pos[:, :, :]])
    for i in range(NTT):
        S.op("dve", lambda e, i=i: e.tensor_tensor_scan(slot.h[:, i, :], ones_f.h[:, 0:32], mask_all.h[:, i, :], 0.0,
                                                         ALU.mult, ALU.add),
             reads=[ones_f[:, :], mask_all[:, :, :]], writes=[slot[:, :, :]])
    kb.tt("dve", slot[:, :, :], slot[:, :, :], mask_all[:, :, :], ALU.subtract)
    for i in range(NTT):
        kb.ts("dve", pay[:, i, :, 0], mask_all[:, i, :], 0.0, ALU.mult, tokf[:, i:i + 1], ALU.add)
        kb.ts("dve", pay[:, i, :, 2], slot[:, i, :], float(NTOK), ALU.mult, tokp1[:, i:i + 1], ALU.add)
    kb.copy("dve", pay[:, :, :, 1], gate_all[:, :, :])
    n = 0
    for e_ in range(32):
        Pb = (M2, PO)[e_ % 2]
        kb.mm(Pb[:, 0:NJ * 3], zer[:, :], zer[:, 0:NJ * 3], start=True, stop=False)
        for i in range(NTT):
            sel = Sel[n % 3]
            n += 1
            kb.ts("dve", sel[:, :], io_f[:, :], pos[:, i, e_:e_ + 1], ALU.is_equal, mask_all[:, i, e_:e_ + 1], ALU.mult)
            for j in range(NJ):
                kb.mm(Pb[:, 3 * j:3 * j + 3], sel[:, j * 128:(j + 1) * 128], pay[:, i, e_, :],
                      start=False, stop=(i == NTT - 1 and j == NJ - 1))
        kb.copy("act", lst[:, e_, :], Pb[:, 0:NJ * 3])
    l4 = lst.h[:, :, :].rearrange("p e (j c) -> p e j c", c=3)
    iv = idx_tok.h[:, :].rearrange("p (e j) -> p e j", e=32)
    gv = gl.h[:, :].rearrange("p (e j) -> p e j", e=32)
    tv = tmpd.h[:, :].rearrange("p (e j) -> p e j", e=32)
    S.op("dve", lambda e: e.tensor_copy(iv, l4[:, :, :, 0]), reads=[lst[:, :, :]], writes=[idx_tok[:, :]])
    S.op("dve", lambda e: e.tensor_copy(gv, l4[:, :, :, 1]), reads=[lst[:, :, :]], writes=[gl[:, :]])
    S.op("dve", lambda e: e.tensor_scalar(tv, l4[:, :, :, 2], 0.0, tr_f.h[:, 0:1], ALU.is_equal, ALU.mult),
         reads=[lst[:, :, :], tr_f[:, :]], writes=[tmpd[:, :]])
    S.op("dve", lambda e: e.tensor_tensor(tv, tv, l4[:, :, :, 2], ALU.add),
         reads=[lst[:, :, :], tmpd[:, :]], writes=[tmpd[:, :]])
    kb.copy("dve", dest[:, :], tmpd[:, :])
    S.emit()
    pB.close()
    pR.close()

    pB = contextlib.ExitStack()
    kb.stack = pB
    stg = [kb.sb([128, 16, 512], F32, f"E_stg{i}") for i in range(2)]
    wb = [kb.sb([128, 16, 512], BF16, f"E_wb{i}") for i in range(2)]
    XgT = kb.sb([128, 16, CAP], BF16, "E_XgT")
    xg0 = kb.sb([128, 2048], F32, "E_xg0")
    xg = [xg0, xg0]
    actT = kb.sb([128, 16, CAP], BF16, "E_actT")
    Yp = [kb.sb([128, 512], F32, f"E_Y{j}") for j in range(2)]
    bgu = kb.sb([128, 1024], F32, "E_bgu")
    bdr = kb.sb([128, 512], F32, "E_bdr")
    bdb = [kb.sb([128, 512], BF16, f"E_bdb{i}") for i in range(2)]
    glu = kb.sb([128, 512], F32, "E_glu")
    lin = kb.sb([128, 512], F32, "E_lin")
    sg = kb.sb([128, 512], F32, "E_sg")
    kb.dma(bgu[:, :], V(io["bgu_col"], None), "c0")
    kb.memset("pool", bdr[:, :], 0.0)
    for i in range(2):
        kb.memset("pool", bdb[i][:, :], 0.0)
    w_gu, w_dn, b_dn = io["w_gu"], io["w_dn"], io["b_dn"]
    nblk = 0
    ny = 0
    gbanks = ((F0, F1), (M0, M1), (M2, PO))
    dbanks = (F0, F1, M0, M1, M2, PO)
    for e_ in range(n_exp):
        for j in range(NJ):
            col = e_ * NJ + j
            xs = 0
            S.dma("pool", lambda e, xs=xs, col=col: e.indirect_dma_start(
                xg[xs].h[:, :], None, h2s[:, :], bass.IndirectOffsetOnAxis(idx_tok.h[:, col:col + 1], 0)),
                f"xg{xs}", reads=[idx_tok[:, :], V(None, "h2s")], writes=[xg[xs][:, :]])
            for g in range(4):
                Tb = T0 if g % 2 == 0 else T1
                for jj in range(4):
                    kk = 4 * g + jj
                    kb.tr(Tb[:, jj * 128:(jj + 1) * 128], xg[xs][:, kk * 128:(kk + 1) * 128], ident[:, :])
                q = kb.evac_q()
                fn = (lambda e, Tb=Tb, g=g, j=j: e.copy(
                    XgT.h[:, 4 * g:4 * g + 4, j * 128:(j + 1) * 128], Tb.h[:, :].rearrange("p (a b) -> p a b", a=4))) \
                    if q == "act" else (lambda e, Tb=Tb, g=g, j=j: e.tensor_copy(
                        XgT.h[:, 4 * g:4 * g + 4, j * 128:(j + 1) * 128], Tb.h[:, :].rearrange("p (a b) -> p a b", a=4)))
                S.op(q, fn, reads=[Tb[:, :]], writes=[XgT[:, :, :]])
        for cb in range(8):
            bs = nblk % 2
            nblk += 1
            kb.dma(stg[bs][:, :, :], V(w_gu[e_ * D:(e_ + 1) * D, cb * 512:(cb + 1) * 512].rearrange("(k p) c -> p k c", p=128), None),
                   f"stg{bs}")
            sv = stg[bs].h[:, :, :].rearrange("p k (c two) -> p k c two", two=2)
            S.op("act", lambda e, bs=bs, sv=sv: e.copy(wb[bs].h[:, :, 0:256], sv[:, :, :, 0]),
                 reads=[stg[bs][:, :, :]], writes=[wb[bs][:, :, :]])
            S.op("pool", lambda e, bs=bs, sv=sv: e.tensor_copy(wb[bs].h[:, :, 256:512], sv[:, :, :, 1]),
                 reads=[stg[bs][:, :, :]], writes=[wb[bs][:, :, :]])
            for m in range(2):
                mc = cb * 2 + m
                bg = bgu[:, e_ * 32 + mc:e_ * 32 + mc + 1]
                bl = bgu[:, e_ * 32 + 16 + mc:e_ * 32 + 16 + mc + 1]
                for gi, (r0, rn) in enumerate(RGS):
                    Pg, Pl = gbanks[gi]
                    for kk in range(16):
                        kb.mm(Pg[:, 0:rn], wb[bs][:, kk, m * 128:(m + 1) * 128], XgT[:, kk, r0:r0 + rn],
                              start=(kk == 0), stop=(kk == 15))
                    for kk in range(16):
                        kb.mm(Pl[:, 0:rn], wb[bs][:, kk, 256 + m * 128:256 + (m + 1) * 128], XgT[:, kk, r0:r0 + rn],
                              start=(kk == 0), stop=(kk == 15))
                    kb.ts("dve", glu[:, 0:rn], Pg[:, 0:rn], bg, ALU.add, 7.0, ALU.min)
                    kb.ts("dve", lin[:, 0:rn], Pl[:, 0:rn], bl, ALU.add, 7.0, ALU.min)
                    kb.ts("dve", lin[:, 0:rn], lin[:, 0:rn], -7.0, ALU.max, 1.0, ALU.add)
                    kb.act(sg[:, 0:rn], glu[:, 0:rn], AF.Sigmoid, scale=1.702)
                    kb.tt("dve", glu[:, 0:rn], glu[:, 0:rn], sg[:, 0:rn], ALU.mult)
                    kb.tt("dve", actT[:, mc, r0:r0 + rn], glu[:, 0:rn], lin[:, 0:rn], ALU.mult)
        for nb in range(4):
            bs = nblk % 2
            nblk += 1
            kb.dma(stg[bs][:, :, :], V(w_dn[e_ * D:(e_ + 1) * D, nb * 512:(nb + 1) * 512].rearrange("(k p) c -> p k c", p=128), None),
                   f"stg{bs}")
            S.op("act", lambda e, bs=bs: e.copy(wb[bs].h[:, 0:8, :], stg[bs].h[:, 0:8, :]),
                 reads=[stg[bs][:, :, :]], writes=[wb[bs][:, :, :]])
            S.op("pool", lambda e, bs=bs: e.tensor_copy(wb[bs].h[:, 8:16, :], stg[bs].h[:, 8:16, :]),
                 reads=[stg[bs][:, :, :]], writes=[wb[bs][:, :, :]])
            ds_ = nb % 2
            kb.dma(bdr[0:1, :], V(b_dn[e_:e_ + 1, nb * 512:(nb + 1) * 512], None), "bd")
            kb.copy("pool", bdb[ds_][0:1, :], bdr[0:1, :])
            yb = ybufs[nb]
            for j in range(NJ):
                Pb = dbanks[j % 6]
                col = e_ * NJ + j
                for kk in range(16):
                    kb.mm(Pb[:, :], actT[:, kk, j * 128:(j + 1) * 128], wb[bs][:, kk, :], start=(kk == 0), stop=False)
                kb.mm(Pb[:, :], ones_b[:, :], bdb[ds_][:, :], start=False, stop=True)
                ys_ = ny % 2
                ny += 1
                kb.ts("dve", Yp[ys_][:, :], Pb[:, :], gl[:, col:col + 1], ALU.mult)
                S.dma("pool", lambda e, ys_=ys_, col=col, yb=yb: e.indirect_dma_start(
                    yb[:, :], bass.IndirectOffsetOnAxis(dest.h[:, col:col + 1], 0), Yp[ys_].h[:, :], None),
                    f"ys{ys_}", reads=[dest[:, :], Yp[ys_][:, :]], writes=[V(None, "ybuf")])
    S.emit()
    pB.close()

    pB = contextlib.ExitStack()
    kb.stack = pB
    g_rep = kb.sb([128, 2048], F32, "F_g")
    b_rep = kb.sb([128, 2048], F32, "F_b")
    ys = [[kb.sb([128, 2048], F32, f"F_ys{i}_{s_}") for s_ in range(4)] for i in range(2)]
    hh2 = [kb.sb([128, 2048], F32, f"F_h{i}") for i in range(2)]
    yt = [kb.sb([128, 2048], F32, f"F_y{i}") for i in range(2)]
    stats = kb.sb([128, 24], F32, "F_stats")
    mv = kb.sb([128, 2], F32, "F_mv")
    rr = kb.sb([128, 1], F32, "F_r")
    kb.dma(g_rep[:, :], V(io["ln3g"], None), "c0")
    kb.dma(b_rep[:, :], V(io["ln3b"], None), "c1")
    for i in range(NTT):
        sl = i % 2
        kb.dma(hh2[sl][:, :], V(h2s[i * 128:(i + 1) * 128, :], "h2s"), f"fh{sl}")
        for s_ in range(4):
            r0 = 1 + s_ * NTOK + i * 128
            for nb in range(4):
                kb.dma(ys[sl][s_][:, nb * 512:(nb + 1) * 512], V(ybufs[nb][r0:r0 + 128, :], "ybuf"), f"fy{sl}_{s_}")
        kb.tt("dve", yt[sl][:, :], ys[sl][0][:, :], ys[sl][1][:, :], ALU.add)
        kb.tt("pool", ys[sl][2][:, :], ys[sl][2][:, :], ys[sl][3][:, :], ALU.add)
        kb.tt("dve", yt[sl][:, :], yt[sl][:, :], ys[sl][2][:, :], ALU.add)
        kb.stt(yt[sl][:, :], hh2[sl][:, :], ALPHA, yt[sl][:, :], ALU.mult, ALU.add)
        _ln_tile(kb, yt[sl], g_rep, b_rep, yt[sl], stats, mv, rr)
        kb.dma(V(out_ap[i * 128:(i + 1) * 128, :], None), yt[sl][:, :], f"fo{sl}")
    S.emit()
    pB.close()


def _set_cfg(npb):
    global NPB, NPRE, NTT, NTOK, CAP, NJ, RGS, YROWS
    NPB = npb
    NTT = NT_SEQ // npb
    NPRE = NT_SEQ - NTT
    NTOK = NTT * 128
    CAP, RGS = {1: (1280, ((0, 512), (512, 512), (1024, 256))), 2: (640, ((0, 512), (512, 128))),
                4: (384, ((0, 384),))}[npb]
    NJ = CAP // 128
    YROWS = 1 + 4 * NTOK + 128


def build_program(mode="full", dbg_tiles=None, n_exp=32, ng=4, npb=2, npre_dbg=None):
    global NPRE
    _set_cfg(npb)
    if npre_dbg is not None:
        NPRE = npre_dbg
    nc = bass.Bass("TRN2", target_bir_lowering=False)
    stack = contextlib.ExitStack()
    nt = dbg_tiles or NT_SEQ
    dbg = (mode == "A")
    doA = mode in ("full", "A")
    doB = mode in ("full", "BCD")
    NG = ng

    def din(name, shape, dt=F32):
        return nc.dram_tensor(name, list(shape), dt, kind="ExternalInput").ap()

    x_b = din("x_b", [SEQ, D])
    if doA:
        keep_in = din("keep_rep", [128, NT_SEQ])
        w_h_all = din("w_h", [NG * D, 2560])
        bcol_h_all = din("bcol_h", [NG * 128, 8])
        brow_h_all = din("brow_h", [NG, 1536])
        lbl_all = din("lbl", [NG * 128, 8])
        hng_rep = din("hng_rep", [128, 512])
        w_g_all = din("w_g", [NG * D, 2064])
        bcol_g_all = din("bcol_g", [NG * 128, 8])
        brow_g_all = din("brow_g", [NG, 1536])
        wa2_all = din("wa2", [NG * 16, 256])
        gng_rep = din("gng_rep", [128, 512])
        if dbg:
            gh_out = nc.dram_tensor("gh_out", [SEQ, 512], F32, kind="ExternalOutput").ap()
            mg_all = nc.dram_tensor("mg_out", [SEQ, NG * 512], F32, kind="ExternalOutput").ap()
        else:
            gh_out = nc.dram_tensor("gh_scr", [SEQ, 512], F32).ap()
            mg_all = nc.dram_tensor("merged", [NTOK, D], F32).ap()
    io = {}
    if doB:
        if mode == "BCD":
            io["merged"] = din("merged", [NTOK, D])
        else:
            io["merged"] = mg_all
        io["x_tok"] = x_b[NPRE * 128:, :]
        for nm in ("ln1g", "ln1b", "ln2g", "ln2b", "ln3g", "ln3b"):
            io[nm] = din(nm, [128, D])
        io["w_o"] = din("w_o", [D, D])
        io["w_kv"] = din("w_kv", [D, 2 * D])
        io["w_xq"] = din("w_xq", [D, D])
        io["w_xo"] = din("w_xo", [D, D])
        io["mem_b"] = din("mem_b", [256, D])
        io["w_router"] = din("w_router", [D, 32])
        io["br_rep"] = din("br_rep", [128, 32])
        io["bgu_col"] = din("bgu_col", [128, 1024])
        io["w_gu"] = din("w_gu", [32 * D, 2 * D])
        io["w_dn"] = din("w_dn", [32 * D, D])
        io["b_dn"] = din("b_dn", [32, D])
        io["out"] = nc.dram_tensor("out", [NTOK, D], F32, kind="ExternalOutput").ap()
        if mode == "BCD":
            io["h1s"] = nc.dram_tensor("h1s", [NTOK, D], F32, kind="ExternalOutput").ap()
            io["h2s"] = nc.dram_tensor("h2s", [NTOK, D], F32, kind="ExternalOutput").ap()
        else:
            io["h1s"] = nc.dram_tensor("h1s", [NTOK, D], F32).ap()
            io["h2s"] = nc.dram_tensor("h2s", [NTOK, D], F32).ap()
        io["ybuf"] = [nc.dram_tensor(f"ybuf{nb}", [YROWS, 512], F32).ap() for nb in range(4)]
        io["n_exp"] = n_exp

    with stack:
        S = Sched(nc, stack)
        kb = K(nc, S, stack)
        C = _consts(kb)
        ident, cmask, ones_b = C["ident"], C["cmask"], C["ones_b"]

        T0, T1 = kb.ps("pT0"), kb.ps("pT1")
        F0, F1 = kb.ps("pF0"), kb.ps("pF1")
        M0, M1, M2 = kb.ps("pM0"), kb.ps("pM1"), kb.ps("pM2")
        PO = kb.ps("pO")

        if doA:
            pA = contextlib.ExitStack()
            kb.stack = pA
            bF0, bF1, bM0, bM1, bM2, bPO = F0, F1, M0, M1, M2, PO
            for hh in range(NG):
                F0, F1, M0 = bF0, bF1, bM0
                keep = kb.sb([128, NT_SEQ], F32, "keep")
                if hh == 0:
                    kb.dma(keep[:, :], V(keep_in, None), "ck")
                w_h = w_h_all[hh * D:(hh + 1) * D, :]
                bcol_h = bcol_h_all[hh * 128:(hh + 1) * 128, :]
                brow_h = brow_h_all[hh:hh + 1, :]
                lbl = lbl_all[hh * 128:(hh + 1) * 128, :]
                w_g = w_g_all[hh * D:(hh + 1) * D, :]
                bcol_g = bcol_g_all[hh * 128:(hh + 1) * 128, :]
                brow_g = brow_g_all[hh:hh + 1, :]
                wa2_in = wa2_all[hh * 16:(hh + 1) * 16, :]
                mg_out = mg_all[:, hh * 512:(hh + 1) * 512]
                w_sb = kb.sb([128, 16, 2560], BF16, "w_sb")
                stage = [kb.sb([128, 2560], F32, f"wstage{i}") for i in range(2)]
                xt = [kb.sb([128, D], F32, f"xt{i}") for i in range(2)]
                xT = [kb.sb([128, 16 * 128], BF16, f"xT{i}") for i in range(2)]
                bcol = kb.sb([128, 8], F32, "bcol")
                brow = kb.sb([128, 1536], F32, "brow")
                brow_b = kb.sb([128, 1536], BF16, "brow_b")
                lb_in = kb.sb([128, 8], F32, "lb_in")
                lbv = kb.sb([128, 4], F32, "lbv")
                omlb = kb.sb([128, 4], F32, "omlb")
                hng = kb.sb([128, 512], F32, "hng")

                kb.dma(bcol[:, :], V(bcol_h, None), "c0")
                kb.memset("dve", brow[:, :], 0.0)
                kb.dma(brow[0:1, :], V(brow_h, None), "c1")
                kb.dma(lb_in[:, :], V(lbl, None), "c2")
                kb.dma(hng[:, :], V(hng_rep, None), "c3")
                kb.copy("dve", brow_b[:, :], brow[:, :])
                kb.tt("dve", lbv[:, :], lb_in[:, 0:4], lb_in[:, 4:8], ALU.subtract)
                kb.act(lbv[:, :], lbv[:, :], AF.Sigmoid)
                kb.ts("dve", omlb[:, :], lbv[:, :], -1.0, ALU.mult, 1.0, ALU.add)

                _load_weights_bf16(kb, w_h, 2560, w_sb, stage, "ws")

                def wt(name, shape, dt=F32):
                    return kb.sb(shape, dt, name)
                WS = []
                for wi in range(2):
                    WS.append((
                        wt(f"qT{wi}", [128, 512]), wt(f"fT{wi}", [128, 512]), wt(f"kTt{wi}", [128, 512]),
                        wt(f"cum{wi}", [128, 512]), wt(f"e1{wi}", [128, 512]), wt(f"e2{wi}", [128, 512]),
                        wt(f"mid{wi}", [128, 8]), wt(f"sc{wi}", [128, 8]),
                        wt(f"Qt{wi}", [128, 512], BF16), wt(f"Qc{wi}", [128, 512], BF16), wt(f"Kt{wi}", [128, 512], BF16),
                        wt(f"KlT{wi}", [128, 512]), wt(f"Kl{wi}", [128, 512], BF16), wt(f"Vb{wi}", [128, 512], BF16),
                        wt(f"AT{wi}", [128, 512], BF16), wt(f"gate{wi}", [128, 512]), wt(f"g2{wi}", [128, 512]),
                        wt(f"osq{wi}", [128, 512]), wt(f"ss{wi}", [128, 4]), wt(f"rstd{wi}", [128, 4])))
                St = wt("St", [128, 512])
                Sb = wt("Sb", [128, 512], BF16)
                ot = [wt(f"ot{i}", [128, 512]) for i in range(2)]
                onesf = C["ones_f"]

                kb.memset("dve", St[:, :], 0.0)
                kb.memset("dve", Sb[:, :], 0.0)

                kb.dma(xt[0][:, :], V(x_b[0:128, :], None), "x0")
                def tile_H(t):
                    sl = t % 2
                    full = (t >= NPRE)
                    (qT, fT, kTt, cum, e1, e2, mid, sc, Qt, Qc, Kt, KlT, Kl, Vb, AT, gate, g2, osq, ss, rstd) = WS[sl]
                    if full or sl == 0:
                        F0, F1, M0 = bF0, bF1, bM0
                    else:
                        F0, F1, M0 = bF1, bF0, bM1
                    if t + 1 < nt:
                        kb.dma(xt[1 - sl][:, :], V(x_b[(t + 1) * 128:(t + 2) * 128, :], None), f"x{1 - sl}")
                    for g in range(4):
                        Tb = T0 if g % 2 == 0 else T1
                        for j in range(4):
                            kk = 4 * g + j
                            kb.tr(Tb[:, j * 128:(j + 1) * 128], xt[sl][:, kk * 128:(kk + 1) * 128], ident[:, :])
                        src = V(Tb.h[:, :], None)
                        dst = xT[sl].k(g)[:, g * 512:(g + 1) * 512]
                        q = kb.evac_q()
                        rd = [Tb[:, 0:1] for j in range(4)]
                        if q == "act":
                            S.op("act", lambda e, d=dst, s=src: e.copy(d.ap, s.ap), reads=rd, writes=[dst])
                        else:
                            S.op("dve", lambda e, d=dst, s=src: e.tensor_copy(d.ap, s.ap), reads=rd, writes=[dst])
                    xTr = [xT[sl].k(kk // 4)[:, kk * 128:(kk + 1) * 128] for kk in range(16)]

                    for which, Fb in ((0, F0), (1, F1)):
                        if which == 0 and not full:
                            continue
                        for h in range(4):
                            c0 = which * 512 + h * 128
                            for kk in range(16):
                                kb.mm(Fb[:, h * 128:(h + 1) * 128], w_sb.k(kk)[:, kk, c0:c0 + 128], xTr[kk],
                                      start=(kk == 0), stop=(kk == 15))
                    for n, Mb in enumerate((M0, M1, M2)):
                        if n > 0 and not full:
                            continue
                        c0 = 1024 + n * 512
                        for kk in range(16):
                            kb.mm(Mb[:, :], xTr[kk], w_sb.k(kk)[:, kk, c0:c0 + 512], start=(kk == 0), stop=False)
                        kb.mm(Mb[:, :], ones_b[:, :], brow_b[:, n * 512:(n + 1) * 512], start=False, stop=True)

                    for h in range(4):
                        cs = slice(h * 128, (h + 1) * 128)
                        if full:
                            kb.act(qT[:, cs], F0[:, cs], AF.Silu, bias=bcol[:, h:h + 1])
                        kb.act(fT[:, cs], F1[:, cs], AF.Sigmoid, bias=bcol[:, 4 + h:5 + h])
                        kb.ts("dve", fT[:, cs], fT[:, cs], omlb[:, h:h + 1], ALU.mult, lbv[:, h:h + 1], ALU.add)
                        kb.ts("dve", kTt[:, cs], fT[:, cs], -1.0, ALU.mult, 1.0, ALU.add)
                    kb.act(fT[:, :], fT[:, :], AF.Ln)
                    for h in range(4):
                        cs = slice(h * 128, (h + 1) * 128)
                        S.op("dve", lambda e, cs=cs, cum=cum, fT=fT: e.tensor_tensor_scan(cum.h[:, cs], onesf.h[:, :], fT.h[:, cs], 0.0,
                                                                            ALU.mult, ALU.add),
                             reads=[onesf[:, :], fT[:, cs]], writes=[cum[:, cs]])
                        c63 = h * 128 + 63
                        cl = h * 128 + 127
                        kb.ts("dve", mid[:, 2 * h:2 * h + 1], cum[:, c63:c63 + 1], -1.0, ALU.mult)
                        if full:
                            kb.act(e1[:, cs], cum[:, cs], AF.Exp, bias=mid[:, 2 * h:2 * h + 1])
                        kb.act(e2[:, cs], cum[:, cs], AF.Exp, bias=cum[:, c63:c63 + 1], scale=-1.0)
                        if full:
                            kb.act(sc[:, 2 * h:2 * h + 1], cum[:, c63:c63 + 1], AF.Exp)
                        kb.act(sc[:, 2 * h + 1:2 * h + 2], cum[:, cl:cl + 1], AF.Exp, bias=mid[:, 2 * h:2 * h + 1])
                        kb.act(mid[:, 2 * h + 1:2 * h + 2], cum[:, cl:cl + 1], AF.Exp)
                        if full:
                            kb.tt("dve", Qt[:, cs], qT[:, cs], e1[:, cs], ALU.mult)
                            kb.ts("dve", Qc[:, cs], Qt[:, cs], sc[:, 2 * h:2 * h + 1], ALU.mult)
                        kb.tt("dve", Kt[:, cs], kTt[:, cs], e2[:, cs], ALU.mult)
                        kb.ts("dve", KlT[:, cs], Kt[:, cs], sc[:, 2 * h + 1:2 * h + 2], ALU.mult)
                    kb.copy("act", Vb[:, :], M0[:, :])
                    if full:
                        kb.act(gate[:, :], M1[:, :], AF.Sigmoid)
                        kb.act(g2[:, :], M2[:, :], AF.Sigmoid)
                        kb.tt("dve", gate[:, :], gate[:, :], g2[:, :], ALU.mult)
                        kb.tt("pool", gate[:, :], gate[:, :], hng[:, :], ALU.mult)
                    yield
                    for h in range(4 if full else 0):
                        cs = slice(h * 128, (h + 1) * 128)
                        kb.mm(F0[:, cs], Kt[:, cs], Qt[:, cs], start=True, stop=True)
                        kb.tt("dve", AT[:, cs], F0[:, cs], cmask[:, :], ALU.mult)
                    for h in range(4):
                        cs = slice(h * 128, (h + 1) * 128)
                        kb.tr(F1[:, cs], KlT[:, cs], ident[:, :])
                        kb.copy("act", Kl[:, cs], F1[:, cs])
                    for h in range(4 if full else 0):
                        cs = slice(h * 128, (h + 1) * 128)
                        kb.mm(PO[:, cs], AT[:, cs], Vb[:, cs], start=True, stop=False)
                        kb.mm(PO[:, cs], Qc[:, cs], Sb[:, cs], start=False, stop=True)
                    for h in range(4):
                        cs = slice(h * 128, (h + 1) * 128)
                        kb.mm(M0[:, cs], Kl[:, cs], Vb[:, cs], start=True, stop=True)
                    for h in range(4):
                        cs = slice(h * 128, (h + 1) * 128)
                        kb.stt(St[:, cs], St[:, cs], mid[:, 2 * h + 1:2 * h + 2], M0[:, cs], ALU.mult, ALU.add)
                    if not full:
                        kb.ts("dve", St[:, :], St[:, :], keep[:, t:t + 1], ALU.mult)
                    kb.copy("act", Sb[:, :], St[:, :])
                    if not full:
                        return
                    for h in range(4):
                        cs = slice(h * 128, (h + 1) * 128)
                        kb.act(osq[:, cs], PO[:, cs], AF.Square, accum=ss[:, h:h + 1])
                    kb.ts("dve", rstd[:, :], ss[:, :], 1.0 / 128.0, ALU.mult, EPS, ALU.add)
                    kb.act(rstd[:, :], rstd[:, :], AF.Sqrt)
                    S.op("dve", lambda e, rstd=rstd: e.reciprocal(rstd.h[:, :], rstd.h[:, :]), reads=[rstd[:, :]], writes=[rstd[:, :]])
                    o_t = ot[sl]
                    for h in range(4):
                        cs = slice(h * 128, (h + 1) * 128)
                        kb.stt(o_t[:, cs], PO[:, cs], rstd[:, h:h + 1], gate[:, cs], ALU.mult, ALU.mult)
                    kb.dma(V(gh_out[(t - NPRE) * 128:(t - NPRE + 1) * 128, :], f"gh{t}"), o_t[:, :], f"o{sl}")

                gens = {}
                for t in range(nt + 1):
                    if t < nt:
                        gens[t] = tile_H(t)
                        next(gens[t])
                    if t >= 1:
                        for _ in gens.pop(t - 1):
                            pass

                bcg = kb.sb([128, 8], F32, "bcg")
                wa2 = kb.sb([16, 256], F32, "wa2_sb")
                gng = kb.sb([128, 512], F32, "gng")
                ga1T = kb.sb([16, 128], F32, "ga1T")
                ght = [kb.sb([128, 512], F32, f"ght{i}") for i in range(2)]
                kb.dma(bcg[:, :], V(bcol_g, None), "c0")
                kb.memset("dve", brow[:, :], 0.0)
                kb.dma(brow[0:1, :], V(brow_g, None), "c1")
                kb.dma(wa2[:, :], V(wa2_in, None), "c2")
                kb.dma(gng[:, :], V(gng_rep, None), "c3")
                kb.copy("dve", brow_b[:, :], brow[:, :])
                kb.ts("dve", bcg[:, 0:2], bcg[:, 0:2], 1.0 / 16.0, ALU.mult)
                kb.ts("dve", bcg[:, 5:7], bcg[:, 5:7], -1.0, ALU.mult)
                _load_weights_bf16(kb, w_g, 2064, w_sb, stage, "ws")
                kb.memset("dve", St[:, :], 0.0)
                kb.memset("dve", Sb[:, :], 0.0)
                St2 = kb.sb([128, 512], F32, "St2")
                Sb2 = kb.sb([128, 512], BF16, "Sb2")
                kb.memset("dve", St2[:, :], 0.0)
                kb.memset("dve", Sb2[:, :], 0.0)
                Sts, Sbs = (St, St2), (Sb, Sb2)

                kb.dma(xt[0][:, :], V(x_b[0:128, :], None), "x0")
                def tile_G(t):
                    sl = t % 2
                    full = (t >= NPRE)
                    (qT, fT, kTt, cum, e1, e2, mid, sc, Qt, Qc, Kt, KlT, Kl, Vb, AT, gate, g2, osq, ss, rstd) = WS[sl]
                    if full or sl == 0:
                        F0, F1, M0 = bF0, bF1, bM0
                    else:
                        F0, F1, M0 = bM1, bM2, bPO
                    if t + 1 < nt:
                        kb.dma(xt[1 - sl][:, :], V(x_b[(t + 1) * 128:(t + 2) * 128, :], None), f"x{1 - sl}")
                    if full:
                        kb.dma(ght[sl][:, :], V(gh_out[(t - NPRE) * 128:(t - NPRE + 1) * 128, :], f"gh{t}"), f"g{sl}")
                    for g in range(4):
                        Tb = T0 if g % 2 == 0 else T1
                        for j in range(4):
                            kk = 4 * g + j
                            kb.tr(Tb[:, j * 128:(j + 1) * 128], xt[sl][:, kk * 128:(kk + 1) * 128], ident[:, :])
                        dst = xT[sl].k(g)[:, g * 512:(g + 1) * 512]
                        kb.copy(kb.evac_q(), dst, Tb[:, :])
                    xTr = [xT[sl].k(kk // 4)[:, kk * 128:(kk + 1) * 128] for kk in range(16)]
                    for u in range(4):
                        if u < 2 and not full:
                            continue
                        for kk in range(16):
                            kb.mm(F0[:, u * 128:(u + 1) * 128], w_sb.k(kk)[:, kk, u * 128:(u + 1) * 128], xTr[kk],
                                  start=(kk == 0), stop=(kk == 15))
                    for kk in range(16):
                        kb.mm(F1[0:16, 0:128], w_sb.k(kk)[:, kk, 512:528], xTr[kk], start=(kk == 0), stop=(kk == 15))
                    for n, Mb in enumerate((M0, M1, M2)):
                        if n > 0 and not full:
                            continue
                        c0 = 528 + n * 512
                        for kk in range(16):
                            kb.mm(Mb[:, :], xTr[kk], w_sb.k(kk)[:, kk, c0:c0 + 512], start=(kk == 0), stop=False)
                        kb.mm(Mb[:, :], ones_b[:, :], brow_b[:, n * 512:(n + 1) * 512], start=False, stop=True)
                    kb.act(ga1T[:, :], F1[0:16, 0:128], AF.Identity, bias=bcg[0:16, 4:5])
                    for u in range(2):
                        kb.mm(F1[:, 128 + u * 128:256 + u * 128], wa2[:, u * 128:(u + 1) * 128], ga1T[:, :],
                              start=True, stop=True)
                    for u in range(2):
                        cs = slice(u * 128, (u + 1) * 128)
                        if full:
                            kb.act(qT[:, cs], F0[:, cs], AF.Identity, bias=bcg[:, u:u + 1], scale=1.0 / 16.0)
                        kb.act(kTt[:, cs], F0[:, 256 + u * 128:384 + u * 128], AF.Identity, bias=bcg[:, 2 + u:3 + u])
                        kb.act(fT[:, cs], F1[:, 128 + u * 128:256 + u * 128], AF.Exp, bias=bcg[:, 5 + u:6 + u], scale=-1.0)
                    kb.act(fT[:, 0:256], fT[:, 0:256], AF.Ln, bias=1.0)
                    kb.ts("dve", fT[:, 0:256], fT[:, 0:256], -1.0 / 16.0, ALU.mult)
                    for h in range(2):
                        cs = slice(h * 128, (h + 1) * 128)
                        S.op("dve", lambda e, cs=cs, cum=cum, fT=fT: e.tensor_tensor_scan(cum.h[:, cs], onesf.h[:, :], fT.h[:, cs], 0.0,
                                                                            ALU.mult, ALU.add),
                             reads=[onesf[:, :], fT[:, cs]], writes=[cum[:, cs]])
                        c63 = h * 128 + 63
                        cl = h * 128 + 127
                        kb.ts("dve", mid[:, 2 * h:2 * h + 1], cum[:, c63:c63 + 1], -1.0, ALU.mult)
                        if full:
                            kb.act(e1[:, cs], cum[:, cs], AF.Exp, bias=mid[:, 2 * h:2 * h + 1])
                        kb.act(e2[:, cs], cum[:, cs], AF.Exp, bias=cum[:, c63:c63 + 1], scale=-1.0)
                        if full:
                            kb.act(sc[:, 2 * h:2 * h + 1], cum[:, c63:c63 + 1], AF.Exp)
                        kb.act(sc[:, 2 * h + 1:2 * h + 2], cum[:, cl:cl + 1], AF.Exp, bias=mid[:, 2 * h:2 * h + 1])
                        kb.act(mid[:, 2 * h + 1:2 * h + 2], cum[:, cl:cl + 1], AF.Exp)
                        if full:
                            kb.tt("dve", Qt[:, cs], qT[:, cs], e1[:, cs], ALU.mult)
                            kb.ts("dve", Qc[:, cs], Qt[:, cs], sc[:, 2 * h:2 * h + 1], ALU.mult)
                        kb.tt("dve", Kt[:, cs], kTt[:, cs], e2[:, cs], ALU.mult)
                        kb.ts("dve", KlT[:, cs], Kt[:, cs], sc[:, 2 * h + 1:2 * h + 2], ALU.mult)
                    kb.copy("act", Vb[:, :], M0[:, :])
                    if full:
                        kb.act(gate[:, :], M1[:, :], AF.Silu)
                        kb.act(g2[:, :], M2[:, :], AF.Sigmoid)
                        kb.tt("dve", gate[:, :], gate[:, :], g2[:, :], ALU.mult)
                        kb.tt("pool", gate[:, :], gate[:, :], gng[:, :], ALU.mult)
                    yield
                    for u in range(2 if full else 0):
                        cs = slice(u * 128, (u + 1) * 128)
                        kb.mm(F0[:, 0:128], Kt[:, cs], Qt[:, cs], start=(u == 0), stop=(u == 1))
                    if full:
                        kb.tt("dve", AT[:, 0:128], F0[:, 0:128], cmask[:, :], ALU.mult)
                    for u in range(2):
                        cs = slice(u * 128, (u + 1) * 128)
                        kb.tr(F1[:, cs], KlT[:, cs], ident[:, :])
                    kb.copy("act", Kl[:, 0:256], F1[:, 0:256])
                    if full:
                        kb.mm(PO[:, :], AT[:, 0:128], Vb[:, :], start=True, stop=False)
                    for u in range(2 if full else 0):
                        cs = slice(u * 128, (u + 1) * 128)
                        kb.mm(PO[:, :], Qc[:, cs], Sbs[u][:, :], start=False, stop=(u == 1))
                    for u, Pb in enumerate((M0, F0)):
                        cs = slice(u * 128, (u + 1) * 128)
                        kb.mm(Pb[:, :], Kl[:, cs], Vb[:, :], start=True, stop=True)
                    for u, Pb in enumerate((M0, F0)):
                        kb.stt(Sts[u][:, :], Sts[u][:, :], mid[:, 2 * u + 1:2 * u + 2], Pb[:, :], ALU.mult, ALU.add)
                        if not full:
                            kb.ts("dve", Sts[u][:, :], Sts[u][:, :], keep[:, t:t + 1], ALU.mult)
                        kb.copy("act", Sbs[u][:, :], Sts[u][:, :])
                    if not full:
                        return
                    kb.act(osq[:, :], PO[:, :], AF.Square, accum=ss[:, 0:1])
                    kb.ts("dve", rstd[:, 0:1], ss[:, 0:1], 1.0 / 512.0, ALU.mult, EPS, ALU.add)
                    kb.act(rstd[:, 0:1], rstd[:, 0:1], AF.Sqrt)
                    S.op("dve", lambda e, rstd=rstd: e.reciprocal(rstd.h[:, 0:1], rstd.h[:, 0:1]), reads=[rstd[:, :]], writes=[rstd[:, :]])
                    o_t = ot[sl]
                    kb.stt(o_t[:, :], PO[:, :], rstd[:, 0:1], gate[:, :], ALU.mult, ALU.mult)
                    kb.tt("dve", o_t[:, :], o_t[:, :], ght[sl][:, :], ALU.add)
                    kb.dma(V(mg_out[(t - NPRE) * 128:(t - NPRE + 1) * 128, :], "merged"), o_t[:, :], f"o{sl}")
                gens = {}
                for t in range(nt + 1):
                    if t < nt:
                        gens[t] = tile_G(t)
                        next(gens[t])
                    if t >= 1:
                        for _ in gens.pop(t - 1):
                            pass
            S.emit()
            pA.close()
        if doB:
            phases_bcd(nc, S, kb, C, (T0, T1, F0, F1, M0, M1, M2, PO), io, dbg)
    return nc


def _prep_core_inputs(inp, c):
    b, hh = c // 4, c % 4
    w_in = inp["w_in"][0]
    b_in = inp["b_in"][0]
    offs = np.cumsum([0, 1024, 1024, 2048, 2048, 16, 2048, 2048, 2048, 2048, 2048, 2048])
    o_gq, o_gk, o_gv, o_gr, o_ga1, o_hq, o_hf, o_hi, o_hg, o_ma, o_mb = offs[:11]
    ch = slice(512 * hh, 512 * hh + 512)

    def cols(o, s):
        return np.arange(o + s.start, o + s.stop)
    hcols = np.concatenate([cols(o_hq, ch), cols(o_hf, ch), cols(o_hi, ch), cols(o_hg, ch), cols(o_mb, ch)])
    w_h = np.ascontiguousarray(w_in[:, hcols])
    bh = b_in[hcols]
    bcol_h = np.ascontiguousarray(bh[:1024].reshape(8, 128).T)
    brow_h = np.ascontiguousarray(bh[1024:].reshape(1, 1536))
    lb = inp["hgrn_lb_logits"][:, ch]
    lbl = np.ascontiguousarray(np.concatenate([lb[0].reshape(4, 128).T, lb[1].reshape(4, 128).T], axis=1))
    hng_rep = np.ascontiguousarray(np.broadcast_to(np.tile(inp["hgrn_norm_g"][0], 4)[None, :], (128, 512)))
    kq = slice(256 * hh, 256 * hh + 256)
    gcols = np.concatenate([cols(o_gq, kq), cols(o_gk, kq), np.arange(o_ga1, o_ga1 + 16), cols(o_gv, ch),
                            cols(o_gr, ch), cols(o_ma, ch)])
    w_g = np.ascontiguousarray(w_in[:, gcols])
    bg = b_in[gcols]
    bcol_g = np.zeros((128, 8), np.float32)
    bcol_g[:, 0:4] = bg[:512].reshape(4, 128).T
    bcol_g[:16, 4] = bg[512:528]
    bcol_g[:, 5:7] = inp["b_gla_a"][0][kq].reshape(2, 128).T
    brow_g = np.ascontiguousarray(bg[528:].reshape(1, 1536))
    wa2 = np.ascontiguousarray(inp["w_gla_a2"][0][:, kq])
    gng_rep = np.ascontiguousarray(np.broadcast_to(inp["gla_norm_g"][0][None, :], (128, 512)))
    return dict(x_b=np.ascontiguousarray(inp["x"][b]), w_h=w_h, bcol_h=bcol_h, brow_h=brow_h, lbl=lbl,
                hng_rep=hng_rep, w_g=w_g, bcol_g=bcol_g, brow_g=brow_g, wa2=wa2, gng_rep=gng_rep)


def _prep_a_inputs(inp, b, ng=4, j=0, npb=1):
    parts = [_prep_core_inputs(inp, 4 * b + hh) for hh in range(ng)]
    ntok = SEQ // npb
    own_end = (j + 1) * ntok
    start = own_end - SEQ
    xw = np.zeros((SEQ, D), np.float32)
    xw[max(0, -start):] = inp["x"][b, max(0, start):own_end]
    keep = ((start + 128 * np.arange(NT_SEQ)) >= 0).astype(np.float32)
    out = dict(x_b=xw, hng_rep=parts[0]["hng_rep"], gng_rep=parts[0]["gng_rep"],
               keep_rep=np.ascontiguousarray(np.broadcast_to(keep[None, :], (128, NT_SEQ))))
    for k in ("w_h", "bcol_h", "brow_h", "lbl", "w_g", "bcol_g", "brow_g", "wa2"):
        out[k] = np.ascontiguousarray(np.concatenate([p[k] for p in parts], axis=0))
    return out


def _prep_bcd_inputs(inp, b):
    def rep(v):
        return np.ascontiguousarray(np.broadcast_to(v[None, :], (128, v.shape[0])))
    bgu = inp["b_gate_up"][0]
    bg = bgu[:, 0::2].reshape(32, 16, 128)
    bl = bgu[:, 1::2].reshape(32, 16, 128)
    bgu_col = np.ascontiguousarray(np.concatenate([bg, bl], axis=1).transpose(2, 0, 1).reshape(128, 1024))
    return dict(
        x_b=np.ascontiguousarray(inp["x"][b]),
        ln1g=rep(inp["ln1_g"][0]), ln1b=rep(inp["ln1_b"][0]), ln2g=rep(inp["ln2_g"][0]), ln2b=rep(inp["ln2_b"][0]),
        ln3g=rep(inp["ln3_g"][0]), ln3b=rep(inp["ln3_b"][0]),
        w_o=inp["w_mix_o"][0], w_kv=inp["w_mem_kv"][0], w_xq=inp["w_xq"][0], w_xo=inp["w_xo"][0],
        mem_b=np.ascontiguousarray(inp["mem"][b]), w_router=inp["w_router"][0], br_rep=rep(inp["b_router"][0]),
        bgu_col=bgu_col, b_dn=inp["b_down"][0],
        w_gu=inp["w_gate_up"][0].reshape(32 * D, 2 * D), w_dn=inp["w_down"][0].reshape(32 * D, D))


NPB_FULL = 4


def kernel(**inputs):
    inp = {k: np.asarray(v) for k, v in inputs.items()}
    npb = NPB_FULL
    nc = build_program("full", npb=npb)
    in_maps = []
    for b in range(2):
        bcd = _prep_bcd_inputs(inp, b)
        for j in range(npb):
            m = dict(bcd)
            m.update(_prep_a_inputs(inp, b, 4, j, npb))
            in_maps.append(m)
    n = 2 * npb
    res = run_bass_kernel_spmd(nc, in_maps, core_ids=list(range(n)))
    out = np.stack([np.asarray(r["out"]) for r in res.results], axis=0)
    return out.reshape(2, SEQ, D).astype(np.float32)
```

```python
import contextlib
import numpy as np
import concourse.bass as bass
import concourse.mybir as mybir
from concourse.bass_utils import run_bass_kernel_spmd

F32 = mybir.dt.float32
BF16 = mybir.dt.bfloat16
I32 = mybir.dt.int32
U32 = mybir.dt.uint32
AF = mybir.ActivationFunctionType
ALU = mybir.AluOpType
AX = mybir.AxisListType

D = 2048
SEQ = 8192
NT_SEQ = SEQ // 128
EPS = 1e-5
ALPHA = 2.0 ** 0.25
SAME_ENGINE_SYNC = True


class V:
    __slots__ = ("ap", "key")

    def __init__(self, ap, key):
        self.ap = ap
        self.key = key


class Tile:
    def __init__(self, handle, key):
        self.h = handle
        self.key = key

    def __getitem__(self, idx):
        return V(self.h[idx], self.key)

    def k(self, sfx):
        return _Keyed(self.h, self.key + ":" + str(sfx))


class _Keyed:
    def __init__(self, h, key):
        self.h = h
        self.key = key

    def __getitem__(self, idx):
        return V(self.h[idx], self.key)


class Sched:
    QS = ("pe", "act", "dve", "pool", "sp")

    def __init__(self, nc, stack):
        self.nc = nc
        self.stack = stack
        self.items = {q: [] for q in self.QS}
        self.esem = {}
        for q in ("pe", "act", "dve", "pool"):
            self.esem[q] = stack.enter_context(nc.semaphore("es_" + q))
        self.cnt = {q: 0 for q in self.QS}
        self.waited = {q: {} for q in self.QS}
        self.res = {}
        self.dsem = {}
        self.semh = {"es_" + q: h for q, h in self.esem.items()}
        self.n_ops = 0

    def _deps(self, q, reads, writes):
        evs = []
        for r in reads:
            st = self.res.get(r)
            if st and st[0]:
                evs.append(st[0])
        for w in writes:
            st = self.res.get(w)
            if st:
                if st[0]:
                    evs.append(st[0])
                evs.extend(st[1])
        waits = {}
        for (sem, val, srcq) in evs:
            if srcq == q and (q == "pe" or not SAME_ENGINE_SYNC):
                continue
            if self.waited[q].get(sem, 0) >= val:
                continue
            if waits.get(sem, 0) < val:
                waits[sem] = val
        for sem, val in waits.items():
            self.waited[q][sem] = val
        return list(waits.items())

    def _commit(self, ev, reads, writes):
        for r in reads:
            if r in writes:
                continue
            self.res.setdefault(r, [None, []])[1].append(ev)
        for w in writes:
            self.res[w] = [ev, []]

    @staticmethod
    def _keys(views):
        return [v.key for v in views if v is not None and v.key is not None]

    def op(self, q, fn, reads=(), writes=()):
        rk, wk = self._keys(reads), self._keys(writes)
        waits = self._deps(q, rk, wk)
        self.cnt[q] += 1
        ev = ("es_" + q, self.cnt[q], q)
        self.items[q].append((waits, fn, ("es_" + q, 1)))
        self._commit(ev, rk, wk)
        self.n_ops += 1

    def dma(self, q, fn, sem, reads=(), writes=()):
        if sem not in self.dsem:
            h = self.stack.enter_context(self.nc.semaphore("ds_" + sem))
            self.dsem[sem] = [h, 0]
            self.semh["ds_" + sem] = h
        rk, wk = self._keys(reads), self._keys(writes)
        waits = self._deps(q, rk, wk)
        self.dsem[sem][1] += 16
        ev = ("ds_" + sem, self.dsem[sem][1], "dma")
        self.items[q].append((waits, fn, ("ds_" + sem, 16)))
        self._commit(ev, rk, wk)
        self.n_ops += 1

    def emit(self, final=False):
        nc = self.nc
        semh = self.semh
        items = self.items
        bar = [("es_" + q, self.cnt[q]) for q in ("pe", "act", "dve", "pool") if self.cnt[q] > 0]
        bar += [("ds_" + k, v[1]) for k, v in self.dsem.items() if v[1] > 0]

        def replay(q):
            def run(eng):
                for waits, fn, inc in items[q]:
                    for sem, val in waits:
                        eng.wait_ge(semh[sem], val)
                    ins = fn(eng)
                    ins.then_inc(semh[inc[0]], inc[1])
                for sem, val in bar:
                    if self.waited[q].get(sem, 0) < val:
                        eng.wait_ge(semh[sem], val)
                        self.waited[q][sem] = val
            return run

        with nc.Block() as block:
            block.sync(replay("sp"))
            block.tensor(replay("pe"))
            block.scalar(replay("act"))
            block.vector(replay("dve"))
            block.gpsimd(replay("pool"))
        self.items = {q: [] for q in self.QS}


class K:
    def __init__(self, nc, S, stack):
        self.nc, self.S, self.stack = nc, S, stack
        self._n = 0
        self.rr = 0
        self._tiles = {}

    def sb(self, shape, dt, name):
        if name in self._tiles:
            return self._tiles[name]
        h = self.stack.enter_context(self.nc.sbuf_tensor(name, list(shape), dt))
        t = Tile(h, name)
        self._tiles[name] = t
        return t

    def ps(self, name, dt=F32, cols=512):
        h = self.stack.enter_context(self.nc.psum_tensor(name, [128, cols], dt))
        return Tile(h, name)

    def mm(self, out, lhsT, rhs, start, stop):
        self.S.op("pe", lambda e: e.matmul(out.ap, lhsT.ap, rhs.ap, start=start, stop=stop),
                  reads=[lhsT, rhs] + ([] if start else [out]), writes=[out])

    def tr(self, out, in_, ident):
        self.S.op("pe", lambda e: e.transpose(out.ap, in_.ap, ident.ap), reads=[in_, ident], writes=[out])

    def act(self, out, in_, func, bias=None, scale=None, accum=None, q="act"):
        kw = {}
        rd = [in_]
        if bias is not None:
            if isinstance(bias, V):
                kw["bias"] = bias.ap
                rd.append(bias)
            else:
                kw["bias"] = float(bias)
        if scale is not None:
            if isinstance(scale, V):
                kw["scale"] = scale.ap
                rd.append(scale)
            else:
                kw["scale"] = float(scale)
        wr = [out]
        if accum is not None:
            kw["accum_out"] = accum.ap
            wr.append(accum)
        self.S.op("act", lambda e: e.activation(out.ap, in_.ap, func, **kw), reads=rd, writes=wr)

    def copy(self, q, out, in_):
        if q == "act":
            self.S.op("act", lambda e: e.copy(out.ap, in_.ap), reads=[in_], writes=[out])
        else:
            self.S.op(q, lambda e: e.tensor_copy(out.ap, in_.ap), reads=[in_], writes=[out])

    def tt(self, q, out, a, b, op):
        self.S.op(q, lambda e: e.tensor_tensor(out.ap, a.ap, b.ap, op), reads=[a, b], writes=[out])

    def ts(self, q, out, a, s1, op0, s2=None, op1=None, accum=None):
        rd = [a]
        s1v = s1.ap if isinstance(s1, V) else float(s1)
        if isinstance(s1, V):
            rd.append(s1)
        s2v = None
        if s2 is not None:
            s2v = s2.ap if isinstance(s2, V) else float(s2)
            if isinstance(s2, V):
                rd.append(s2)
        wr = [out]
        kw = {}
        if accum is not None:
            kw["accum_out"] = accum.ap
            wr.append(accum)
        o1 = op1 if op1 is not None else ALU.bypass
        self.S.op(q, lambda e: e.tensor_scalar(out.ap, a.ap, s1v, s2v, op0, o1, **kw), reads=rd, writes=wr)

    def stt(self, out, a, s, b, op0, op1):
        rd = [a, b]
        sv = s.ap if isinstance(s, V) else float(s)
        if isinstance(s, V):
            rd.append(s)
        self.S.op("dve", lambda e: e.scalar_tensor_tensor(out.ap, a.ap, sv, b.ap, op0, op1), reads=rd, writes=[out])

    def memset(self, q, out, val):
        self.S.op(q, lambda e: e.memset(out.ap, val), writes=[out])

    def dma(self, out, in_, sem, q="sp", **kw):
        self.S.dma(q, lambda e: e.dma_start(out.ap, in_.ap, **kw), sem, reads=[in_], writes=[out])

    def evac_q(self):
        self.rr += 1
        return "act" if self.rr % 2 else "dve"


def _consts(kb):
    nc, S = kb.nc, kb.S
    ones_f = kb.sb([128, 128], F32, "c_onesf")
    ident = kb.sb([128, 128], F32, "c_ident")
    cmask = kb.sb([128, 128], F32, "c_cmask")
    ones_b = kb.sb([128, 128], BF16, "c_onesb")
    kb.memset("pool", ones_f[:, :], 1.0)
    kb.memset("pool", ones_b[:, :], 1.0)
    S.op("pool", lambda e: e.affine_select(ident.h[:, :], ones_f.h[:, :], [[-1, 128]], ALU.is_equal, 0.0,
                                           base=0, channel_multiplier=1),
         reads=[ones_f[:, :]], writes=[ident[:, :]])
    S.op("pool", lambda e: e.affine_select(cmask.h[:, :], ones_f.h[:, :], [[1, 128]], ALU.is_ge, 0.0,
                                           base=0, channel_multiplier=-1),
         reads=[ones_f[:, :]], writes=[cmask[:, :]])
    return dict(ones_f=ones_f, ident=ident, cmask=cmask, ones_b=ones_b)


def _load_weights_bf16(kb, w_dram, ncols, w_sb, stage, tagsem):
    for k in range(16):
        st = stage[k % 2]
        kb.dma(st[:, :ncols], V(w_dram[k * 128:(k + 1) * 128, :], None), f"{tagsem}{k % 2}")
        q = ("act", "dve", "pool")[k % 3]
        kb.copy(q, w_sb.k(k)[:, k, :ncols], st[:, :ncols])


def _tr_tile(kb, src, dst, Pa, Pb, ident, ncols=128):
    for g in range(4):
        Tb = Pa if g % 2 == 0 else Pb
        for j in range(4):
            kk = 4 * g + j
            kb.tr(Tb[:, j * 128:(j + 1) * 128], src[:, kk * 128:(kk + 1) * 128], ident[:, :])
        kb.copy(kb.evac_q(), dst[:, g * 512:(g + 1) * 512], Tb[:, :])


def _ln_tile(kb, y, g_rep, b_rep, out, stats, mv, r):
    S = kb.S
    for c in range(4):
        S.op("dve", lambda e, c=c: e.bn_stats(stats.h[:, c * 6:(c + 1) * 6], y.h[:, c * 512:(c + 1) * 512]),
             reads=[y[:, :]], writes=[stats[:, :]])
    S.op("dve", lambda e: e.bn_aggr(mv.h[:, 0:2], stats.h[:, 0:24]), reads=[stats[:, :]], writes=[mv[:, :]])
    kb.ts("dve", r[:, :], mv[:, 1:2], EPS, ALU.add)
    kb.act(r[:, :], r[:, :], AF.Sqrt)
    S.op("dve", lambda e: e.reciprocal(r.h[:, :], r.h[:, :]), reads=[r[:, :]], writes=[r[:, :]])
    kb.ts("dve", out[:, :], y[:, :], mv[:, 0:1], ALU.subtract, r[:, 0:1], ALU.mult)
    kb.tt("pool", out[:, :], out[:, :], g_rep[:, :], ALU.mult)
    kb.tt("pool", out[:, :], out[:, :], b_rep[:, :], ALU.add)


def _load_w(kb, w_ap, ncols, w_sb, stage, tagsem, col0=0):
    for k in range(16):
        st = stage[k % 2]
        kb.dma(st[:, :ncols], V(w_ap[k * 128:(k + 1) * 128, col0:col0 + ncols], None), f"{tagsem}{k % 2}")
        q = ("act", "dve", "pool")[k % 3]
        kb.copy(q, w_sb.k(k)[:, k, :ncols], st[:, :ncols])


NTT = 64
NTOK = NTT * 128
CAP = 1280
NJ = CAP // 128
RGS = ((0, 512), (512, 512), (1024, 256))
YROWS = 1 + 4 * NTOK + 128


def phases_bcd(nc, S, kb, C, P, io, dbg):
    ident, ones_b, ones_f = C["ident"], C["ones_b"], C["ones_f"]
    T0, T1, F0, F1, M0, M1, M2, PO = P
    mg, x_tok, out_ap = io["merged"], io["x_tok"], io["out"]
    h1s, h2s, ybufs = io["h1s"], io["h2s"], io["ybuf"]
    n_exp = io.get("n_exp", 32)

    kb.stack = S.stack
    idx_tok = kb.sb([128, 32 * NJ], I32, "R_idxtok")
    dest = kb.sb([128, 32 * NJ], I32, "R_dest")
    gl = kb.sb([128, 32 * NJ], F32, "R_gl")

    pB = contextlib.ExitStack()
    kb.stack = pB
    wsb = kb.sb([128, 16, 2048], BF16, "B_w")
    stage = [kb.sb([128, 2048], F32, f"B_st{i}") for i in range(2)]
    g_rep = kb.sb([128, 2048], F32, "B_g")
    b_rep = kb.sb([128, 2048], F32, "B_b")
    mt = [kb.sb([128, 2048], F32, f"B_mt{i}") for i in range(2)]
    xk = [kb.sb([128, 2048], F32, f"B_xk{i}") for i in range(2)]
    mT = [kb.sb([128, 2048], BF16, f"B_mT{i}") for i in range(2)]
    yt = [kb.sb([128, 2048], F32, f"B_y{i}") for i in range(2)]
    stats = kb.sb([128, 24], F32, "B_stats")
    mv = kb.sb([128, 2], F32, "B_mv")
    rr = kb.sb([128, 1], F32, "B_r")
    kb.dma(g_rep[:, :], V(io["ln1g"], None), "c0")
    kb.dma(b_rep[:, :], V(io["ln1b"], None), "c1")
    _load_w(kb, io["w_o"], 2048, wsb, stage, "ws")
    kb.dma(mt[0][:, :], V(mg[0:128, :], "merged"), "mt0")
    kb.dma(xk[0][:, :], V(x_tok[0:128, :], None), "xk0")
    for i in range(NTT):
        sl = i % 2
        if i + 1 < NTT:
            kb.dma(mt[1 - sl][:, :], V(mg[(i + 1) * 128:(i + 2) * 128, :], "merged"), f"mt{1 - sl}")
            kb.dma(xk[1 - sl][:, :], V(x_tok[(i + 1) * 128:(i + 2) * 128, :], None), f"xk{1 - sl}")
        _tr_tile(kb, mt[sl], mT[sl], T0, T1, ident)
        for n, Pb in enumerate((F0, F1, M0, M1)):
            for kk in range(16):
                kb.mm(Pb[:, :], mT[sl][:, kk * 128:(kk + 1) * 128], wsb.k(kk)[:, kk, n * 512:(n + 1) * 512],
                      start=(kk == 0), stop=(kk == 15))
            kb.stt(yt[sl][:, n * 512:(n + 1) * 512], xk[sl][:, n * 512:(n + 1) * 512], ALPHA, Pb[:, :],
                   ALU.mult, ALU.add)
        _ln_tile(kb, yt[sl], g_rep, b_rep, yt[sl], stats, mv, rr)
        kb.dma(V(h1s[i * 128:(i + 1) * 128, :], "h1s"), yt[sl][:, :], f"ho{sl}")
    S.emit()
    pB.close()

    pR = contextlib.ExitStack()
    kb.stack = pR
    gate_all = kb.sb([128, NTT, 32], F32, "R_gate")

    pB = contextlib.ExitStack()
    kb.stack = pB
    wq = kb.sb([128, 16, 2048], BF16, "C_wq")
    wo = kb.sb([128, 16, 2048], BF16, "C_wo")
    g_rep = kb.sb([128, 2048], F32, "C_g")
    b_rep = kb.sb([128, 2048], F32, "C_b")
    big = kb.sb([128, 4096], BF16, "C_big")
    memT = Tile(big.h[:, :].rearrange("p (k m) -> p k m", k=16), "C_memT")
    KT = kb.sb([128, 16, 256], BF16, "C_KT")
    Vs = kb.sb([128, 2, 2048], BF16, "C_V")
    ht0 = kb.sb([128, 2048], F32, "C_ht0")
    ht = [ht0, ht0]
    stage = ht
    hT = kb.sb([128, 2048], BF16, "C_hT")
    qT = Tile(big.h[:, 0:2048], "C_qT")
    pT = Tile(big.h[:, 2048:3072], "C_pT")
    oT = hT
    yt = kb.sb([128, 2048], F32, "C_y")
    memt = yt
    pf = Tile(yt.h[:, 0:1024], "C_y")
    wr = kb.sb([128, 16, 32], F32, "C_wr")
    br = kb.sb([128, 32], F32, "C_br")
    stats = kb.sb([128, 24], F32, "C_stats")
    mv = kb.sb([128, 2], F32, "C_mv")
    rr = kb.sb([128, 1], F32, "C_r")
    mx = kb.sb([128, 4], F32, "C_mx")
    sm = kb.sb([128, 4], F32, "C_sm")
    m8 = kb.sb([128, 8], F32, "C_m8")
    ex = kb.sb([128, 32], F32, "C_ex")
    s1 = kb.sb([128, 2], F32, "C_s1")
    mk = kb.sb([128, 32], F32, "C_mk")
    kb.dma(g_rep[:, :], V(io["ln2g"], None), "c0")
    kb.dma(b_rep[:, :], V(io["ln2b"], None), "c1")
    kb.dma(wr[:, :, :], V(io["w_router"].rearrange("(k p) e -> p k e", p=128), None), "c2")
    kb.dma(br[:, :], V(io["br_rep"], None), "c3")
    for mtile in range(2):
        kb.dma(memt[:, :], V(io["mem_b"][mtile * 128:(mtile + 1) * 128, :], None), "mm")
        for g in range(4):
            Tb = T0 if g % 2 == 0 else T1
            for j in range(4):
                kk = 4 * g + j
                kb.tr(Tb[:, j * 128:(j + 1) * 128], memt[:, kk * 128:(kk + 1) * 128], ident[:, :])
            S.op("dve", lambda e, Tb=Tb, g=g, mtile=mtile: e.tensor_copy(
                memT.h[:, 4 * g:4 * g + 4, mtile * 128:(mtile + 1) * 128],
                Tb.h[:, :].rearrange("p (a b) -> p a b", a=4)), reads=[Tb[:, :]], writes=[memT[:, :, :]])
    _load_w(kb, io["w_kv"], 2048, wq, stage, "ws", col0=0)
    banks = (F0, F1, M0, M1)
    for c in range(16):
        Pb = banks[(c // 2) % 4]
        cs = slice((c % 2) * 256, (c % 2) * 256 + 256)
        for kk in range(16):
            kb.mm(Pb[:, cs], wq.k(kk)[:, kk, c * 128:(c + 1) * 128], memT[:, kk, :], start=(kk == 0), stop=(kk == 15))
        if c % 2 == 1:
            S.op("act", lambda e, Pb=Pb, c=c: e.copy(KT.h[:, c - 1:c + 1, :], Pb.h[:, :].rearrange("p (a b) -> p a b", a=2)),
                 reads=[Pb[:, :]], writes=[KT[:, :, :]])
    _load_w(kb, io["w_kv"], 2048, wq, stage, "ws", col0=2048)
    for mtile in range(2):
        for n in range(4):
            Pb = banks[n]
            for kk in range(16):
                kb.mm(Pb[:, :], memT[:, kk, mtile * 128:(mtile + 1) * 128], wq.k(kk)[:, kk, n * 512:(n + 1) * 512],
                      start=(kk == 0), stop=(kk == 15))
            kb.copy(kb.evac_q(), Vs[:, mtile, n * 512:(n + 1) * 512], Pb[:, :])
    _load_w(kb, io["w_xq"], 2048, wq, stage, "ws")
    _load_w(kb, io["w_xo"], 2048, wo, stage, "ws")
    SC = 512.0 ** -0.5
    for i in range(NTT):
        sl = i % 2
        kb.dma(ht[sl][:, :], V(h1s[i * 128:(i + 1) * 128, :], "h1s"), "ht")
        _tr_tile(kb, ht[sl], hT, T0, T1, ident)
        for c in range(16):
            Pb = banks[c // 4]
            cs = slice((c % 4) * 128, (c % 4) * 128 + 128)
            for kk in range(16):
                kb.mm(Pb[:, cs], wq.k(kk)[:, kk, c * 128:(c + 1) * 128], hT[:, kk * 128:(kk + 1) * 128],
                      start=(kk == 0), stop=(kk == 15))
            if c % 4 == 3:
                g = c // 4
                kb.act(qT[:, g * 512:(g + 1) * 512], Pb[:, :], AF.Identity, scale=SC)
        for hx in range(4):
            Pb = M2 if hx < 2 else PO
            cs = slice((hx % 2) * 256, (hx % 2) * 256 + 256)
            for cc in range(4):
                c = hx * 4 + cc
                kb.mm(Pb[:, cs], qT[:, c * 128:(c + 1) * 128], KT[:, c, :], start=(cc == 0), stop=(cc == 3))
        for hx in range(4):
            Pb = M2 if hx < 2 else PO
            cs = slice((hx % 2) * 256, (hx % 2) * 256 + 256)
            S.op("dve", lambda e, Pb=Pb, cs=cs, hx=hx: e.reduce_max(mx.h[:, hx:hx + 1], Pb.h[:, cs], AX.X),
                 reads=[Pb[:, :]], writes=[mx[:, :]])
        kb.ts("dve", mx[:, :], mx[:, :], -1.0, ALU.mult)
        for hx in range(4):
            Pb = M2 if hx < 2 else PO
            cs = slice((hx % 2) * 256, (hx % 2) * 256 + 256)
            kb.act(pf[:, hx * 256:(hx + 1) * 256], Pb[:, cs], AF.Exp, bias=mx[:, hx:hx + 1], accum=sm[:, hx:hx + 1])
        S.op("dve", lambda e: e.reciprocal(sm.h[:, :], sm.h[:, :]), reads=[sm[:, :]], writes=[sm[:, :]])
        for hx in range(4):
            kb.ts("dve", pf[:, hx * 256:(hx + 1) * 256], pf[:, hx * 256:(hx + 1) * 256], sm[:, hx:hx + 1], ALU.mult)
        for g in range(2):
            Tb = T0 if g == 0 else T1
            for j in range(4):
                kk = 4 * g + j
                kb.tr(Tb[:, j * 128:(j + 1) * 128], pf[:, kk * 128:(kk + 1) * 128], ident[:, :])
            kb.copy(kb.evac_q(), pT[:, g * 512:(g + 1) * 512], Tb[:, :])
        for c in range(16):
            Pb = banks[c // 4]
            cs = slice((c % 4) * 128, (c % 4) * 128 + 128)
            hx = c // 4
            for mc in range(2):
                kb.mm(Pb[:, cs], Vs[:, mc, c * 128:(c + 1) * 128], pT[:, (hx * 2 + mc) * 128:(hx * 2 + mc + 1) * 128],
                      start=(mc == 0), stop=(mc == 1))
            if c % 4 == 3:
                g = c // 4
                kb.copy(kb.evac_q(), oT[:, g * 512:(g + 1) * 512], Pb[:, :])
        for n in range(4):
            Pb = banks[n]
            for kk in range(16):
                kb.mm(Pb[:, :], oT[:, kk * 128:(kk + 1) * 128], wo.k(kk)[:, kk, n * 512:(n + 1) * 512],
                      start=(kk == 0), stop=(kk == 15))
            kb.stt(yt[:, n * 512:(n + 1) * 512], ht[sl][:, n * 512:(n + 1) * 512], ALPHA, Pb[:, :], ALU.mult, ALU.add)
        _ln_tile(kb, yt, g_rep, b_rep, yt, stats, mv, rr)
        kb.dma(V(h2s[i * 128:(i + 1) * 128, :], "h2s"), yt[:, :], "ho")
        h2T = ht[sl]
        for g in range(4):
            Tb = T0 if g % 2 == 0 else T1
            for j in range(4):
                kk = 4 * g + j
                kb.tr(Tb[:, j * 128:(j + 1) * 128], yt[:, kk * 128:(kk + 1) * 128], ident[:, :])
            kb.copy(kb.evac_q(), h2T[:, g * 512:(g + 1) * 512], Tb[:, :])
        for kk in range(16):
            kb.mm(M2[:, 0:32], h2T[:, kk * 128:(kk + 1) * 128], wr[:, kk, :], start=(kk == 0), stop=(kk == 15))
        lg = ex
        kb.tt("dve", lg[:, :], M2[:, 0:32], br[:, :], ALU.add)
        S.op("dve", lambda e: e.max(m8.h[:, :], lg.h[:, :]), reads=[lg[:, :]], writes=[m8[:, :]])
        kb.ts("dve", mk[:, :], lg[:, :], m8[:, 3:4], ALU.is_ge)
        kb.ts("dve", s1[:, 0:1], m8[:, 0:1], -1.0, ALU.mult)
        kb.act(lg[:, :], lg[:, :], AF.Exp, bias=s1[:, 0:1])
        kb.tt("dve", lg[:, :], lg[:, :], mk[:, :], ALU.mult)
        S.op("dve", lambda e: e.reduce_sum(s1.h[:, 1:2], lg.h[:, :], AX.X), reads=[lg[:, :]], writes=[s1[:, :]])
        S.op("dve", lambda e: e.reciprocal(s1.h[:, 1:2], s1.h[:, 1:2]), reads=[s1[:, :]], writes=[s1[:, :]])
        kb.ts("dve", gate_all[:, i, :], lg[:, :], s1[:, 1:2], ALU.mult)
    S.emit()
    pB.close()

    pB = contextlib.ExitStack()
    kb.stack = pB
    mask_bf = kb.sb([128, NTT, 32], BF16, "D_maskbf")
    SU = kb.sb([128, 128], BF16, "D_SU")
    pos = kb.sb([128, NTT, 32], F32, "D_pos")
    slot = kb.sb([128, NTT, 32], F32, "D_slot")
    toki = kb.sb([128, NTT], I32, "D_toki")
    tokf = kb.sb([128, NTT], F32, "D_tokf")
    tokp1 = kb.sb([128, NTT], F32, "D_tokp1")
    pay = kb.sb([128, NTT, 32, 3], F32, "D_pay")
    io_i = kb.sb([128, CAP], I32, "D_ioi")
    io_f = kb.sb([128, CAP], F32, "D_iof")
    tr_i = kb.sb([128, 1], I32, "D_tri")
    tr_f = kb.sb([128, 1], F32, "D_trf")
    zer = kb.sb([128, 128], F32, "D_zer")
    Sel = [kb.sb([128, CAP], F32, f"D_sel{i}") for i in range(3)]
    lst = kb.sb([128, 32, NJ * 3], F32, "D_lst")
    tmpd = kb.sb([128, 32 * NJ], F32, "D_tmpd")
    mask_all = kb.sb([128, NTT, 32], F32, "D_mask")
    kb.memset("pool", zer[:, :], 0.0)
    kb.ts("dve", mask_all[:, :, :], gate_all[:, :, :], 0.0, ALU.is_gt)
    kb.copy("dve", mask_bf[:, :, :], mask_all[:, :, :])
    S.op("pool", lambda e: e.affine_select(SU.h[:, :], ones_f.h[:, :], [[1, 128]], ALU.is_ge, 0.0,
                                           base=-1, channel_multiplier=-1), reads=[ones_f[:, :]], writes=[SU[:, :]])
    S.op("pool", lambda e: e.iota(toki.h[:, :], [[128, NTT]], base=0, channel_multiplier=1), writes=[toki[:, :]])
    S.op("pool", lambda e: e.iota(io_i.h[:, :], [[1, CAP]], base=0, channel_multiplier=0), writes=[io_i[:, :]])
    S.op("pool", lambda e: e.iota(tr_i.h[:, :], [[0, 1]], base=1 + 4 * NTOK, channel_multiplier=1), writes=[tr_i[:, :]])
    kb.copy("dve", tokf[:, :], toki[:, :])
    kb.copy("dve", io_f[:, :], io_i[:, :])
    kb.copy("dve", tr_f[:, :], tr_i[:, :])
    kb.ts("dve", tokp1[:, :], tokf[:, :], 1.0, ALU.add)
    pbanks = (F0, F1, M0, M1)
    for i in range(NTT):
        Pb = pbanks[i // 16]
        cs = slice((i % 16) * 32, (i % 16) * 32 + 32)
        for ip in range(i):
            kb.mm(Pb[:, cs], ones_b[:, :], mask_bf[:, ip, :], start=(ip == 0), stop=False)
        kb.mm(Pb[:, cs], SU[:, :], mask_bf[:, i, :], start=(i == 0), stop=True)
    for q_ in range(NTT // 16):
        S.op("dve", lambda e, q_=q_: e.tensor_copy(pos.h[:, q_ * 16:(q_ + 1) * 16, :],
                                                  pbanks[q_].h[:, :].rearrange("p (a b) -> p a b", a=16)),
             reads=[pbanks[q_][:, :]], writes=[pos[:, :, :]])
    for i in range(NTT):
        S.op("dve", lambda e, i=i: e.tensor_tensor_scan(slot.h[:, i, :], ones_f.h[:, 0:32], mask_all.h[:, i, :], 0.0,
                                                         ALU.mult, ALU.add),
             reads=[ones_f[:, :], mask_all[:, :, :]], writes=[slot[:, :, :]])
    kb.tt("dve", slot[:, :, :], slot[:, :, :], mask_all[:, :, :], ALU.subtract)
    for i in range(NTT):
        kb.ts("dve", pay[:, i, :, 0], mask_all[:, i, :], 0.0, ALU.mult, tokf[:, i:i + 1], ALU.add)
        kb.ts("dve", pay[:, i, :, 2], slot[:, i, :], float(NTOK), ALU.mult, tokp1[:, i:i + 1], ALU.add)
    kb.copy("dve", pay[:, :, :, 1], gate_all[:, :, :])
    n = 0
    for e_ in range(32):
        Pb = (M2, PO)[e_ % 2]
        kb.mm(Pb[:, 0:NJ * 3], zer[:, :], zer[:, 0:NJ * 3], start=True, stop=False)
        for i in range(NTT):
            sel = Sel[n % 3]
            n += 1
            kb.ts("dve", sel[:, :], io_f[:, :], pos[:, i, e_:e_ + 1], ALU.is_equal, mask_all[:, i, e_:e_ + 1], ALU.mult)
            for j in range(NJ):
                kb.mm(Pb[:, 3 * j:3 * j + 3], sel[:, j * 128:(j + 1) * 128], pay[:, i, e_, :],
                      start=False, stop=(i == NTT - 1 and j == NJ - 1))
        kb.copy("act", lst[:, e_, :], Pb[:, 0:NJ * 3])
    l4 = lst.h[:, :, :].rearrange("p e (j c) -> p e j c", c=3)
    iv = idx_tok.h[:, :].rearrange("p (e j) -> p e j", e=32)
    gv = gl.h[:, :].rearrange("p (e j) -> p e j", e=32)
    tv = tmpd.h[:, :].rearrange("p (e j) -> p e j", e=32)
    S.op("dve", lambda e: e.tensor_copy(iv, l4[:, :, :, 0]), reads=[lst[:, :, :]], writes=[idx_tok[:, :]])
    S.op("dve", lambda e: e.tensor_copy(gv, l4[:, :, :, 1]), reads=[lst[:, :, :]], writes=[gl[:, :]])
    S.op("dve", lambda e: e.tensor_scalar(tv, l4[:, :, :, 2], 0.0, tr_f.h[:, 0:1], ALU.is_equal, ALU.mult),
         reads=[lst[:, :, :], tr_f[:, :]], writes=[tmpd[:, :]])
    S.op("dve", lambda e: e.tensor_tensor(tv, tv, l4[:, :, :, 2], ALU.add),
         reads=[lst[:, :, :], tmpd[:, :]], writes=[tmpd[:, :]])
    kb.copy("dve", dest[:, :], tmpd[:, :])
    S.emit()
    pB.close()
    pR.close()

    pB = contextlib.ExitStack()
    kb.stack = pB
    stg = [kb.sb([128, 16, 512], F32, f"E_stg{i}") for i in range(2)]
    wb = [kb.sb([128, 16, 512], BF16, f"E_wb{i}") for i in range(2)]
    XgT = kb.sb([128, 16, CAP], BF16, "E_XgT")
    xg0 = kb.sb([128, 2048], F32, "E_xg0")
    xg = [xg0, xg0]
    actT = kb.sb([128, 16, CAP], BF16, "E_actT")
    Yp = [kb.sb([128, 512], F32, f"E_Y{j}") for j in range(2)]
    bgu = kb.sb([128, 1024], F32, "E_bgu")
    bdr = kb.sb([128, 512], F32, "E_bdr")
    bdb = [kb.sb([128, 512], BF16, f"E_bdb{i}") for i in range(2)]
    glu = kb.sb([128, 512], F32, "E_glu")
    lin = kb.sb([128, 512], F32, "E_lin")
    sg = kb.sb([128, 512], F32, "E_sg")
    kb.dma(bgu[:, :], V(io["bgu_col"], None), "c0")
    kb.memset("pool", bdr[:, :], 0.0)
    for i in range(2):
        kb.memset("pool", bdb[i][:, :], 0.0)
    w_gu, w_dn, b_dn = io["w_gu"], io["w_dn"], io["b_dn"]
    nblk = 0
    ny = 0
    gbanks = ((F0, F1), (M0, M1), (M2, PO))
    dbanks = (F0, F1, M0, M1, M2, PO)
    for e_ in range(n_exp):
        for j in range(NJ):
            col = e_ * NJ + j
            xs = 0
            S.dma("pool", lambda e, xs=xs, col=col: e.indirect_dma_start(
                xg[xs].h[:, :], None, h2s[:, :], bass.IndirectOffsetOnAxis(idx_tok.h[:, col:col + 1], 0)),
                f"xg{xs}", reads=[idx_tok[:, :], V(None, "h2s")], writes=[xg[xs][:, :]])
            for g in range(4):
                Tb = T0 if g % 2 == 0 else T1
                for jj in range(4):
                    kk = 4 * g + jj
                    kb.tr(Tb[:, jj * 128:(jj + 1) * 128], xg[xs][:, kk * 128:(kk + 1) * 128], ident[:, :])
                q = kb.evac_q()
                fn = (lambda e, Tb=Tb, g=g, j=j: e.copy(
                    XgT.h[:, 4 * g:4 * g + 4, j * 128:(j + 1) * 128], Tb.h[:, :].rearrange("p (a b) -> p a b", a=4))) \
                    if q == "act" else (lambda e, Tb=Tb, g=g, j=j: e.tensor_copy(
                        XgT.h[:, 4 * g:4 * g + 4, j * 128:(j + 1) * 128], Tb.h[:, :].rearrange("p (a b) -> p a b", a=4)))
                S.op(q, fn, reads=[Tb[:, :]], writes=[XgT[:, :, :]])
        for cb in range(8):
            bs = nblk % 2
            nblk += 1
            kb.dma(stg[bs][:, :, :], V(w_gu[e_ * D:(e_ + 1) * D, cb * 512:(cb + 1) * 512].rearrange("(k p) c -> p k c", p=128), None),
                   f"stg{bs}")
            sv = stg[bs].h[:, :, :].rearrange("p k (c two) -> p k c two", two=2)
            S.op("act", lambda e, bs=bs, sv=sv: e.copy(wb[bs].h[:, :, 0:256], sv[:, :, :, 0]),
                 reads=[stg[bs][:, :, :]], writes=[wb[bs][:, :, :]])
            S.op("dve", lambda e, bs=bs, sv=sv: e.tensor_copy(wb[bs].h[:, :, 256:512], sv[:, :, :, 1]),
                 reads=[stg[bs][:, :, :]], writes=[wb[bs][:, :, :]])
            for m in range(2):
                mc = cb * 2 + m
                bg = bgu[:, e_ * 32 + mc:e_ * 32 + mc + 1]
                bl = bgu[:, e_ * 32 + 16 + mc:e_ * 32 + 16 + mc + 1]
                for gi, (r0, rn) in enumerate(RGS):
                    Pg, Pl = gbanks[gi]
                    for kk in range(16):
                        kb.mm(Pg[:, 0:rn], wb[bs][:, kk, m * 128:(m + 1) * 128], XgT[:, kk, r0:r0 + rn],
                              start=(kk == 0), stop=(kk == 15))
                    for kk in range(16):
                        kb.mm(Pl[:, 0:rn], wb[bs][:, kk, 256 + m * 128:256 + (m + 1) * 128], XgT[:, kk, r0:r0 + rn],
                              start=(kk == 0), stop=(kk == 15))
                    kb.ts("dve", glu[:, 0:rn], Pg[:, 0:rn], bg, ALU.add, 7.0, ALU.min)
                    kb.ts("dve", lin[:, 0:rn], Pl[:, 0:rn], bl, ALU.add, 7.0, ALU.min)
                    kb.ts("dve", lin[:, 0:rn], lin[:, 0:rn], -7.0, ALU.max, 1.0, ALU.add)
                    kb.act(sg[:, 0:rn], glu[:, 0:rn], AF.Sigmoid, scale=1.702)
                    kb.tt("dve", glu[:, 0:rn], glu[:, 0:rn], sg[:, 0:rn], ALU.mult)
                    kb.tt("dve", actT[:, mc, r0:r0 + rn], glu[:, 0:rn], lin[:, 0:rn], ALU.mult)
        for nb in range(4):
            bs = nblk % 2
            nblk += 1
            kb.dma(stg[bs][:, :, :], V(w_dn[e_ * D:(e_ + 1) * D, nb * 512:(nb + 1) * 512].rearrange("(k p) c -> p k c", p=128), None),
                   f"stg{bs}")
            S.op("act", lambda e, bs=bs: e.copy(wb[bs].h[:, 0:8, :], stg[bs].h[:, 0:8, :]),
                 reads=[stg[bs][:, :, :]], writes=[wb[bs][:, :, :]])
            S.op("dve", lambda e, bs=bs: e.tensor_copy(wb[bs].h[:, 8:16, :], stg[bs].h[:, 8:16, :]),
                 reads=[stg[bs][:, :, :]], writes=[wb[bs][:, :, :]])
            ds_ = nb % 2
            kb.dma(bdr[0:1, :], V(b_dn[e_:e_ + 1, nb * 512:(nb + 1) * 512], None), "bd")
            kb.copy("dve", bdb[ds_][0:1, :], bdr[0:1, :])
            yb = ybufs[nb]
            for j in range(NJ):
                Pb = dbanks[j % 6]
                col = e_ * NJ + j
                for kk in range(16):
                    kb.mm(Pb[:, :], actT[:, kk, j * 128:(j + 1) * 128], wb[bs][:, kk, :], start=(kk == 0), stop=False)
                kb.mm(Pb[:, :], ones_b[:, :], bdb[ds_][:, :], start=False, stop=True)
                ys_ = ny % 2
                ny += 1
                kb.ts("dve", Yp[ys_][:, :], Pb[:, :], gl[:, col:col + 1], ALU.mult)
                S.dma("pool", lambda e, ys_=ys_, col=col, yb=yb: e.indirect_dma_start(
                    yb[:, :], bass.IndirectOffsetOnAxis(dest.h[:, col:col + 1], 0), Yp[ys_].h[:, :], None),
                    f"ys{ys_}", reads=[dest[:, :], Yp[ys_][:, :]], writes=[V(None, "ybuf")])
    S.emit()
    pB.close()

    pB = contextlib.ExitStack()
    kb.stack = pB
    g_rep = kb.sb([128, 2048], F32, "F_g")
    b_rep = kb.sb([128, 2048], F32, "F_b")
    ys = [[kb.sb([128, 2048], F32, f"F_ys{i}_{s_}") for s_ in range(4)] for i in range(2)]
    hh2 = [kb.sb([128, 2048], F32, f"F_h{i}") for i in range(2)]
    yt = [kb.sb([128, 2048], F32, f"F_y{i}") for i in range(2)]
    stats = kb.sb([128, 24], F32, "F_stats")
    mv = kb.sb([128, 2], F32, "F_mv")
    rr = kb.sb([128, 1], F32, "F_r")
    kb.dma(g_rep[:, :], V(io["ln3g"], None), "c0")
    kb.dma(b_rep[:, :], V(io["ln3b"], None), "c1")
    for i in range(NTT):
        sl = i % 2
        kb.dma(hh2[sl][:, :], V(h2s[i * 128:(i + 1) * 128, :], "h2s"), f"fh{sl}")
        for s_ in range(4):
            r0 = 1 + s_ * NTOK + i * 128
            for nb in range(4):
                kb.dma(ys[sl][s_][:, nb * 512:(nb + 1) * 512], V(ybufs[nb][r0:r0 + 128, :], "ybuf"), f"fy{sl}_{s_}")
        kb.tt("dve", yt[sl][:, :], ys[sl][0][:, :], ys[sl][1][:, :], ALU.add)
        kb.tt("pool", ys[sl][2][:, :], ys[sl][2][:, :], ys[sl][3][:, :], ALU.add)
        kb.tt("dve", yt[sl][:, :], yt[sl][:, :], ys[sl][2][:, :], ALU.add)
        kb.stt(yt[sl][:, :], hh2[sl][:, :], ALPHA, yt[sl][:, :], ALU.mult, ALU.add)
        _ln_tile(kb, yt[sl], g_rep, b_rep, yt[sl], stats, mv, rr)
        kb.dma(V(out_ap[i * 128:(i + 1) * 128, :], None), yt[sl][:, :], f"fo{sl}")
    S.emit()
    pB.close()


def _set_cfg(npb):
    global NPB, NPRE, NTT, NTOK, CAP, NJ, RGS, YROWS
    NPB = npb
    NTT = NT_SEQ // npb
    NPRE = NT_SEQ - NTT
    NTOK = NTT * 128
    CAP, RGS = {1: (1280, ((0, 512), (512, 512), (1024, 256))), 2: (640, ((0, 512), (512, 128))),
                4: (384, ((0, 384),))}[npb]
    NJ = CAP // 128
    YROWS = 1 + 4 * NTOK + 128


def build_program(mode="full", dbg_tiles=None, n_exp=32, ng=4, npb=2, npre_dbg=None):
    global NPRE
    _set_cfg(npb)
    if npre_dbg is not None:
        NPRE = npre_dbg
    nc = bass.Bass("TRN2", target_bir_lowering=False)
    stack = contextlib.ExitStack()
    nt = dbg_tiles or NT_SEQ
    dbg = (mode == "A")
    doA = mode in ("full", "A")
    doB = mode in ("full", "BCD")
    NG = ng

    def din(name, shape, dt=F32):
        return nc.dram_tensor(name, list(shape), dt, kind="ExternalInput").ap()

    x_b = din("x_b", [SEQ, D])
    if doA:
        keep_in = din("keep_rep", [128, NT_SEQ])
        w_h_all = din("w_h", [NG * D, 2560])
        bcol_h_all = din("bcol_h", [NG * 128, 8])
        brow_h_all = din("brow_h", [NG, 1536])
        lbl_all = din("lbl", [NG * 128, 8])
        hng_rep = din("hng_rep", [128, 512])
        w_g_all = din("w_g", [NG * D, 2064])
        bcol_g_all = din("bcol_g", [NG * 128, 8])
        brow_g_all = din("brow_g", [NG, 1536])
        wa2_all = din("wa2", [NG * 16, 256])
        gng_rep = din("gng_rep", [128, 512])
        if dbg:
            gh_out = nc.dram_tensor("gh_out", [SEQ, 512], F32, kind="ExternalOutput").ap()
            mg_all = nc.dram_tensor("mg_out", [SEQ, NG * 512], F32, kind="ExternalOutput").ap()
        else:
            gh_out = nc.dram_tensor("gh_scr", [SEQ, 512], F32).ap()
            mg_all = nc.dram_tensor("merged", [NTOK, D], F32).ap()
    io = {}
    if doB:
        if mode == "BCD":
            io["merged"] = din("merged", [NTOK, D])
        else:
            io["merged"] = mg_all
        io["x_tok"] = x_b[NPRE * 128:, :]
        for nm in ("ln1g", "ln1b", "ln2g", "ln2b", "ln3g", "ln3b"):
            io[nm] = din(nm, [128, D])
        io["w_o"] = din("w_o", [D, D])
        io["w_kv"] = din("w_kv", [D, 2 * D])
        io["w_xq"] = din("w_xq", [D, D])
        io["w_xo"] = din("w_xo", [D, D])
        io["mem_b"] = din("mem_b", [256, D])
        io["w_router"] = din("w_router", [D, 32])
        io["br_rep"] = din("br_rep", [128, 32])
        io["bgu_col"] = din("bgu_col", [128, 1024])
        io["w_gu"] = din("w_gu", [32 * D, 2 * D])
        io["w_dn"] = din("w_dn", [32 * D, D])
        io["b_dn"] = din("b_dn", [32, D])
        io["out"] = nc.dram_tensor("out", [NTOK, D], F32, kind="ExternalOutput").ap()
        if mode == "BCD":
            io["h1s"] = nc.dram_tensor("h1s", [NTOK, D], F32, kind="ExternalOutput").ap()
            io["h2s"] = nc.dram_tensor("h2s", [NTOK, D], F32, kind="ExternalOutput").ap()
        else:
            io["h1s"] = nc.dram_tensor("h1s", [NTOK, D], F32).ap()
            io["h2s"] = nc.dram_tensor("h2s", [NTOK, D], F32).ap()
        io["ybuf"] = [nc.dram_tensor(f"ybuf{nb}", [YROWS, 512], F32).ap() for nb in range(4)]
        io["n_exp"] = n_exp

    with stack:
        S = Sched(nc, stack)
        kb = K(nc, S, stack)
        C = _consts(kb)
        ident, cmask, ones_b = C["ident"], C["cmask"], C["ones_b"]

        T0, T1 = kb.ps("pT0"), kb.ps("pT1")
        F0, F1 = kb.ps("pF0"), kb.ps("pF1")
        M0, M1, M2 = kb.ps("pM0"), kb.ps("pM1"), kb.ps("pM2")
        PO = kb.ps("pO")

        if doA:
            pA = contextlib.ExitStack()
            kb.stack = pA
            bF0, bF1, bM0, bM1, bM2, bPO = F0, F1, M0, M1, M2, PO
            for hh in range(NG):
                F0, F1, M0 = bF0, bF1, bM0
                keep = kb.sb([128, NT_SEQ], F32, "keep")
                if hh == 0:
                    kb.dma(keep[:, :], V(keep_in, None), "ck")
                w_h = w_h_all[hh * D:(hh + 1) * D, :]
                bcol_h = bcol_h_all[hh * 128:(hh + 1) * 128, :]
                brow_h = brow_h_all[hh:hh + 1, :]
                lbl = lbl_all[hh * 128:(hh + 1) * 128, :]
                w_g = w_g_all[hh * D:(hh + 1) * D, :]
                bcol_g = bcol_g_all[hh * 128:(hh + 1) * 128, :]
                brow_g = brow_g_all[hh:hh + 1, :]
                wa2_in = wa2_all[hh * 16:(hh + 1) * 16, :]
                mg_out = mg_all[:, hh * 512:(hh + 1) * 512]
                w_sb = kb.sb([128, 16, 2560], BF16, "w_sb")
                stage = [kb.sb([128, 2560], F32, f"wstage{i}") for i in range(2)]
                xt = [kb.sb([128, D], F32, f"xt{i}") for i in range(2)]
                xT = [kb.sb([128, 16 * 128], BF16, f"xT{i}") for i in range(2)]
                bcol = kb.sb([128, 8], F32, "bcol")
                brow = kb.sb([128, 1536], F32, "brow")
                brow_b = kb.sb([128, 1536], BF16, "brow_b")
                lb_in = kb.sb([128, 8], F32, "lb_in")
                lbv = kb.sb([128, 4], F32, "lbv")
                omlb = kb.sb([128, 4], F32, "omlb")
                hng = kb.sb([128, 512], F32, "hng")

                kb.dma(bcol[:, :], V(bcol_h, None), "c0")
                kb.memset("dve", brow[:, :], 0.0)
                kb.dma(brow[0:1, :], V(brow_h, None), "c1")
                kb.dma(lb_in[:, :], V(lbl, None), "c2")
                kb.dma(hng[:, :], V(hng_rep, None), "c3")
                kb.copy("dve", brow_b[:, :], brow[:, :])
                kb.tt("dve", lbv[:, :], lb_in[:, 0:4], lb_in[:, 4:8], ALU.subtract)
                kb.act(lbv[:, :], lbv[:, :], AF.Sigmoid)
                kb.ts("dve", omlb[:, :], lbv[:, :], -1.0, ALU.mult, 1.0, ALU.add)

                _load_weights_bf16(kb, w_h, 2560, w_sb, stage, "ws")

                def wt(name, shape, dt=F32):
                    return kb.sb(shape, dt, name)
                WS = []
                for wi in range(2):
                    WS.append((
                        wt(f"qT{wi}", [128, 512]), wt(f"fT{wi}", [128, 512]), wt(f"kTt{wi}", [128, 512]),
                        wt(f"cum{wi}", [128, 512]), wt(f"e1{wi}", [128, 512]), wt(f"e2{wi}", [128, 512]),
                        wt(f"mid{wi}", [128, 8]), wt(f"sc{wi}", [128, 8]),
                        wt(f"Qt{wi}", [128, 512], BF16), wt(f"Qc{wi}", [128, 512], BF16), wt(f"Kt{wi}", [128, 512], BF16),
                        wt(f"KlT{wi}", [128, 512]), wt(f"Kl{wi}", [128, 512], BF16), wt(f"Vb{wi}", [128, 512], BF16),
                        wt(f"AT{wi}", [128, 512], BF16), wt(f"gate{wi}", [128, 512]), wt(f"g2{wi}", [128, 512]),
                        wt(f"osq{wi}", [128, 512]), wt(f"ss{wi}", [128, 4]), wt(f"rstd{wi}", [128, 4]), wt(f"sx{wi}", [128, 16])))
                St = wt("St", [128, 512])
                Sb = wt("Sb", [128, 512], BF16)
                ot = [wt(f"ot{i}", [128, 512]) for i in range(2)]
                onesf = C["ones_f"]

                kb.memset("dve", St[:, :], 0.0)
                kb.memset("dve", Sb[:, :], 0.0)

                kb.dma(xt[0][:, :], V(x_b[0:128, :], None), "x0")
                def tile_H(t):
                    sl = t % 2
                    full = (t >= NPRE)
                    (qT, fT, kTt, cum, e1, e2, mid, sc, Qt, Qc, Kt, KlT, Kl, Vb, AT, gate, g2, osq, ss, rstd, sx) = WS[sl]
                    if full or sl == 0:
                        F0, F1, M0 = bF0, bF1, bM0
                    else:
                        F0, F1, M0 = bF1, bF0, bM1
                    if t + 1 < nt:
                        kb.dma(xt[1 - sl][:, :], V(x_b[(t + 1) * 128:(t + 2) * 128, :], None), f"x{1 - sl}")
                    for g in range(4):
                        Tb = T0 if g % 2 == 0 else T1
                        for j in range(4):
                            kk = 4 * g + j
                            kb.tr(Tb[:, j * 128:(j + 1) * 128], xt[sl][:, kk * 128:(kk + 1) * 128], ident[:, :])
                        src = V(Tb.h[:, :], None)
                        dst = xT[sl].k(g)[:, g * 512:(g + 1) * 512]
                        q = kb.evac_q()
                        rd = [Tb[:, 0:1] for j in range(4)]
                        if q == "act":
                            S.op("act", lambda e, d=dst, s=src: e.copy(d.ap, s.ap), reads=rd, writes=[dst])
                        else:
                            S.op("dve", lambda e, d=dst, s=src: e.tensor_copy(d.ap, s.ap), reads=rd, writes=[dst])
                    xTr = [xT[sl].k(kk // 4)[:, kk * 128:(kk + 1) * 128] for kk in range(16)]

                    for which, Fb in ((0, F0), (1, F1)):
                        if which == 0 and not full:
                            continue
                        for h in range(4):
                            c0 = which * 512 + h * 128
                            for kk in range(16):
                                kb.mm(Fb[:, h * 128:(h + 1) * 128], w_sb.k(kk)[:, kk, c0:c0 + 128], xTr[kk],
                                      start=(kk == 0), stop=(kk == 15))
                    for n, Mb in enumerate((M0, M1, M2)):
                        if n > 0 and not full:
                            continue
                        c0 = 1024 + n * 512
                        for kk in range(16):
                            kb.mm(Mb[:, :], xTr[kk], w_sb.k(kk)[:, kk, c0:c0 + 512], start=(kk == 0), stop=False)
                        kb.mm(Mb[:, :], ones_b[:, :], brow_b[:, n * 512:(n + 1) * 512], start=False, stop=True)

                    for h in range(4):
                        cs = slice(h * 128, (h + 1) * 128)
                        if full:
                            kb.act(qT[:, cs], F0[:, cs], AF.Silu, bias=bcol[:, h:h + 1])
                        kb.act(fT[:, cs], F1[:, cs], AF.Sigmoid, bias=bcol[:, 4 + h:5 + h])
                        kb.ts("dve", fT[:, cs], fT[:, cs], omlb[:, h:h + 1], ALU.mult, lbv[:, h:h + 1], ALU.add)
                        kb.ts("dve", kTt[:, cs], fT[:, cs], -1.0, ALU.mult, 1.0, ALU.add)
                    kb.act(fT[:, :], fT[:, :], AF.Ln)
                    for h in range(4):
                        cs = slice(h * 128, (h + 1) * 128)
                        S.op("dve", lambda e, cs=cs, cum=cum, fT=fT: e.tensor_tensor_scan(cum.h[:, cs], onesf.h[:, :], fT.h[:, cs], 0.0,
                                                                            ALU.mult, ALU.add),
                             reads=[onesf[:, :], fT[:, cs]], writes=[cum[:, cs]])
                        c63 = h * 128 + 63
                        kb.ts("dve", mid[:, 2 * h:2 * h + 1], cum[:, c63:c63 + 1], -1.0, ALU.mult)
                    cv = cum.h[:, 0:512].rearrange("p (h t) -> p h t", t=128)
                    S.op("act", lambda e, cv=cv, sx=sx: e.activation(sx.h[:, 0:4], cv[:, :, 63], AF.Exp),
                         reads=[cum[:, :]], writes=[sx[:, :]])
                    S.op("act", lambda e, cv=cv, sx=sx: e.activation(sx.h[:, 4:4 + 4], cv[:, :, 127], AF.Exp),
                         reads=[cum[:, :]], writes=[sx[:, :]])
                    S.op("dve", lambda e, sx=sx: e.reciprocal(sx.h[:, 8:8 + 4], sx.h[:, 0:4]), reads=[sx[:, :]], writes=[sx[:, :]])
                    kb.tt("dve", sx[:, 12:12 + 4], sx[:, 4:4 + 4], sx[:, 8:8 + 4], ALU.mult)
                    for h in range(4):
                        cs = slice(h * 128, (h + 1) * 128)
                        c63 = h * 128 + 63
                        if full:
                            kb.act(e1[:, cs], cum[:, cs], AF.Exp, bias=mid[:, 2 * h:2 * h + 1])
                        kb.act(e2[:, cs], cum[:, cs], AF.Exp, bias=cum[:, c63:c63 + 1], scale=-1.0)
                        if full:
                            kb.tt("dve", Qt[:, cs], qT[:, cs], e1[:, cs], ALU.mult)
                            kb.ts("dve", Qc[:, cs], Qt[:, cs], sx[:, h:h + 1], ALU.mult)
                        kb.tt("dve", Kt[:, cs], kTt[:, cs], e2[:, cs], ALU.mult)
                        kb.ts("dve", KlT[:, cs], Kt[:, cs], sx[:, 12 + h:13 + h], ALU.mult)
                    kb.copy("act", Vb[:, :], M0[:, :])
                    if full:
                        kb.act(gate[:, :], M1[:, :], AF.Sigmoid)
                        kb.act(g2[:, :], M2[:, :], AF.Sigmoid)
                        kb.tt("dve", gate[:, :], gate[:, :], g2[:, :], ALU.mult)
                        kb.tt("pool", gate[:, :], gate[:, :], hng[:, :], ALU.mult)
                    yield
                    for h in range(4 if full else 0):
                        cs = slice(h * 128, (h + 1) * 128)
                        kb.mm(F0[:, cs], Kt[:, cs], Qt[:, cs], start=True, stop=True)
                        kb.tt("dve", AT[:, cs], F0[:, cs], cmask[:, :], ALU.mult)
                    for h in range(4):
                        cs = slice(h * 128, (h + 1) * 128)
                        kb.tr(F1[:, cs], KlT[:, cs], ident[:, :])
                    kb.copy("act", Kl[:, :], F1[:, :])
                    for h in range(4 if full else 0):
                        cs = slice(h * 128, (h + 1) * 128)
                        kb.mm(PO[:, cs], AT[:, cs], Vb[:, cs], start=True, stop=False)
                        kb.mm(PO[:, cs], Qc[:, cs], Sb[:, cs], start=False, stop=True)
                    for h in range(4):
                        cs = slice(h * 128, (h + 1) * 128)
                        kb.mm(M0[:, cs], Kl[:, cs], Vb[:, cs], start=True, stop=True)
                    for h in range(4):
                        cs = slice(h * 128, (h + 1) * 128)
                        kb.stt(St[:, cs], St[:, cs], sx[:, 4 + h:5 + h], M0[:, cs], ALU.mult, ALU.add)
                    if not full:
                        kb.ts("dve", St[:, :], St[:, :], keep[:, t:t + 1], ALU.mult)
                    kb.copy("act", Sb[:, :], St[:, :])
                    if not full:
                        return
                    for h in range(4):
                        cs = slice(h * 128, (h + 1) * 128)
                        kb.act(osq[:, cs], PO[:, cs], AF.Square, accum=ss[:, h:h + 1])
                    kb.ts("dve", rstd[:, :], ss[:, :], 1.0 / 128.0, ALU.mult, EPS, ALU.add)
                    kb.act(rstd[:, :], rstd[:, :], AF.Sqrt)
                    S.op("dve", lambda e, rstd=rstd: e.reciprocal(rstd.h[:, :], rstd.h[:, :]), reads=[rstd[:, :]], writes=[rstd[:, :]])
                    o_t = ot[sl]
                    for h in range(4):
                        cs = slice(h * 128, (h + 1) * 128)
                        kb.stt(o_t[:, cs], PO[:, cs], rstd[:, h:h + 1], gate[:, cs], ALU.mult, ALU.mult)
                    kb.dma(V(gh_out[(t - NPRE) * 128:(t - NPRE + 1) * 128, :], f"gh{t}"), o_t[:, :], f"o{sl}")

                gens = {}
                for t in range(nt + 1):
                    if t < nt:
                        gens[t] = tile_H(t)
                        next(gens[t])
                    if t >= 1:
                        for _ in gens.pop(t - 1):
                            pass

                bcg = kb.sb([128, 8], F32, "bcg")
                wa2 = kb.sb([16, 256], F32, "wa2_sb")
                gng = kb.sb([128, 512], F32, "gng")
                ga1T = kb.sb([16, 128], F32, "ga1T")
                ght = [kb.sb([128, 512], F32, f"ght{i}") for i in range(2)]
                kb.dma(bcg[:, :], V(bcol_g, None), "c0")
                kb.memset("dve", brow[:, :], 0.0)
                kb.dma(brow[0:1, :], V(brow_g, None), "c1")
                kb.dma(wa2[:, :], V(wa2_in, None), "c2")
                kb.dma(gng[:, :], V(gng_rep, None), "c3")
                kb.copy("dve", brow_b[:, :], brow[:, :])
                kb.ts("dve", bcg[:, 0:2], bcg[:, 0:2], 1.0 / 16.0, ALU.mult)
                kb.ts("dve", bcg[:, 5:7], bcg[:, 5:7], -1.0, ALU.mult)
                _load_weights_bf16(kb, w_g, 2064, w_sb, stage, "ws")
                kb.memset("dve", St[:, :], 0.0)
                kb.memset("dve", Sb[:, :], 0.0)
                St2 = kb.sb([128, 512], F32, "St2")
                Sb2 = kb.sb([128, 512], BF16, "Sb2")
                kb.memset("dve", St2[:, :], 0.0)
                kb.memset("dve", Sb2[:, :], 0.0)
                Sts, Sbs = (St, St2), (Sb, Sb2)

                kb.dma(xt[0][:, :], V(x_b[0:128, :], None), "x0")
                def tile_G(t):
                    sl = t % 2
                    full = (t >= NPRE)
                    (qT, fT, kTt, cum, e1, e2, mid, sc, Qt, Qc, Kt, KlT, Kl, Vb, AT, gate, g2, osq, ss, rstd, sx) = WS[sl]
                    if full or sl == 0:
                        F0, F1, M0 = bF0, bF1, bM0
                    else:
                        F0, F1, M0 = bM1, bM2, bPO
                    if t + 1 < nt:
                        kb.dma(xt[1 - sl][:, :], V(x_b[(t + 1) * 128:(t + 2) * 128, :], None), f"x{1 - sl}")
                    if full:
                        kb.dma(ght[sl][:, :], V(gh_out[(t - NPRE) * 128:(t - NPRE + 1) * 128, :], f"gh{t}"), f"g{sl}")
                    for g in range(4):
                        Tb = T0 if g % 2 == 0 else T1
                        for j in range(4):
                            kk = 4 * g + j
                            kb.tr(Tb[:, j * 128:(j + 1) * 128], xt[sl][:, kk * 128:(kk + 1) * 128], ident[:, :])
                        dst = xT[sl].k(g)[:, g * 512:(g + 1) * 512]
                        kb.copy(kb.evac_q(), dst, Tb[:, :])
                    xTr = [xT[sl].k(kk // 4)[:, kk * 128:(kk + 1) * 128] for kk in range(16)]
                    for u in range(4):
                        if u < 2 and not full:
                            continue
                        for kk in range(16):
                            kb.mm(F0[:, u * 128:(u + 1) * 128], w_sb.k(kk)[:, kk, u * 128:(u + 1) * 128], xTr[kk],
                                  start=(kk == 0), stop=(kk == 15))
                    for kk in range(16):
                        kb.mm(F1[0:16, 0:128], w_sb.k(kk)[:, kk, 512:528], xTr[kk], start=(kk == 0), stop=(kk == 15))
                    for n, Mb in enumerate((M0, M1, M2)):
                        if n > 0 and not full:
                            continue
                        c0 = 528 + n * 512
                        for kk in range(16):
                            kb.mm(Mb[:, :], xTr[kk], w_sb.k(kk)[:, kk, c0:c0 + 512], start=(kk == 0), stop=False)
                        kb.mm(Mb[:, :], ones_b[:, :], brow_b[:, n * 512:(n + 1) * 512], start=False, stop=True)
                    kb.act(ga1T[:, :], F1[0:16, 0:128], AF.Identity, bias=bcg[0:16, 4:5])
                    for u in range(2):
                        kb.mm(F1[:, 128 + u * 128:256 + u * 128], wa2[:, u * 128:(u + 1) * 128], ga1T[:, :],
                              start=True, stop=True)
                    for u in range(2):
                        cs = slice(u * 128, (u + 1) * 128)
                        if full:
                            kb.act(qT[:, cs], F0[:, cs], AF.Identity, bias=bcg[:, u:u + 1], scale=1.0 / 16.0)
                        kb.act(kTt[:, cs], F0[:, 256 + u * 128:384 + u * 128], AF.Identity, bias=bcg[:, 2 + u:3 + u])
                        kb.act(fT[:, cs], F1[:, 128 + u * 128:256 + u * 128], AF.Exp, bias=bcg[:, 5 + u:6 + u], scale=-1.0)
                    kb.act(fT[:, 0:256], fT[:, 0:256], AF.Ln, bias=1.0)
                    kb.ts("dve", fT[:, 0:256], fT[:, 0:256], -1.0 / 16.0, ALU.mult)
                    for h in range(2):
                        cs = slice(h * 128, (h + 1) * 128)
                        S.op("dve", lambda e, cs=cs, cum=cum, fT=fT: e.tensor_tensor_scan(cum.h[:, cs], onesf.h[:, :], fT.h[:, cs], 0.0,
                                                                            ALU.mult, ALU.add),
                             reads=[onesf[:, :], fT[:, cs]], writes=[cum[:, cs]])
                        c63 = h * 128 + 63
                        kb.ts("dve", mid[:, 2 * h:2 * h + 1], cum[:, c63:c63 + 1], -1.0, ALU.mult)
                    cv = cum.h[:, 0:256].rearrange("p (h t) -> p h t", t=128)
                    S.op("act", lambda e, cv=cv, sx=sx: e.activation(sx.h[:, 0:2], cv[:, :, 63], AF.Exp),
                         reads=[cum[:, :]], writes=[sx[:, :]])
                    S.op("act", lambda e, cv=cv, sx=sx: e.activation(sx.h[:, 4:4 + 2], cv[:, :, 127], AF.Exp),
                         reads=[cum[:, :]], writes=[sx[:, :]])
                    S.op("dve", lambda e, sx=sx: e.reciprocal(sx.h[:, 8:8 + 2], sx.h[:, 0:2]), reads=[sx[:, :]], writes=[sx[:, :]])
                    kb.tt("dve", sx[:, 12:12 + 2], sx[:, 4:4 + 2], sx[:, 8:8 + 2], ALU.mult)
                    for h in range(2):
                        cs = slice(h * 128, (h + 1) * 128)
                        c63 = h * 128 + 63
                        if full:
                            kb.act(e1[:, cs], cum[:, cs], AF.Exp, bias=mid[:, 2 * h:2 * h + 1])
                        kb.act(e2[:, cs], cum[:, cs], AF.Exp, bias=cum[:, c63:c63 + 1], scale=-1.0)
                        if full:
                            kb.tt("dve", Qt[:, cs], qT[:, cs], e1[:, cs], ALU.mult)
                            kb.ts("dve", Qc[:, cs], Qt[:, cs], sx[:, h:h + 1], ALU.mult)
                        kb.tt("dve", Kt[:, cs], kTt[:, cs], e2[:, cs], ALU.mult)
                        kb.ts("dve", KlT[:, cs], Kt[:, cs], sx[:, 12 + h:13 + h], ALU.mult)
                    kb.copy("act", Vb[:, :], M0[:, :])
                    if full:
                        kb.act(gate[:, :], M1[:, :], AF.Silu)
                        kb.act(g2[:, :], M2[:, :], AF.Sigmoid)
                        kb.tt("dve", gate[:, :], gate[:, :], g2[:, :], ALU.mult)
                        kb.tt("pool", gate[:, :], gate[:, :], gng[:, :], ALU.mult)
                    yield
                    for u in range(2 if full else 0):
                        cs = slice(u * 128, (u + 1) * 128)
                        kb.mm(F0[:, 0:128], Kt[:, cs], Qt[:, cs], start=(u == 0), stop=(u == 1))
                    if full:
                        kb.tt("dve", AT[:, 0:128], F0[:, 0:128], cmask[:, :], ALU.mult)
                    for u in range(2):
                        cs = slice(u * 128, (u + 1) * 128)
                        kb.tr(F1[:, cs], KlT[:, cs], ident[:, :])
                    kb.copy("act", Kl[:, 0:256], F1[:, 0:256])
                    if full:
                        kb.mm(PO[:, :], AT[:, 0:128], Vb[:, :], start=True, stop=False)
                    for u in range(2 if full else 0):
                        cs = slice(u * 128, (u + 1) * 128)
                        kb.mm(PO[:, :], Qc[:, cs], Sbs[u][:, :], start=False, stop=(u == 1))
                    for u, Pb in enumerate((M0, F0)):
                        cs = slice(u * 128, (u + 1) * 128)
                        kb.mm(Pb[:, :], Kl[:, cs], Vb[:, :], start=True, stop=True)
                    for u, Pb in enumerate((M0, F0)):
                        kb.stt(Sts[u][:, :], Sts[u][:, :], sx[:, 4 + u:5 + u], Pb[:, :], ALU.mult, ALU.add)
                        if not full:
                            kb.ts("dve", Sts[u][:, :], Sts[u][:, :], keep[:, t:t + 1], ALU.mult)
                        kb.copy("act", Sbs[u][:, :], Sts[u][:, :])
                    if not full:
                        return
                    kb.act(osq[:, :], PO[:, :], AF.Square, accum=ss[:, 0:1])
                    kb.ts("dve", rstd[:, 0:1], ss[:, 0:1], 1.0 / 512.0, ALU.mult, EPS, ALU.add)
                    kb.act(rstd[:, 0:1], rstd[:, 0:1], AF.Sqrt)
                    S.op("dve", lambda e, rstd=rstd: e.reciprocal(rstd.h[:, 0:1], rstd.h[:, 0:1]), reads=[rstd[:, :]], writes=[rstd[:, :]])
                    o_t = ot[sl]
                    kb.stt(o_t[:, :], PO[:, :], rstd[:, 0:1], gate[:, :], ALU.mult, ALU.mult)
                    kb.tt("dve", o_t[:, :], o_t[:, :], ght[sl][:, :], ALU.add)
                    kb.dma(V(mg_out[(t - NPRE) * 128:(t - NPRE + 1) * 128, :], "merged"), o_t[:, :], f"o{sl}")
                gens = {}
                for t in range(nt + 1):
                    if t < nt:
                        gens[t] = tile_G(t)
                        next(gens[t])
                    if t >= 1:
                        for _ in gens.pop(t - 1):
                            pass
            S.emit()
            pA.close()
        if doB:
            phases_bcd(nc, S, kb, C, (T0, T1, F0, F1, M0, M1, M2, PO), io, dbg)
    return nc


def _prep_core_inputs(inp, c):
    b, hh = c // 4, c % 4
    w_in = inp["w_in"][0]
    b_in = inp["b_in"][0]
    offs = np.cumsum([0, 1024, 1024, 2048, 2048, 16, 2048, 2048, 2048, 2048, 2048, 2048])
    o_gq, o_gk, o_gv, o_gr, o_ga1, o_hq, o_hf, o_hi, o_hg, o_ma, o_mb = offs[:11]
    ch = slice(512 * hh, 512 * hh + 512)

    def cols(o, s):
        return np.arange(o + s.start, o + s.stop)
    hcols = np.concatenate([cols(o_hq, ch), cols(o_hf, ch), cols(o_hi, ch), cols(o_hg, ch), cols(o_mb, ch)])
    w_h = np.ascontiguousarray(w_in[:, hcols])
    bh = b_in[hcols]
    bcol_h = np.ascontiguousarray(bh[:1024].reshape(8, 128).T)
    brow_h = np.ascontiguousarray(bh[1024:].reshape(1, 1536))
    lb = inp["hgrn_lb_logits"][:, ch]
    lbl = np.ascontiguousarray(np.concatenate([lb[0].reshape(4, 128).T, lb[1].reshape(4, 128).T], axis=1))
    hng_rep = np.ascontiguousarray(np.broadcast_to(np.tile(inp["hgrn_norm_g"][0], 4)[None, :], (128, 512)))
    kq = slice(256 * hh, 256 * hh + 256)
    gcols = np.concatenate([cols(o_gq, kq), cols(o_gk, kq), np.arange(o_ga1, o_ga1 + 16), cols(o_gv, ch),
                            cols(o_gr, ch), cols(o_ma, ch)])
    w_g = np.ascontiguousarray(w_in[:, gcols])
    bg = b_in[gcols]
    bcol_g = np.zeros((128, 8), np.float32)
    bcol_g[:, 0:4] = bg[:512].reshape(4, 128).T
    bcol_g[:16, 4] = bg[512:528]
    bcol_g[:, 5:7] = inp["b_gla_a"][0][kq].reshape(2, 128).T
    brow_g = np.ascontiguousarray(bg[528:].reshape(1, 1536))
    wa2 = np.ascontiguousarray(inp["w_gla_a2"][0][:, kq])
    gng_rep = np.ascontiguousarray(np.broadcast_to(inp["gla_norm_g"][0][None, :], (128, 512)))
    return dict(x_b=np.ascontiguousarray(inp["x"][b]), w_h=w_h, bcol_h=bcol_h, brow_h=brow_h, lbl=lbl,
                hng_rep=hng_rep, w_g=w_g, bcol_g=bcol_g, brow_g=brow_g, wa2=wa2, gng_rep=gng_rep)


def _prep_a_inputs(inp, b, ng=4, j=0, npb=1):
    parts = [_prep_core_inputs(inp, 4 * b + hh) for hh in range(ng)]
    ntok = SEQ // npb
    own_end = (j + 1) * ntok
    start = own_end - SEQ
    xw = np.zeros((SEQ, D), np.float32)
    xw[max(0, -start):] = inp["x"][b, max(0, start):own_end]
    keep = ((start + 128 * np.arange(NT_SEQ)) >= 0).astype(np.float32)
    out = dict(x_b=xw, hng_rep=parts[0]["hng_rep"], gng_rep=parts[0]["gng_rep"],
               keep_rep=np.ascontiguousarray(np.broadcast_to(keep[None, :], (128, NT_SEQ))))
    for k in ("w_h", "bcol_h", "brow_h", "lbl", "w_g", "bcol_g", "brow_g", "wa2"):
        out[k] = np.ascontiguousarray(np.concatenate([p[k] for p in parts], axis=0))
    return out


def _prep_bcd_inputs(inp, b):
    def rep(v):
        return np.ascontiguousarray(np.broadcast_to(v[None, :], (128, v.shape[0])))
    bgu = inp["b_gate_up"][0]
    bg = bgu[:, 0::2].reshape(32, 16, 128)
    bl = bgu[:, 1::2].reshape(32, 16, 128)
    bgu_col = np.ascontiguousarray(np.concatenate([bg, bl], axis=1).transpose(2, 0, 1).reshape(128, 1024))
    return dict(
        x_b=np.ascontiguousarray(inp["x"][b]),
        ln1g=rep(inp["ln1_g"][0]), ln1b=rep(inp["ln1_b"][0]), ln2g=rep(inp["ln2_g"][0]), ln2b=rep(inp["ln2_b"][0]),
        ln3g=rep(inp["ln3_g"][0]), ln3b=rep(inp["ln3_b"][0]),
        w_o=inp["w_mix_o"][0], w_kv=inp["w_mem_kv"][0], w_xq=inp["w_xq"][0], w_xo=inp["w_xo"][0],
        mem_b=np.ascontiguousarray(inp["mem"][b]), w_router=inp["w_router"][0], br_rep=rep(inp["b_router"][0]),
        bgu_col=bgu_col, b_dn=inp["b_down"][0],
        w_gu=inp["w_gate_up"][0].reshape(32 * D, 2 * D), w_dn=inp["w_down"][0].reshape(32 * D, D))


NPB_FULL = 4


def kernel(**inputs):
    inp = {k: np.asarray(v) for k, v in inputs.items()}
    npb = NPB_FULL
    nc = build_program("full", npb=npb)
    in_maps = []
    for b in range(2):
        bcd = _prep_bcd_inputs(inp, b)
        for j in range(npb):
            m = dict(bcd)
            m.update(_prep_a_inputs(inp, b, 4, j, npb))
            in_maps.append(m)
    n = 2 * npb
    res = run_bass_kernel_spmd(nc, in_maps, core_ids=list(range(n)))
    out = np.stack([np.asarray(r["out"]) for r in res.results], axis=0)
    return out.reshape(2, SEQ, D).astype(np.float32)
```
